# Optimizing a Trainium2 kernel written in Bass

```python
import math
import jax
import jax.numpy as jnp
from jax import lax
import numpy as np

D_MODEL = 1024
BATCH = 8
SEQ = 2048
DEPTH = 4

GRID_W = 64
CTX_LEN = 256
BLK = 128
WINDOW = 128
ROPE_BASE = 10000.0
EPS = 1e-6

HD_A = 64
N_HA = 6
N_KVA = 2
G_A = N_HA // N_KVA
D_INNER = 384
HD_S = 64
N_HS = D_INNER // HD_S
N_GROUPS = 2
D_STATE = 128
CONV_K = 5
CONV_DIM = D_INNER + 2 * N_GROUPS * D_STATE
CHUNK = 128
N_HC = 4
HD_C = 32
D_C = N_HC * 2 * HD_C

D_MIX = N_HA * HD_A + D_INNER + D_C
IN_SPLITS = (N_HA * HD_A, N_KVA * HD_A, N_KVA * HD_A, D_INNER, CONV_DIM, N_HS, N_HS, D_C, D_C, D_C)
D_IN = sum(IN_SPLITS)
IN_OFFSETS = tuple(int(o) for o in np.cumsum(IN_SPLITS)[:-1])

D_FF = 2816
N_EXP = 8
TOP_K = 2
N_DENSE = (DEPTH + 1) // 2
N_MOE = DEPTH // 2
ALPHA = (2 * DEPTH) ** 0.25
BETA = (8 * DEPTH) ** -0.25

kernel_name = "hymba_style_hybrid_dit_block"


def _layernorm(x):
    xf = x.astype(jnp.float32)
    mu = jnp.mean(xf, -1, keepdims=True)
    var = jnp.mean(jnp.square(xf - mu), -1, keepdims=True)
    return ((xf - mu) * lax.rsqrt(var + EPS)).astype(x.dtype)


def _rmsnorm(x, w):
    xf = x.astype(jnp.float32)
    return (xf * lax.rsqrt(jnp.mean(xf * xf, -1, keepdims=True) + EPS)).astype(x.dtype) * w


def _modulate(x, shift, scale):
    return _layernorm(x) * (1 + scale) + shift


def _post_ln(x, g, b):
    return _layernorm(x) * g + b


def _axial_rope(rows, cols, dim, dtype):
    quarter = dim // 4
    inv = ROPE_BASE ** (-jnp.arange(quarter, dtype=jnp.float32) / quarter)
    ang = jnp.concatenate([rows[:, None] * inv, cols[:, None] * inv], axis=-1)
    return jnp.cos(ang).astype(dtype), jnp.sin(ang).astype(dtype)


def _rope(x, cos, sin):
    x1, x2 = jnp.split(x, 2, axis=-1)
    c, s = cos[:, None, :], sin[:, None, :]
    return jnp.concatenate([x1 * c - x2 * s, x1 * s + x2 * c], axis=-1)


def _window_gqa(q, k, v, qc, kc, vc, sink, cos, sin, with_ctx_out):
    B, S, _ = q.shape
    nb = S // BLK
    scale = HD_A ** -0.5
    q = _rope(q.reshape(B, S, N_HA, HD_A), cos, sin)
    k = _rope(k.reshape(B, S, N_KVA, HD_A), cos, sin)
    v = v.reshape(B, S, N_KVA, HD_A)
    kc = kc.reshape(B, -1, N_KVA, HD_A)
    vc = vc.reshape(B, -1, N_KVA, HD_A)
    n_ctx = kc.shape[1]
    sink_f = sink.astype(jnp.float32).reshape(N_KVA, G_A)
    qb = q.reshape(B, nb, BLK, N_KVA, G_A, HD_A)
    pad = ((0, 0), (BLK, BLK), (0, 0), (0, 0))
    kp, vp = jnp.pad(k, pad), jnp.pad(v, pad)
    kw = jnp.concatenate([kp[:, j * BLK:j * BLK + S].reshape(B, nb, BLK, N_KVA, HD_A) for j in range(3)], axis=2)
    vw = jnp.concatenate([vp[:, j * BLK:j * BLK + S].reshape(B, nb, BLK, N_KVA, HD_A) for j in range(3)], axis=2)
    s_loc = jnp.einsum('bnqhgd,bnkhd->bnhgqk', qb, kw).astype(jnp.float32) * scale
    s_ctx = jnp.einsum('bnqhgd,bkhd->bnhgqk', qb, kc).astype(jnp.float32) * scale
    qpos = jnp.arange(S).reshape(nb, BLK)[:, :, None]
    kpos = (jnp.arange(nb)[:, None] * BLK - BLK + jnp.arange(3 * BLK)[None, :])[:, None, :]
    valid = (jnp.abs(qpos - kpos) <= WINDOW) & (kpos >= 0) & (kpos < S)
    s_loc = jnp.where(valid[None, :, None, None], s_loc, -jnp.inf)
    s_sink = jnp.broadcast_to(sink_f[None, None, :, :, None, None], s_loc.shape[:-1] + (1,))
    p = jax.nn.softmax(jnp.concatenate([s_loc, s_ctx, s_sink], axis=-1), axis=-1).astype(v.dtype)
    y = (jnp.einsum('bnhgqk,bnkhd->bnqhgd', p[..., :3 * BLK], vw)
         + jnp.einsum('bnhgqk,bkhd->bnqhgd', p[..., 3 * BLK:3 * BLK + n_ctx], vc)).reshape(B, S, N_HA * HD_A)
    yc = None
    if with_ctx_out:
        qc = qc.reshape(B, n_ctx, N_KVA, G_A, HD_A)
        sc = jnp.einsum('bqhgd,bkhd->bhgqk', qc, kc).astype(jnp.float32) * scale
        sc_sink = jnp.broadcast_to(sink_f[None, :, :, None, None], sc.shape[:-1] + (1,))
        pc = jax.nn.softmax(jnp.concatenate([sc, sc_sink], axis=-1), axis=-1)[..., :n_ctx].astype(vc.dtype)
        yc = jnp.einsum('bhgqk,bkhd->bqhgd', pc, vc).reshape(B, n_ctx, N_HA * HD_A)
    return y, yc


def _segsum(a):
    T = a.shape[-1]
    cs = jnp.cumsum(a, axis=-1)
    diff = cs[..., :, None] - cs[..., None, :]
    return jnp.where(jnp.tril(jnp.ones((T, T), dtype=bool)), diff, -jnp.inf)


def _ssd_chunked(x, a, b, c, h0):
    Bsz, L, H, P = x.shape
    nc = L // CHUNK
    x = x.reshape(Bsz, nc, CHUNK, H, P)
    b = b.reshape(Bsz, nc, CHUNK, H, -1)
    c = c.reshape(Bsz, nc, CHUNK, H, -1)
    a = jnp.transpose(a.reshape(Bsz, nc, CHUNK, H), (0, 3, 1, 2))
    a_cum = jnp.cumsum(a, axis=-1)
    scores = jnp.einsum('bclhn,bcshn->bhcls', c, b) * jnp.exp(_segsum(a))
    y_diag = jnp.einsum('bhcls,bcshp->bclhp', scores, x)
    decay_states = jnp.exp(a_cum[..., -1:] - a_cum)
    states = jnp.einsum('bclhn,bhcl,bclhp->bchpn', b, decay_states, x)
    states = jnp.concatenate([h0[:, None], states], axis=1)
    chunk_tot = jnp.pad(a_cum[..., -1], ((0, 0), (0, 0), (1, 0)))
    decay_chunk = jnp.exp(_segsum(chunk_tot))
    new_states = jnp.einsum('bhzc,bchpn->bzhpn', decay_chunk, states)
    y_off = jnp.einsum('bclhn,bchpn,bhcl->bclhp', c, new_states[:, :-1], jnp.exp(a_cum))
    return (y_diag + y_off).reshape(Bsz, L, H, P), new_states[:, -1]


def _ssd_final_state(x, a, b):
    a_cum = jnp.cumsum(a, axis=1)
    w = jnp.exp(a_cum[:, -1:] - a_cum)
    return jnp.einsum('blhn,blh,blhp->bhpn', b, w, x)


def _dwconv(u, w, bias):
    out = lax.conv_general_dilated(u, w[:, None, :], (1,), ((CONV_K // 2, CONV_K // 2),),
                                   dimension_numbers=('NWC', 'WIO', 'NWC'), feature_group_count=CONV_DIM)
    return out + bias


def _ssd_inputs(xbc, conv_w, conv_b):
    B, L, _ = xbc.shape
    u = jax.nn.silu(_dwconv(xbc, conv_w, conv_b)).astype(jnp.float32)
    xs, bm, cm = jnp.split(u, [D_INNER, D_INNER + N_GROUPS * D_STATE], axis=-1)
    rep = N_HS // N_GROUPS
    bm = jnp.repeat(bm.reshape(B, L, N_GROUPS, D_STATE), rep, axis=2)
    cm = jnp.repeat(cm.reshape(B, L, N_GROUPS, D_STATE), rep, axis=2)
    return xs.reshape(B, L, N_HS, HD_S), bm, cm


def _ssd_direction(lat, ctx_in, dt_raw, dt_raw_c, dt_bias, a_log, reverse, with_ctx_out):
    xs, bm, cm = lat
    xs_c, bm_c, cm_c = ctx_in
    A = -jnp.exp(a_log.astype(jnp.float32))

    def prep(u, d):
        dt = jax.nn.softplus(d.astype(jnp.float32) + dt_bias.astype(jnp.float32))
        return u * dt[..., None], dt * A

    flip = (lambda t: jnp.flip(t, axis=1)) if reverse else (lambda t: t)
    xc_in, ac = prep(xs_c, dt_raw_c)
    xc_in, ac, bc, cc = flip(xc_in), flip(ac), flip(bm_c), flip(cm_c)
    yc = None
    if with_ctx_out:
        h0 = jnp.zeros((xs_c.shape[0], N_HS, HD_S, D_STATE), jnp.float32)
        yc, h_ctx = _ssd_chunked(xc_in, ac, bc, cc, h0)
        yc = flip(yc)
    else:
        h_ctx = _ssd_final_state(xc_in, ac, bc)
    x_in, a = prep(xs, dt_raw)
    y, _ = _ssd_chunked(flip(x_in), flip(a), flip(bm), flip(cm), h_ctx)
    return flip(y), yc


def _ssd_mixer(z, xbc, dtf, dtb, zc, xbcc, dtfc, dtbc, conv_w, conv_b, dt_bias, a_log, d_skip, norm_w,
               with_ctx_out):
    lat = _ssd_inputs(xbc, conv_w, conv_b)
    ctx_in = _ssd_inputs(xbcc, conv_w, conv_b)
    yf, yfc = _ssd_direction(lat, ctx_in, dtf, dtfc, dt_bias[0], a_log[0], False, with_ctx_out)
    yb, ybc = _ssd_direction(lat, ctx_in, dtb, dtbc, dt_bias[1], a_log[1], True, with_ctx_out)
    d = d_skip.astype(jnp.float32)[:, None]

    def finish(y_sum, xs, zz):
        y = (y_sum + d * xs).reshape(zz.shape).astype(zz.dtype)
        return _rmsnorm(y * jax.nn.silu(zz), norm_w)

    y = finish(yf + yb, lat[0], z)
    yc = finish(yfc + ybc, ctx_in[0], zc) if with_ctx_out else None
    return y, yc


def _diff_core(q, k, v, lam):
    s = jnp.einsum('bqhid,bkhid->bhiqk', q, k).astype(jnp.float32) * HD_C ** -0.5
    p = jax.nn.softmax(s, axis=-1)
    a = (p[:, :, 0] - lam * p[:, :, 1]).astype(v.dtype)
    return jnp.einsum('bhqk,bkhe->bqhe', a, v)


def _diff_attn(q, k, v, qc, kc, vc, lam_q, lam_k, lam_init, norm_w, cos, sin, with_ctx_out):
    B, S, _ = q.shape
    nb = S // BLK
    lq = lam_q.astype(jnp.float32)
    lk = lam_k.astype(jnp.float32)
    lam = jnp.exp(jnp.sum(lq[0] * lk[0])) - jnp.exp(jnp.sum(lq[1] * lk[1])) + lam_init
    q = _rope(q.reshape(B, S, N_HC * 2, HD_C), cos, sin).reshape(B, S, N_HC, 2, HD_C)
    k = _rope(k.reshape(B, S, N_HC * 2, HD_C), cos, sin).reshape(B, S, N_HC, 2, HD_C)
    v = v.reshape(B, S, N_HC, 2 * HD_C)
    n_ctx = kc.shape[1]
    kc = kc.reshape(B, n_ctx, N_HC, 2, HD_C)
    vc = vc.reshape(B, n_ctx, N_HC, 2 * HD_C)
    k_all = jnp.concatenate([k, kc], axis=1)
    v_all = jnp.concatenate([v, vc], axis=1)
    qb = jnp.moveaxis(q.reshape(B, nb, BLK, N_HC, 2, HD_C), 1, 0)
    o = lax.map(lambda qblk: _diff_core(qblk, k_all, v_all, lam), qb)
    o = jnp.moveaxis(o, 0, 1).reshape(B, S, N_HC, 2 * HD_C)
    y = (_rmsnorm(o, norm_w) * (1 - lam_init)).reshape(B, S, D_C)
    yc = None
    if with_ctx_out:
        oc = _diff_core(qc.reshape(B, n_ctx, N_HC, 2, HD_C), kc, vc, lam)
        yc = (_rmsnorm(oc, norm_w) * (1 - lam_init)).reshape(B, n_ctx, D_C)
    return y, yc


def _swiglu(h, w_gu, w_down):
    g, u = jnp.split(h @ w_gu, 2, axis=-1)
    return (jax.nn.silu(g) * u) @ w_down


def _moe(h, w_r, b_r, we_gu, we_down):
    logits = (h @ w_r + b_r).astype(jnp.float32)
    top_v, top_i = lax.top_k(logits, TOP_K)
    top_w = jax.nn.softmax(top_v, axis=-1)
    gates = jnp.sum(jax.nn.one_hot(top_i, N_EXP, dtype=jnp.float32) * top_w[..., None], axis=-2).astype(h.dtype)
    y = jnp.zeros_like(h)
    for e in range(N_EXP):
        y = y + gates[..., e:e + 1] * _swiglu(h, we_gu[e], we_down[e])
    return y


def setup_inputs(seed: int = 0) -> dict:
    key = jax.random.key(seed)
    ks = jax.random.split(key, 28)
    f32 = jnp.float32

    def nrm(i, shape, scale):
        return jax.random.normal(ks[i], shape, f32) * scale

    dt0 = jnp.exp(jax.random.uniform(ks[10], (DEPTH, 2, N_HS), f32, math.log(1e-3), math.log(1e-1)))
    return {
        "x": nrm(0, (BATCH, SEQ, D_MODEL), 1.0),
        "c": nrm(1, (BATCH, D_MODEL), 1.0),
        "ctx": nrm(2, (BATCH, CTX_LEN, D_MODEL), 1.0),
        "c_ctx": nrm(3, (D_MODEL,), 1.0),
        "w_ada": nrm(4, (DEPTH, D_MODEL, 6 * D_MODEL), D_MODEL ** -0.5),
        "b_ada": nrm(5, (DEPTH, 6 * D_MODEL), 0.02),
        "w_in": nrm(6, (DEPTH, D_MODEL, D_IN), D_MODEL ** -0.5),
        "attn_sink": nrm(7, (DEPTH, N_HA), 0.5),
        "conv_w": nrm(8, (DEPTH, CONV_K, CONV_DIM), CONV_K ** -0.5),
        "conv_b": nrm(9, (DEPTH, CONV_DIM), 0.02),
        "dt_bias": dt0 + jnp.log(-jnp.expm1(-dt0)),
        "a_log": jnp.log(jax.random.uniform(ks[11], (DEPTH, 2, N_HS), f32, 1.0, 16.0)),
        "d_skip": 1.0 + nrm(12, (DEPTH, N_HS), 0.1),
        "ssm_norm_w": 1.0 + nrm(13, (DEPTH, D_INNER), 0.02),
        "lam_q": nrm(14, (DEPTH, 2, HD_C), 0.1),
        "lam_k": nrm(15, (DEPTH, 2, HD_C), 0.1),
        "diff_norm_w": 1.0 + nrm(16, (DEPTH, 2 * HD_C), 0.02),
        "w_out": nrm(17, (DEPTH, D_MIX, D_MODEL), D_MIX ** -0.5 * BETA),
        "ln1_g": 1.0 + nrm(18, (DEPTH, D_MODEL), 0.02),
        "ln1_b": nrm(19, (DEPTH, D_MODEL), 0.02),
        "ln2_g": 1.0 + nrm(20, (DEPTH, D_MODEL), 0.02),
        "ln2_b": nrm(21, (DEPTH, D_MODEL), 0.02),
        "ffn_w_gu": nrm(22, (N_DENSE, D_MODEL, 2 * D_FF), D_MODEL ** -0.5),
        "ffn_w_down": nrm(23, (N_DENSE, D_FF, D_MODEL), D_FF ** -0.5 * BETA),
        "router_w": nrm(24, (N_MOE, D_MODEL, N_EXP), D_MODEL ** -0.5),
        "router_b": nrm(25, (N_MOE, N_EXP), 0.01),
        "exp_w_gu": nrm(26, (N_MOE, N_EXP, D_MODEL, 2 * D_FF), D_MODEL ** -0.5),
        "exp_w_down": nrm(27, (N_MOE, N_EXP, D_FF, D_MODEL), D_FF ** -0.5 * BETA),
    }


def reference(x, c, ctx, c_ctx, w_ada, b_ada, w_in, attn_sink, conv_w, conv_b, dt_bias, a_log, d_skip,
              ssm_norm_w, lam_q, lam_k, diff_norm_w, w_out, ln1_g, ln1_b, ln2_g, ln2_b, ffn_w_gu, ffn_w_down,
              router_w, router_b, exp_w_gu, exp_w_down):
    B, S, _ = x.shape
    rows_n = S // GRID_W
    t_row = jnp.repeat(jnp.arange(rows_n, dtype=jnp.float32), GRID_W)
    t_col = jnp.tile(jnp.arange(GRID_W, dtype=jnp.float32), rows_n)
    cos_a, sin_a = _axial_rope(t_row, t_col, HD_A, x.dtype)
    cos_c, sin_c = _axial_rope(t_row, t_col, HD_C, x.dtype)
    c_act = jax.nn.silu(c)
    cc_act = jax.nn.silu(c_ctx)

    for li in range(DEPTH):
        ctx_out = li < DEPTH - 1
        lam_init = 0.8 - 0.6 * math.exp(-0.3 * li)
        if li % 2 == 0:
            ffn = lambda h, j=li // 2: _swiglu(h, ffn_w_gu[j], ffn_w_down[j])
        else:
            ffn = lambda h, j=li // 2: _moe(h, router_w[j], router_b[j], exp_w_gu[j], exp_w_down[j])

        sh1, sc1, g1, sh2, sc2, g2 = jnp.split((c_act @ w_ada[li] + b_ada[li])[:, None, :], 6, axis=-1)
        sh1c, sc1c, g1c, sh2c, sc2c, g2c = jnp.split(cc_act @ w_ada[li] + b_ada[li], 6, axis=-1)

        p = jnp.split(_modulate(x, sh1, sc1) @ w_in[li], IN_OFFSETS, axis=-1)
        pc = jnp.split(_modulate(ctx, sh1c, sc1c) @ w_in[li], IN_OFFSETS, axis=-1)
        ya, yac = _window_gqa(p[0], p[1], p[2], pc[0], pc[1], pc[2], attn_sink[li], cos_a, sin_a, ctx_out)
        yb, ybc = _ssd_mixer(p[3], p[4], p[5], p[6], pc[3], pc[4], pc[5], pc[6], conv_w[li], conv_b[li],
                             dt_bias[li], a_log[li], d_skip[li], ssm_norm_w[li], ctx_out)
        yd, ydc = _diff_attn(p[7], p[8], p[9], pc[7], pc[8], pc[9], lam_q[li], lam_k[li], lam_init,
                             diff_norm_w[li], cos_c, sin_c, ctx_out)
        mix = jnp.concatenate([ya, yb, yd], axis=-1) @ w_out[li]
        x = _post_ln(ALPHA * x + g1 * mix, ln1_g[li], ln1_b[li])
        x = _post_ln(ALPHA * x + g2 * ffn(_modulate(x, sh2, sc2)), ln2_g[li], ln2_b[li])

        if ctx_out:
            mix_c = jnp.concatenate([yac, ybc, ydc], axis=-1) @ w_out[li]
            ctx = _post_ln(ALPHA * ctx + g1c * mix_c, ln1_g[li], ln1_b[li])
            ctx = _post_ln(ALPHA * ctx + g2c * ffn(_modulate(ctx, sh2c, sc2c)), ln2_g[li], ln2_b[li])
    return x
```

```python
import contextlib
import math
import numpy as np
import ml_dtypes
import concourse.bass as bass
import concourse.mybir as mybir
from concourse.bass_utils import run_bass_kernel_spmd

F32, BF16 = mybir.dt.float32, mybir.dt.bfloat16
AF = mybir.ActivationFunctionType
ALU = mybir.AluOpType
AX = mybir.AxisListType

D = 1024
NT = 18
T = NT * 128
KC = 8
DEPTH = 4
DFF = 2816
FG = 256
NFG = DFF // FG
NEXP = 8
ALPHA = (2 * DEPTH) ** 0.25
EPS = 1e-6
TGS = [(0, 256), (256, 512), (768, 512), (1280, 512), (1792, 512)]

O_QA, O_KA, O_VA, O_Z, O_XBC, O_DTF, O_DTB, O_QC, O_KC, O_VC = 0, 384, 512, 640, 1024, 1920, 1926, 1932, 2188, 2444


class Op:
    __slots__ = ("idx", "eng", "fn", "dma", "deps", "sig", "cnt", "dsem", "dval", "dprev")

    def __init__(self, idx, eng, fn, dma):
        self.idx, self.eng, self.fn, self.dma = idx, eng, fn, dma
        self.deps = set()
        self.sig = False
        self.cnt = 0
        self.dsem = None
        self.dval = 0
        self.dprev = 0


class Prog:
    ENGS = ("pe", "act", "dve", "pool", "sp")
    NDS = 20
    SEM_CAP = 30000

    def __init__(self, nc):
        self.nc = nc
        self.ops = []
        self.lastw = {}
        self.rd = {}
        self.bar_from = 0

    def add(self, eng, fn, r=(), w=(), dma=False):
        op = Op(len(self.ops), eng, fn, dma)
        deps = set()
        for t in r:
            lw = self.lastw.get(t)
            if lw is not None:
                deps.add(lw)
        for t in w:
            lw = self.lastw.get(t)
            if lw is not None:
                deps.add(lw)
            for o in self.rd.get(t, ()):
                deps.add(o)
        deps.discard(op)
        op.deps = deps
        for t in r:
            self.rd.setdefault(t, set()).add(op)
        for t in w:
            self.lastw[t] = op
            self.rd[t] = set()
        self.ops.append(op)
        return op

    def barrier(self):
        last = {}
        dmas = []
        for op in self.ops[self.bar_from:]:
            if op.dma:
                dmas.append(op)
            elif op.fn is not None:
                last[op.eng] = op
        self.bar_from = len(self.ops)
        for e in self.ENGS:
            op = Op(len(self.ops), e, None, False)
            op.deps = set(last.values()) | set(dmas)
            self.ops.append(op)

    def emit(self):
        nc = self.nc
        ops = self.ops
        for op in ops:
            for d in op.deps:
                if d.dma:
                    continue
                if d.eng == op.eng and op.eng in ("pe", "sp") and not op.dma and op.fn is not None:
                    continue
                d.sig = True
        cnt = {e: 0 for e in self.ENGS}
        for op in ops:
            if op.sig and not op.dma:
                cnt[op.eng] += 1
                op.cnt = cnt[op.eng]
        dcount = {"sp": 0, "pool": 0}
        duse = {}
        for op in ops:
            if op.dma:
                k = dcount[op.eng] % self.NDS
                dcount[op.eng] += 1
                key = (op.eng, k)
                op.dsem = key
                op.dprev = duse.get(key, 0)
                op.dval = op.dprev + 16
                duse[key] = op.dval
        with contextlib.ExitStack() as st:
            esems = {}
            for e in self.ENGS:
                n = cnt[e] // self.SEM_CAP + 1
                esems[e] = [st.enter_context(nc.semaphore(f"se_{e}_{i}")) for i in range(n)]
            dsems = {}
            for q in ("sp", "pool"):
                for k in range(min(self.NDS, dcount[q])):
                    dsems[(q, k)] = st.enter_context(nc.semaphore(f"sd_{q}_{k}"))
            block = st.enter_context(nc.Block())
            cap = self.SEM_CAP

            def run(engname, eh):
                waited = {}

                def wait(sem_key, sem, val):
                    if waited.get(sem_key, 0) >= val:
                        return
                    waited[sem_key] = val
                    eh.wait_ge(sem, val)

                for op in ops:
                    if op.eng != engname:
                        continue
                    for d in sorted(op.deps, key=lambda o: o.idx):
                        if d.dma:
                            wait(("d",) + d.dsem, dsems[d.dsem], d.dval)
                        else:
                            if d.eng == engname and engname in ("pe", "sp") and not op.dma and op.fn is not None:
                                continue
                            si, sv = divmod(d.cnt - 1, cap)
                            wait(("e", d.eng, si), esems[d.eng][si], sv + 1)
                    if op.fn is None:
                        continue
                    if op.dma:
                        if op.dprev > 0:
                            wait(("d",) + op.dsem, dsems[op.dsem], op.dprev)
                        ins = op.fn(eh)
                        ins.then_inc(dsems[op.dsem], 16)
                    else:
                        ins = op.fn(eh)
                        if op.sig:
                            si, sv = divmod(op.cnt - 1, cap)
                            ins.then_inc(esems[engname][si], 1)
                if engname == "sp":
                    for key, v in duse.items():
                        wait(("d",) + key, dsems[key], v)

            @block.tensor
            def _(t):
                run("pe", t)

            @block.scalar
            def _(s):
                run("act", s)

            @block.vector
            def _(v):
                run("dve", v)

            @block.gpsimd
            def _(g):
                run("pool", g)

            @block.sync
            def _(s):
                run("sp", s)


class PsumPool:
    def __init__(self, banks):
        self.banks = list(banks)
        self.i = 0

    def next(self):
        b = self.banks[self.i % len(self.banks)]
        self.i += 1
        return b


class Builder:
    def __init__(self, nc, dbg=None, nlayers=DEPTH):
        self.nc = nc
        self.P = Prog(nc)
        self.dbg = dbg or {}
        self.nlayers = nlayers
        self.uid = 0

    def tok(self, name):
        self.uid += 1
        return (name, self.uid)

    def dram_in(self, name, shape, dt=F32):
        return self.nc.dram_tensor(name, list(shape), dt, kind="ExternalInput").ap()

    def dram_out(self, name, shape, dt=F32):
        return self.nc.dram_tensor(name, list(shape), dt, kind="ExternalOutput").ap()

    def dma(self, q, out, in_, r=(), w=()):
        return self.P.add(q, lambda e: e.dma_start(out=out, in_=in_), r, w, dma=True)

    def mm(self, out, pairs, r=(), w=(), tile_position=None, start=True):
        def fn(e):
            n = len(pairs)
            ins = None
            for i, (l, rh) in enumerate(pairs):
                kw = {}
                if tile_position is not None:
                    kw["tile_position"] = tile_position
                ins = e.matmul(out, lhsT=l, rhs=rh, start=(start and i == 0), stop=(i == n - 1), **kw)
            return ins
        return self.P.add("pe", fn, r, w)

    def act(self, out, in_, func, r=(), w=(), bias=None, scale=None, accum_out=None):
        def fn(e):
            kw = {}
            if bias is not None:
                kw["bias"] = bias
            if scale is not None:
                kw["scale"] = scale
            if accum_out is not None:
                kw["accum_out"] = accum_out
            return e.activation(out=out, in_=in_, func=func, **kw)
        return self.P.add("act", fn, r, w)

    def tt(self, eng, out, in0, in1, op, r=(), w=()):
        return self.P.add(eng, lambda e: e.tensor_tensor(out=out, in0=in0, in1=in1, op=op), r, w)

    def ts(self, eng, out, in0, s1, op0, s2=None, op1=None, r=(), w=()):
        def fn(e):
            if op1 is None:
                return e.tensor_scalar(out=out, in0=in0, scalar1=s1, scalar2=None, op0=op0)
            return e.tensor_scalar(out=out, in0=in0, scalar1=s1, scalar2=s2, op0=op0, op1=op1)
        return self.P.add(eng, fn, r, w)

    def stt(self, out, in0, scalar, in1, op0, op1, r=(), w=()):
        return self.P.add("dve", lambda e: e.scalar_tensor_tensor(out=out, in0=in0, scalar=scalar, in1=in1,
                                                                   op0=op0, op1=op1), r, w)

    def copy(self, eng, out, in_, r=(), w=()):
        if eng == "act":
            return self.P.add("act", lambda e: e.copy(out=out, in_=in_), r, w)
        return self.P.add(eng, lambda e: e.tensor_copy(out=out, in_=in_), r, w)

    def memset(self, eng, ap, val, w=()):
        return self.P.add(eng, lambda e: e.memset(ap, val), (), w)


class Ctx:
    pass


def declare_inputs(B):
    I = {}
    I["xin"] = B.dram_in("xin", [T, D])
    I["cvec"] = B.dram_in("cvec", [128, KC, 2])
    I["ident"] = B.dram_in("ident", [128, 128])
    I["w_ada"] = B.dram_in("w_ada", [DEPTH, D, 6 * D])
    I["b_adaT"] = B.dram_in("b_adaT", [DEPTH, 128, 48])
    I["lnrows"] = B.dram_in("lnrows", [DEPTH, 4, 128, D])
    I["ffn_gu"] = B.dram_in("ffn_gu", [2, NFG, 2, 128, KC, FG])
    I["ffn_dn"] = B.dram_in("ffn_dn", [2, DFF, D])
    I["exp_gu"] = B.dram_in("exp_gu", [2, NEXP, NFG, 2, 128, KC, FG])
    I["exp_dn"] = B.dram_in("exp_dn", [2, NEXP, DFF, D])
    I["router_w"] = B.dram_in("router_w", [2, 128, KC, NEXP])
    I["router_b"] = B.dram_in("router_b", [2, 128, NEXP])
    I["w_fm"] = B.dram_in("w_fm", [DEPTH, NCH, 128, KC, 128])
    I["w_tok"] = B.dram_in("w_tok", [DEPTH, 128, KC, NTK])
    I["w_out"] = B.dram_in("w_out", [DEPTH, D, D])
    I["srow"] = B.dram_in("srow", [DEPTH, 128, NSR])
    I["convp"] = B.dram_in("convp", [DEPTH, 128, 7, 6])
    I["ropeA"] = B.dram_in("ropeA", [2, 128, T], BF16)
    I["ropeC"] = B.dram_in("ropeC", [2, 128, T], BF16)
    I["bmask"] = B.dram_in("bmask", [128, 3, 128], BF16)
    I["ssdc"] = B.dram_in("ssdc", [4, 128, 128])
    I["bandm"] = B.dram_in("bandm", [128, 6])
    return I


def build_program(nc, nlayers=DEPTH, dbg=(), inject_x1=False):
    B = Builder(nc)
    P = B.P
    I = declare_inputs(B)
    out_d = B.dram_out("out", [2048, D])
    dbg_d = {}
    st = contextlib.ExitStack()
    sb = lambda name, shape, dt=F32: st.enter_context(nc.sbuf_tensor(name, list(shape), dt))
    C = Ctx()
    C.B, C.P, C.I, C.nc = B, P, I, nc
    C.x = sb("x", [128, NT, D])
    C.xT = sb("xT", [128, KC, T], BF16)
    C.identf = sb("identf", [128, 128])
    C.identb = sb("identb", [128, 128], BF16)
    C.onesf = sb("onesf", [128, 128])
    C.cact = sb("cact", [128, KC, 2])
    C.adaT = sb("adaT", [128, 48, 2])
    C.scp = sb("scp", [128, 2, KC, 2])
    C.mv = sb("mv", [128, NT, 2])
    C.rstd = sb("rstd", [128, NT])
    C.bnst = sb("bnst", [128, NT, 2, 6])
    C.psum = st.enter_context(nc.psum_tensor("psum", [128, 8, 512], F32))
    ps = C.psum

    xin_v = I["xin"].rearrange("(t p) d -> p t d", p=128)
    for t in range(NT):
        B.dma("sp", C.x[:, t, :], xin_v[:, t, :], w=[("x", t)])
    B.dma("sp", C.identf[:, :], I["ident"][:, :], w=["identf"])
    B.dma("sp", C.cact[:, :, :], I["cvec"][:, :, :], w=["cact"])
    B.copy("dve", C.identb[:, :], C.identf[:, :], r=["identf"], w=["identb"])
    B.memset("pool", C.onesf[:, :], 1.0, w=["onesf"])
    B.act(C.cact[:, :, :], C.cact[:, :, :], AF.Silu, r=["cact"], w=["cact"])
    for t in range(NT):
        B.ts("pool", C.x[:, t, :], C.x[:, t, :], ALPHA, ALU.mult, r=[("x", t)], w=[("x", t)])

    C.nlayers = nlayers
    C.dbg = dbg
    C.dbgd = {}
    for li in range(nlayers):
        last = (li == DEPTH - 1)
        tiles = list(range(2, NT)) if last else list(range(NT))
        mk = lambda sc, tag, li=li: (lambda name, shape, dt=F32: sc.enter_context(nc.sbuf_tensor("%s_%s%d" % (name, tag, li), list(shape), dt)))
        with contextlib.ExitStack() as sc:
            C.sb = mk(sc, "m")
            C.dg = C.sb("dg", [128, 2, 128])
            with contextlib.ExitStack() as sc2:
                C.sb2 = mk(sc2, "m0")
                C.xn = C.sb2("xn", [128, 2, D], BF16)
                ada_params(C, li)
                ln_modulate(C, which_sc=0, sh_base=0)
                P.barrier()
            if "xT%d" % li in dbg:
                d = B.dram_out("dbg_xT%d" % li, [128, KC, T], BF16)
                B.dma("sp", d[:, :, :], C.xT[:, :, :], r=[("xT", t) for t in range(NT)])
            if inject_x1:
                d = B.dram_in("dbg_x1_%d" % li, [T, D])
                dv = d.rearrange("(t p) d -> p t d", p=128)
                for t in range(NT):
                    B.dma("sp", C.x[:, t, :], dv[:, t, :], w=[("x", t)])
            else:
                mixer_phase(C, li, tiles)
                with contextlib.ExitStack() as sc2:
                    C.rows = mk(sc2, "m9")("lnrows", [128, 2, D])
                    post_ln(C, li, 0, tiles, out_scale=ALPHA)
                    P.barrier()
            if "x1_%d" % li in dbg:
                dump_x(C, "dbg_x1_%d" % li)
            P.barrier()
        with contextlib.ExitStack() as sc:
            C.sb = lambda name, shape, dt=F32, li=li, sc=sc: sc.enter_context(nc.sbuf_tensor("%s_f%d" % (name, li), list(shape), dt))
            C.xn = C.sb("xn", [128, 2, D], BF16)
            C.rows = C.sb("lnrows", [128, 2, D])
            C.dg = C.sb("dg", [128, 2, 128])
            ffn_phase(C, li, tiles)
            if "xpre%d" % li in dbg:
                dump_x(C, "dbg_xpre%d" % li)
            post_ln(C, li, 1, tiles, out_scale=(1.0 if li == nlayers - 1 else ALPHA))
            if "x2_%d" % li in dbg:
                dump_x(C, "dbg_x2_%d" % li)
            P.barrier()
    ov = out_d.rearrange("(t p) d -> p t d", p=128)
    for t in range(2, NT):
        B.dma("sp", ov[:, t - 2, :], C.x[:, t, :], r=[("x", t)])
    P.emit()
    st.close()
    return nc


def dbg_out(C, name, w):
    if name not in C.dbgd:
        C.dbgd[name] = C.B.dram_out("dbg_" + name, [T, w], BF16)
    return C.dbgd[name]


def dump_x(C, name):
    d = C.B.dram_out(name, [T, D])
    dv = d.rearrange("(t p) d -> p t d", p=128)
    for t in range(NT):
        C.B.dma("sp", dv[:, t, :], C.x[:, t, :], r=[("x", t)])


def ada_params(C, li):
    B, nc, I, ps = C.B, C.nc, C.I, C.psum
    wb = C.sb2("adaw", [128, 2, KC, 256])
    bT = C.sb2("adab", [128, 48])
    B.dma("sp", bT[:, :], I["b_adaT"][li, :, :], w=["adab"])
    wv = I["w_ada"][li].rearrange("(kc p) f -> p kc f", p=128)
    bank = 7
    for piece in range(24):
        s = piece % 2
        B.dma("sp", wb[:, s, :, :], wv[:, :, piece * 256:(piece + 1) * 256], w=[("adaw", s)])
        for j in range(2):
            fc = piece * 2 + j
            B.mm(ps[:, bank, fc * 2:fc * 2 + 2],
                 [(wb[:, s, kc, j * 128:(j + 1) * 128], C.cact[:, kc, :]) for kc in range(KC)],
                 r=[("adaw", s), "cact"], w=[("ps", bank)])
    B.tt("dve", C.adaT[:, :, :], ps[:, bank, 0:96].rearrange("p (f w) -> p f w", w=2),
         bT[:, :].unsqueeze(2).to_broadcast([128, 48, 2]), ALU.add,
         r=[("ps", bank), "adab"], w=["adaT"])
    B.ts("pool", C.scp[:, 0, :, :], C.adaT[:, 8:16, :], 1.0, ALU.add, r=["adaT"], w=["scp"])
    B.ts("pool", C.scp[:, 1, :, :], C.adaT[:, 32:40, :], 1.0, ALU.add, r=["adaT"], w=["scp"])


def ln_stats(C, tiles, eps=EPS):
    B = C.B
    for t in tiles:
        for h in range(2):
            C.P.add("dve", lambda e, t=t, h=h: e.bn_stats(out=C.bnst[:, t, h, :], in_=C.x[:, t, h * 512:(h + 1) * 512]),
                    r=[("x", t), ("x", t, h)], w=[("bnst", t)])
        C.P.add("dve", lambda e, t=t: e.bn_aggr(out=C.mv[:, t, :], in_=C.bnst[:, t, :, :].rearrange("p a b -> p (a b)")),
                r=[("bnst", t)], w=[("mv", t)])
    t0, t1 = tiles[0], tiles[-1] + 1
    B.act(C.rstd[:, t0:t1], C.mv[:, t0:t1, 1], AF.Sqrt, bias=eps, r=[("mv", t) for t in tiles], w=[("rstd", t) for t in tiles])
    C.P.add("dve", lambda e: e.reciprocal(out=C.rstd[:, t0:t1], in_=C.rstd[:, t0:t1]),
            r=[("rstd", t) for t in tiles], w=[("rstd", t) for t in tiles])


def ln_modulate(C, which_sc, sh_base, ctx_tiles=True):
    B, nc, ps = C.B, C.nc, C.psum
    tiles = list(range(NT)) if ctx_tiles else list(range(2, NT))
    ln_stats(C, tiles, eps=EPS * ALPHA * ALPHA)
    if True:
        xn = C.xn
        pool = PsumPool([0, 1])
        for i, t in enumerate(tiles):
            s = i % 2
            wch = 1 if t < 2 else 0
            B.ts("dve", xn[:, s, :], C.x[:, t, :], C.mv[:, t, 0:1], ALU.subtract, C.rstd[:, t:t + 1], ALU.mult,
                 r=[("x", t), ("mv", t), ("rstd", t)], w=[("xn", s)])
            bk = pool.next()
            psb = ps[:, bk, :].bitcast(BF16)

            def tr(e, s=s, psb=psb):
                ins = None
                for kc in range(KC):
                    ins = e.transpose(out=psb[:, kc * 128:(kc + 1) * 128], in_=xn[:, s, kc * 128:(kc + 1) * 128],
                                      identity=C.identb[:, :])
                return ins
            C.P.add("pe", tr, r=[("xn", s), "identb"], w=[("ps", bk)])
            for kc in range(KC):
                B.act(C.xT[:, kc, t * 128:(t + 1) * 128], psb[:, kc * 128:(kc + 1) * 128], AF.Identity,
                      scale=C.scp[:, which_sc, kc, wch:wch + 1], bias=C.adaT[:, sh_base + kc, wch:wch + 1],
                      r=[("ps", bk), "scp", "adaT"], w=[("xT", t)])


def _shared_inputs(inp):
    f32 = np.float32
    S = {}
    S["ident"] = np.eye(128, dtype=f32)
    S["w_ada"] = np.ascontiguousarray(inp["w_ada"], dtype=f32)
    S["b_adaT"] = np.ascontiguousarray(inp["b_ada"].reshape(DEPTH, 48, 128).transpose(0, 2, 1), dtype=f32)
    ln = np.stack([inp["ln1_g"], inp["ln1_b"], inp["ln2_g"], inp["ln2_b"]], axis=1)
    S["lnrows"] = np.ascontiguousarray(np.broadcast_to(ln[:, :, None, :], (DEPTH, 4, 128, D)), dtype=f32)
    g = inp["ffn_w_gu"].reshape(2, KC, 128, 2, NFG, FG)
    S["ffn_gu"] = np.ascontiguousarray(g.transpose(0, 4, 3, 2, 1, 5), dtype=f32)
    S["ffn_dn"] = np.ascontiguousarray(inp["ffn_w_down"], dtype=f32)
    g = inp["exp_w_gu"].reshape(2, NEXP, KC, 128, 2, NFG, FG)
    S["exp_gu"] = np.ascontiguousarray(g.transpose(0, 1, 5, 4, 3, 2, 6), dtype=f32)
    S["exp_dn"] = np.ascontiguousarray(inp["exp_w_down"], dtype=f32)
    S["router_w"] = np.ascontiguousarray(inp["router_w"].reshape(2, KC, 128, NEXP).transpose(0, 2, 1, 3), dtype=f32)
    S["router_b"] = np.ascontiguousarray(np.broadcast_to(inp["router_b"][:, None, :], (2, 128, NEXP)), dtype=f32)
    i128 = np.arange(128)

    def rot(i, hd):
        d = i % hd
        return (i // hd) * hd + np.where(d < hd // 2, d + hd // 2, d - hd // 2)
    cols = []
    for c in range(3):
        cols.append(O_QA + c * 128 + i128)
    for c in range(3):
        cols.append(O_QA + c * 128 + rot(i128, 64))
    for g in range(2):
        cols.append(O_KA + g * 64 + (i128 % 64))
    for g in range(2):
        cols.append(O_KA + g * 64 + rot(i128 % 64, 64))
    for base in (O_QC, O_KC):
        for c in range(2):
            cols.append(base + c * 128 + i128)
        for c in range(2):
            cols.append(base + c * 128 + rot(i128, 32))
    for ci in range(7):
        cols.append(O_XBC + ci * 128 + i128)
    cols = np.stack(cols)
    w_in = inp["w_in"]
    wf = w_in[:, :, cols]
    S["w_fm"] = np.ascontiguousarray(wf.reshape(DEPTH, KC, 128, NCH, 128).transpose(0, 3, 2, 1, 4), dtype=f32)
    tcols = np.concatenate([O_VA + np.arange(128), O_VC + np.arange(256), O_DTF + np.arange(12), O_Z + np.arange(384)])
    wt = w_in[:, :, tcols]
    S["w_tok"] = np.ascontiguousarray(wt.reshape(DEPTH, KC, 128, NTK).transpose(0, 2, 1, 3), dtype=f32)
    S["w_out"] = np.ascontiguousarray(inp["w_out"], dtype=f32)
    sr = np.concatenate([inp["attn_sink"], inp["dt_bias"].reshape(DEPTH, 12), inp["a_log"].reshape(DEPTH, 12), inp["d_skip"],
                         inp["lam_q"].reshape(DEPTH, 64), inp["lam_k"].reshape(DEPTH, 64), inp["diff_norm_w"], inp["ssm_norm_w"]], axis=1)
    S["srow"] = np.ascontiguousarray(np.broadcast_to(sr[:, None, :], (DEPTH, 128, NSR)), dtype=f32)
    cw = np.concatenate([inp["conv_w"], inp["conv_b"][:, None, :]], axis=1)
    S["convp"] = np.ascontiguousarray(cw.reshape(DEPTH, 6, 7, 128).transpose(0, 3, 2, 1), dtype=f32)
    tt = np.arange(2048)
    rows, colsg = (tt // 64).astype(f32), (tt % 64).astype(f32)

    def rope_tab(hd):
        quarter = hd // 4
        inv = (10000.0 ** (-np.arange(quarter, dtype=f32) / quarter)).astype(f32)
        ang = np.concatenate([rows[:, None] * inv, colsg[:, None] * inv], axis=-1).astype(f32)
        d = i128 % hd
        jj = d % (hd // 2)
        sign = np.where(d < hd // 2, -1.0, 1.0).astype(f32)
        cos = np.ones((128, T), f32)
        sin = np.zeros((128, T), f32)
        cos[:, 256:] = np.cos(ang).astype(f32)[:, jj].T
        sin[:, 256:] = np.sin(ang).astype(f32)[:, jj].T * sign[:, None]
        return np.stack([cos, sin]).astype(ml_dtypes.bfloat16)
    S["ropeA"] = rope_tab(64)
    S["ropeC"] = rope_tab(32)
    kk, qq = np.meshgrid(np.arange(128), np.arange(128), indexing="ij")
    S["bmask"] = np.stack([(qq <= kk), np.ones_like(kk, bool), (qq >= kk)], axis=1).astype(f32).astype(ml_dtypes.bfloat16)
    jj, ll = np.meshgrid(np.arange(128), np.arange(128), indexing="ij")
    pp = np.arange(128)
    S["bandm"] = np.stack([(pp // 32 == 0), (pp // 32 == 1), (pp // 32 == 2), (pp // 32 == 3), (pp // 64 == 0), (pp // 64 == 1)], axis=1).astype(f32)
    S["ssdc"] = np.stack([(jj <= ll).astype(f32), (jj >= ll).astype(f32),
                          np.where(ll >= jj, 0.0, -30000.0).astype(f32), np.where(ll <= jj, 0.0, -30000.0).astype(f32)]).astype(f32)
    return S


def host_inputs(inp, b, shared=None):
    f32 = np.float32
    m = dict(shared if shared is not None else _shared_inputs(inp))
    m["xin"] = np.ascontiguousarray(np.concatenate([inp["ctx"][b], inp["x"][b]], axis=0), dtype=f32)
    cc = np.stack([inp["c"][b], inp["c_ctx"]], axis=0)
    m["cvec"] = np.ascontiguousarray(cc.reshape(2, KC, 128).transpose(2, 1, 0), dtype=f32)
    return m


def bcast_rows(C, dst, src_fc0, name):
    B, nc, ps = C.B, C.nc, C.psum
    if True:
        dg = C.dg
        k = 0
        for wch in range(2):
            for half in range(2):
                bank = 6 + (k % 2)
                for q in range(4):
                    kc = half * 4 + q
                    s = (k * 4 + q) % 2
                    B.ts("dve", dg[:, s, :], C.identf[:, :], C.adaT[:, src_fc0 + kc, wch:wch + 1], ALU.mult,
                         r=["identf", "adaT"], w=[("diag", s)])
                    B.mm(ps[:, bank, q * 128:(q + 1) * 128], [(C.onesf[:, :], dg[:, s, :])],
                         r=["onesf", ("diag", s)], w=[("ps", bank)])
                B.copy("act", dst[:, wch, half * 512:(half + 1) * 512], ps[:, bank, :], r=[("ps", bank)], w=[name])
                k += 1


def post_ln(C, li, idx, tiles, out_scale=1.0):
    B, nc = C.B, C.nc
    ln_stats(C, tiles)
    if True:
        rows = C.rows
        for j in range(2):
            B.dma("sp", rows[:, j, :], C.I["lnrows"][li, 2 * idx + j, :, :], w=[("lnrow", j)])
            if out_scale != 1.0:
                B.ts("pool", rows[:, j, :], rows[:, j, :], out_scale, ALU.mult, r=[("lnrow", j)], w=[("lnrow", j)])
        for t in tiles:
            B.ts("dve", C.x[:, t, :], C.x[:, t, :], C.mv[:, t, 0:1], ALU.subtract, C.rstd[:, t:t + 1], ALU.mult,
                 r=[("x", t), ("mv", t), ("rstd", t)], w=[("x", t)])
            B.tt("pool", C.x[:, t, :], C.x[:, t, :], rows[:, 0, :], ALU.mult, r=[("x", t), ("lnrow", 0)], w=[("x", t)])
            B.tt("pool", C.x[:, t, :], C.x[:, t, :], rows[:, 1, :], ALU.add, r=[("x", t), ("lnrow", 1)], w=[("x", t)])


def ffn_phase(C, li, tiles):
    B, nc, I, ps, P = C.B, C.nc, C.I, C.psum, C.P
    moe = (li % 2 == 1)
    j = li // 2
    use_ctx = 0 in tiles
    ln_modulate(C, which_sc=1, sh_base=24, ctx_tiles=use_ctx)
    if "xTf%d" % li in C.dbg:
        d = B.dram_out("dbg_xTf%d" % li, [128, KC, T], BF16)
        B.dma("sp", d[:, :, :], C.xT[:, :, :], r=[("xT", t) for t in range(NT)])
    tgs = TGS if use_ctx else TGS[1:]
    if True:
        sbt = C.sb
        g2bc = sbt("g2bc", [128, 2, D])
        wgu = sbt("wgu", [128, 2, 2, KC, FG], BF16)
        wdf = sbt("wdf", [128, 1, 2, D])
        wdl = sbt("wdl", [128, 2, 2, D], BF16)
        wdc = sbt("wdc", [128, 2, 2, D], BF16)
        hT = sbt("hT", [128, 2, 2, T], BF16)
        sg = sbt("sg", [128, 3, 512], BF16)
        etmp = sbt("etmp", [128, 2, 512])
        evi = [0]
        bcast_rows(C, g2bc, 40, "g2bc")
        gates = None
        if moe:
            wr = sbt("wr", [128, KC, NEXP])
            wrb = sbt("wrb", [128, KC, NEXP], BF16)
            rb = sbt("rb", [128, NEXP])
            lg = sbt("lg", [128, NT, NEXP])
            mx8 = sbt("mx8", [128, NT, 8])
            mk1 = sbt("mk1", [128, NT, NEXP])
            mk2 = sbt("mk2", [128, NT, NEXP])
            gates = sbt("gates", [128, NT, NEXP])
            w12 = sbt("w12", [128, 3, NT])
            B.dma("sp", wr[:, :, :], I["router_w"][j, :, :, :], w=["wr"])
            B.dma("sp", rb[:, :], I["router_b"][j, :, :], w=["rb"])
            B.copy("dve", wrb[:, :, :], wr[:, :, :], r=["wr"], w=["wrb"])
            bank = 6
            for t in tiles:
                B.mm(ps[:, bank, t * 8:(t + 1) * 8],
                     [(C.xT[:, kc, t * 128:(t + 1) * 128], wrb[:, kc, :]) for kc in range(KC)],
                     r=[("xT", t), "wrb"], w=[("ps", bank)])
            t0, t1 = tiles[0], tiles[-1] + 1
            n = t1 - t0
            B.tt("dve", lg[:, t0:t1, :], ps[:, bank, t0 * 8:t1 * 8].rearrange("p (t e) -> p t e", e=8),
                 rb[:, :].unsqueeze(1).to_broadcast([128, n, NEXP]), ALU.add, r=[("ps", bank), "rb"], w=["lg"])
            for t in tiles:
                P.add("dve", lambda e, t=t: e.max(out=mx8[:, t, :], in_=lg[:, t, :]), r=["lg"], w=["mx8"])
            B.tt("dve", mk1[:, t0:t1, :], lg[:, t0:t1, :], mx8[:, t0:t1, 0:1].to_broadcast([128, n, NEXP]), ALU.is_equal,
                 r=["lg", "mx8"], w=["mk1"])
            B.tt("dve", mk2[:, t0:t1, :], lg[:, t0:t1, :], mx8[:, t0:t1, 1:2].to_broadcast([128, n, NEXP]), ALU.is_equal,
                 r=["lg", "mx8"], w=["mk2"])
            B.tt("dve", w12[:, 0, t0:t1], mx8[:, t0:t1, 1], mx8[:, t0:t1, 0], ALU.subtract, r=["mx8"], w=["w12a"])
            B.act(w12[:, 0, t0:t1], w12[:, 0, t0:t1], AF.Exp, r=["w12a"], w=["w12a"])
            B.ts("dve", w12[:, 1, t0:t1], w12[:, 0, t0:t1], 1.0, ALU.add, r=["w12a"], w=["w12b"])
            P.add("dve", lambda e: e.reciprocal(out=w12[:, 1, t0:t1], in_=w12[:, 1, t0:t1]), r=["w12b"], w=["w12b"])
            B.tt("dve", w12[:, 2, t0:t1], w12[:, 0, t0:t1], w12[:, 1, t0:t1], ALU.mult, r=["w12a", "w12b"], w=["w12c"])
            B.tt("dve", mk1[:, t0:t1, :], mk1[:, t0:t1, :], w12[:, 1, t0:t1].unsqueeze(2).to_broadcast([128, n, NEXP]),
                 ALU.mult, r=["mk1", "w12b"], w=["mk1"])
            B.tt("dve", mk2[:, t0:t1, :], mk2[:, t0:t1, :], w12[:, 2, t0:t1].unsqueeze(2).to_broadcast([128, n, NEXP]),
                 ALU.mult, r=["mk2", "w12c"], w=["mk2"])
            B.tt("dve", gates[:, t0:t1, :], mk1[:, t0:t1, :], mk2[:, t0:t1, :], ALU.add, r=["mk1", "mk2"], w=["gates"])
        groups = [(e, fg) for e in range(NEXP if moe else 1) for fg in range(NFG)]
        poolA = PsumPool([0, 1, 2, 3])
        poolB = PsumPool([4, 5, 6, 7])
        sgi = [0]

        def loads_gu(i):
            e, fg = groups[i]
            s = i % 2
            src_gu = I["exp_gu"][j, e, fg] if moe else I["ffn_gu"][j, fg]
            B.dma("pool", wgu[:, s, :, :, :], src_gu.rearrange("g p k f -> p g k f"), w=[("wgu", s)])

        def loads_dn(i):
            e, fg = groups[i]
            s = i % 2
            src_dn = I["exp_dn"][j, e, fg * FG:(fg + 1) * FG, :] if moe else I["ffn_dn"][j, fg * FG:(fg + 1) * FG, :]
            B.dma("sp", wdf[:, 0, :, :], src_dn.rearrange("(c p) d -> p c d", p=128), w=["wdf"])
            B.tt("pool", wdl[:, s, :, :], wdf[:, 0, :, :], g2bc[:, 0:1, :].to_broadcast([128, 2, D]), ALU.mult,
                 r=["wdf", "g2bc"], w=[("wdl", s)])
            if use_ctx:
                B.tt("pool", wdc[:, s, :, :], wdf[:, 0, :, :], g2bc[:, 1:2, :].to_broadcast([128, 2, D]), ALU.mult,
                     r=["wdf", "g2bc"], w=[("wdc", s)])

        def phaseA(i):
            s = i % 2
            for (t0, n) in tgs:
                tl = [("xT", t) for t in range(t0 // 128, (t0 + n) // 128)]
                for fc in range(2):
                    bg, bu = poolA.next(), poolA.next()
                    B.mm(ps[:, bg, 0:n], [(wgu[:, s, 0, kc, fc * 128:(fc + 1) * 128], C.xT[:, kc, t0:t0 + n]) for kc in range(KC)],
                         r=[("wgu", s)] + tl, w=[("ps", bg)])
                    B.mm(ps[:, bu, 0:n], [(wgu[:, s, 1, kc, fc * 128:(fc + 1) * 128], C.xT[:, kc, t0:t0 + n]) for kc in range(KC)],
                         r=[("wgu", s)] + tl, w=[("ps", bu)])
                    k = sgi[0] % 3
                    sgi[0] += 1
                    B.act(sg[:, k, 0:n], ps[:, bg, 0:n], AF.Silu, r=[("ps", bg)], w=[("sg", k)])
                    B.tt("dve", hT[:, s, fc, t0:t0 + n], sg[:, k, 0:n], ps[:, bu, 0:n], ALU.mult,
                         r=[("sg", k), ("ps", bu)], w=[("hT", s, fc, t0)])

        def phaseB(i):
            e, fg = groups[i]
            s = i % 2
            for t in tiles:
                wd = wdc if t < 2 else wdl
                tg0 = [t0 for (t0, n) in TGS if t0 <= t * 128 < t0 + n][0]
                for half in range(2):
                    bo = poolB.next()
                    B.mm(ps[:, bo, :], [(hT[:, s, fc, t * 128:(t + 1) * 128], wd[:, s, fc, half * 512:(half + 1) * 512])
                                        for fc in range(2)],
                         r=[("hT", s, 0, tg0), ("hT", s, 1, tg0), ("wdc" if t < 2 else "wdl", s)], w=[("ps", bo)])
                    xs = C.x[:, t, half * 512:(half + 1) * 512]
                    sc = gates[:, t, e:e + 1] if moe else 1.0
                    if half == 0:
                        B.stt(xs, ps[:, bo, :], sc, xs, ALU.mult, ALU.add,
                              r=[("ps", bo), ("x", t)] + (["gates"] if moe else []), w=[("x", t, 0)])
                    else:
                        kq = evi[0] % 2
                        evi[0] += 1
                        B.act(etmp[:, kq, :], ps[:, bo, :], AF.Identity, scale=sc,
                              r=[("ps", bo)] + (["gates"] if moe else []), w=[("etmp", kq)])
                        B.tt("pool", xs, xs, etmp[:, kq, :], ALU.add, r=[("etmp", kq), ("x", t)], w=[("x", t, 1)])

        n = len(groups)
        loads_gu(0)
        if n > 1:
            loads_gu(1)
        loads_dn(0)
        for i in range(n):
            phaseA(i)
            if i + 2 < n:
                loads_gu(i + 2)
            if i >= 1:
                phaseB(i - 1)
            if i + 1 < n:
                loads_dn(i + 1)
        phaseB(n - 1)


CH_QA, CH_QAR, CH_KA, CH_KAR = 0, 3, 6, 8
CH_QC, CH_QCR, CH_KC, CH_KCR = 10, 12, 14, 16
CH_XBC = 18
NCH = 25
MIXERS = "ACB"
B_STAGE = 9
B_SUB = 9
TK_VA, TK_VC, TK_DT, TK_Z, NTK = 0, 128, 384, 396, 780
SR_SINK, SR_DTB, SR_ALOG, SR_DSKIP, SR_LQ, SR_LK, SR_DNW, SR_SNW, NSR = 0, 6, 18, 30, 36, 100, 164, 228, 612
PADT = T + 8


def fm_chunks(C, li, ci_list, evac):
    B, ps, I = C.B, C.psum, C.I
    ws = []
    for ci in ci_list:
        s = C.wfm_i % 4
        C.wfm_i += 1
        B.dma("pool", C.wfm[:, s, :, :], I["w_fm"][li, ci, :, :, :], w=[("wfm", s)])
        ws.append(s)
    for gi, (t0, n) in enumerate(TGS):
        tl = [("xT", t) for t in range(t0 // 128, (t0 + n) // 128)]
        banks = []
        for s in ws:
            bk = C.poolF.next()
            B.mm(ps[:, bk, 0:n], [(C.wfm[:, s, kc, :], C.xT[:, kc, t0:t0 + n]) for kc in range(KC)],
                 r=[("wfm", s)] + tl, w=[("ps", bk)])
            banks.append(bk)
        evac(gi, t0, n, banks)


def rope_evac(C, dst, dtok, cos, sin):
    B, ps = C.B, C.psum

    def evac(gi, t0, n, banks):
        bx, br = banks
        if t0 < 256:
            B.copy("act", dst[:, t0:t0 + n], ps[:, bx, 0:n], r=[("ps", bx), ("ps", br)], w=[dtok + (gi,)])
            return
        k = C.rt_i % C.rt_n
        C.rt_i += 1
        B.tt("dve", C.rtmp[:, k, 0, 0:n], ps[:, bx, 0:n], cos[:, t0:t0 + n], ALU.mult, r=[("ps", bx), "rope"], w=[("rtmp", k, 0)])
        B.tt("dve", C.rtmp[:, k, 1, 0:n], ps[:, br, 0:n], sin[:, t0:t0 + n], ALU.mult, r=[("ps", br), "rope"], w=[("rtmp", k, 1)])
        B.tt("pool", dst[:, t0:t0 + n], C.rtmp[:, k, 0, 0:n], C.rtmp[:, k, 1, 0:n], ALU.add,
             r=[("rtmp", k, 0), ("rtmp", k, 1)], w=[dtok + (gi,)])
    return evac


def tg_of(t):
    return [i for i, (t0, n) in enumerate(TGS) if t0 <= t * 128 < t0 + n][0]


def out_proj_tile(C, t, y_bf, nchunk, wo, ytok):
    B, ps = C.B, C.psum
    wch = 1 if t < 2 else 0
    bk = C.poolT.next()
    psb = ps[:, bk, :].bitcast(BF16)

    def tr(e):
        ins = None
        for c in range(nchunk):
            ins = e.transpose(out=psb[:, c * 128:(c + 1) * 128], in_=y_bf[:, c * 128:(c + 1) * 128], identity=C.identb[:, :])
        return ins
    C.P.add("pe", tr, r=[ytok, "identb"], w=[("ps", bk)])
    k = C.yT_i % 2
    C.yT_i += 1
    B.copy("act", C.yT[:, k, 0:nchunk * 128], psb[:, 0:nchunk * 128], r=[("ps", bk)], w=[("yT", k)])
    for half in range(2):
        bo = C.poolO.next()
        B.mm(ps[:, bo, :], [(C.yT[:, k, c * 128:(c + 1) * 128], wo[:, c, half * 512:(half + 1) * 512]) for c in range(nchunk)],
             r=[("yT", k), "wo"], w=[("ps", bo)])
        m = 0
        B.tt("dve", C.otmp[:, m, :], ps[:, bo, :], C.g1bc[:, wch, half * 512:(half + 1) * 512], ALU.mult,
             r=[("ps", bo), "g1bc"], w=[("otmp", m)])
        xs = C.x[:, t, half * 512:(half + 1) * 512]
        B.tt("pool", xs, xs, C.otmp[:, m, :], ALU.add, r=[("otmp", m), ("x", t)], w=[("x", t)])


def mixer_phase(C, li, tiles):
    B, nc, I, ps, P = C.B, C.nc, C.I, C.psum, C.P
    lam_init = 0.8 - 0.6 * math.exp(-0.3 * li)
    C.g1bc = C.sb("g1bc", [128, 2, D])
    C.srow = C.sb("srow", [128, NSR])
    C.yT = C.sb("yT", [128, 2, 384], BF16)
    C.otmp = C.sb("otmp", [128, 1, 512])
    C.wfm_i = C.rt_i = C.yT_i = C.ot_i = 0
    C.poolF = PsumPool([0, 1, 2, 3])
    C.poolT = PsumPool([6])
    C.poolO = PsumPool([4, 5])
    bcast_rows(C, C.g1bc, 16, "g1bc")
    B.dma("sp", C.srow[:, :], I["srow"][li, :, :], w=["srow"])
    with contextlib.ExitStack() as sc:
        sba = lambda name, shape, dt=F32: sc.enter_context(nc.sbuf_tensor("%s_a%d" % (name, li), list(shape), dt))
        if "A" in MIXERS:
            mixer_A(C, li, tiles, sba)
        P.barrier()
    with contextlib.ExitStack() as sc:
        sba = lambda name, shape, dt=F32: sc.enter_context(nc.sbuf_tensor("%s_c%d" % (name, li), list(shape), dt))
        if "C" in MIXERS:
            mixer_C(C, li, tiles, sba, lam_init)
        P.barrier()
    with contextlib.ExitStack() as sc:
        sba = lambda name, shape, dt=F32: sc.enter_context(nc.sbuf_tensor("%s_b%d" % (name, li), list(shape), dt))
        if "B" in MIXERS:
            mixer_B(C, li, tiles, sba)
        P.barrier()


def tok_proj(C, li, t, col0, ncol, wt):
    B, ps = C.B, C.psum
    bk = C.poolF.next()
    B.mm(ps[:, bk, 0:ncol], [(C.xT[:, kc, t * 128:(t + 1) * 128], wt[:, kc, col0:col0 + ncol]) for kc in range(KC)],
         r=[("xT", t), "wtok"], w=[("ps", bk)])
    return bk


def mixer_A(C, li, tiles, sba):
    B, nc, I, ps, P = C.B, C.nc, C.I, C.psum, C.P
    C.wfm = sba("wfm", [128, 4, KC, 128], BF16)
    qT = sba("qT", [128, 3, T], BF16)
    kT = sba("kT", [128, 2, T], BF16)
    vA = sba("vA", [128, NT, 2, 66], BF16)
    rope = sba("rope", [128, 2, T], BF16)
    C.rtmp = sba("rtmp", [128, 2, 2, 512])
    C.rt_n = 2
    wt = sba("wtA", [128, KC, 128], BF16)
    wo = sba("woA", [128, 3, D], BF16)
    bm = sba("bmask", [128, 3, 128], BF16)
    E1 = sba("E1", [128, 3, 384], BF16)
    E2 = sba("E2", [128, 3, 256], BF16)
    esink = sba("esink", [128, 6])
    den = sba("den", [128, 2, 6])
    ya = sba("ya", [128, 2, 384], BF16)
    qm = sba("qm", [128, 3, 128], BF16)
    bandm = sba("bandm", [128, 6])
    B.dma("sp", bandm[:, :], I["bandm"][:, :], w=["bandm"])
    B.dma("sp", rope[:, :, :], I["ropeA"].rearrange("a p t -> p a t"), w=["rope"])
    B.dma("sp", bm[:, :, :], I["bmask"][:, :, :], w=["bmask"])
    B.dma("pool", wt[:, :, :], I["w_tok"][li, :, :, TK_VA:TK_VA + 128], w=["wtok"])
    B.dma("pool", wo[:, :, :], I["w_out"][li, 0:384, :].rearrange("(c p) d -> p c d", p=128), w=["wo"])
    B.memset("pool", vA[:, :, :, 64:66], 1.0, w=["vA1"])
    B.act(esink[:, :], C.srow[:, SR_SINK:SR_SINK + 6], AF.Exp, r=["srow"], w=["esink"])
    for c in range(3):
        fm_chunks(C, li, [CH_QA + c, CH_QAR + c], rope_evac(C, qT[:, c, :], ("qT", c), rope[:, 0, :], rope[:, 1, :]))
    for g in range(2):
        fm_chunks(C, li, [CH_KA + g, CH_KAR + g], rope_evac(C, kT[:, g, :], ("kT", g), rope[:, 0, :], rope[:, 1, :]))
    for t in range(NT):
        bk = tok_proj(C, li, t, 0, 128, wt)
        B.copy("act", vA[:, t, :, 0:64], ps[:, bk, 0:128].rearrange("p (g d) -> p g d", g=2), r=[("ps", bk)], w=[("vA", t)])
    scale = 64 ** -0.5
    poolS = PsumPool([0, 1, 2, 4])
    C.poolO = PsumPool([5])
    ei = 0
    for qi, t in enumerate(tiles):
        if t < 2:
            loc = []
        else:
            loc = [(j, t - 1 + j) for j in range(3) if 2 <= t - 1 + j < NT]
        bo = 7 if qi % 2 == 0 else 3
        pend = []
        for h in range(6):
            g, b, c = h // 3, h % 2, h // 2
            rows = slice(b * 64, (b + 1) * 64)
            qtok = ("qT", c, tg_of(t))
            e = ei % 3
            ei += 1
            pv = []
            B.ts("pool", qm[:, e, :], qT[:, c, t * 128:(t + 1) * 128], bandm[:, 4 + b:5 + b], ALU.mult, r=[qtok, "bandm"], w=[("qm", e)])
            if loc:
                b1 = poolS.next()
                for (j, kt) in loc:
                    B.mm(ps[:, b1, j * 128:(j + 1) * 128], [(kT[:, g, kt * 128:(kt + 1) * 128], qm[:, e, :])],
                         r=[("kT", g, tg_of(kt)), ("qm", e)], w=[("ps", b1)])
                j0, j1 = loc[0][0], loc[-1][0] + 1
                B.act(E1[:, e, j0 * 128:j1 * 128], ps[:, b1, j0 * 128:j1 * 128], AF.Exp, scale=scale, r=[("ps", b1)], w=[("E1", e)])
                B.tt("pool", E1[:, e, j0 * 128:j1 * 128], E1[:, e, j0 * 128:j1 * 128],
                     bm[:, j0:j1, :].rearrange("p a b -> p (a b)"), ALU.mult, r=[("E1", e), "bmask"], w=[("E1", e)])
                pv += [(E1[:, e, j * 128:(j + 1) * 128], vA[:, kt, g, 0:65], ("E1", e), kt) for (j, kt) in loc]
            b2 = poolS.next()
            for kt in range(2):
                B.mm(ps[:, b2, kt * 128:(kt + 1) * 128], [(kT[:, g, kt * 128:(kt + 1) * 128], qm[:, e, :])],
                     r=[("kT", g, 0), ("qm", e)], w=[("ps", b2)])
            B.act(E2[:, e, :], ps[:, b2, 0:256], AF.Exp, scale=scale, r=[("ps", b2)], w=[("E2", e)])
            pv += [(E2[:, e, kt * 128:(kt + 1) * 128], vA[:, kt, g, 0:65], ("E2", e), kt) for kt in range(2)]
            pend.append((ps[:, bo, h * 66:h * 66 + 65], [(l, r_) for (l, r_, _, _) in pv],
                         list({x[2] for x in pv}) + [("vA", x[3]) for x in pv] + ["vA1"]))
            while len(pend) > 1:
                o_, p_, r_ = pend.pop(0)
                B.mm(o_, p_, r=r_, w=[("ps", bo)])
        while pend:
            o_, p_, r_ = pend.pop(0)
            B.mm(o_, p_, r=r_, w=[("ps", bo)])
        k = qi % 2
        pv3 = ps[:, bo, 0:396].rearrange("p (h d) -> p h d", d=66)
        B.tt("dve", den[:, k, :], pv3[:, :, 64], esink[:, :], ALU.add, r=[("ps", bo), "esink"], w=[("den", k)])
        P.add("dve", lambda e_, k=k: e_.reciprocal(out=den[:, k, :], in_=den[:, k, :]), r=[("den", k)], w=[("den", k)])
        B.tt("dve", ya[:, k, :].rearrange("p (h d) -> p h d", d=64), pv3[:, :, 0:64],
             den[:, k, :].unsqueeze(2).to_broadcast([128, 6, 64]), ALU.mult, r=[("ps", bo), ("den", k)], w=[("ya", k)])
        if "mixA%d" % li in C.dbg:
            d = dbg_out(C, "mixA%d" % li, 384)
            B.dma("sp", d[t * 128:(t + 1) * 128, :], ya[:, k, :], r=[("ya", k)])
        out_proj_tile(C, t, ya[:, k, :], 3, wo, ("ya", k))


def mixer_C(C, li, tiles, sba, lam_init):
    B, nc, I, ps, P = C.B, C.nc, C.I, C.psum, C.P
    C.poolO = PsumPool([4, 5])
    C.wfm = sba("wfm", [128, 4, KC, 128], BF16)
    qT = sba("qT", [128, 2, T], BF16)
    kT = sba("kT", [128, 2, T], BF16)
    vC = sba("vC", [128, NT, 4, 66], BF16)
    rope = sba("rope", [128, 2, T], BF16)
    C.rtmp = sba("rtmp", [128, 1, 2, 512])
    C.rt_n = 1
    wt = sba("wtC", [128, KC, 256], BF16)
    wo = sba("woC", [128, 2, D], BF16)
    E = sba("E", [128, 4, 512], BF16)
    yd = sba("yd", [128, NT, 256], BF16)
    lam = sba("lam", [128, 8])
    lqk = sba("lqk", [128, 64])
    nw = sba("nw", [128, 64])
    r12 = sba("r12", [128, 1, 2, 4])
    o12 = sba("o12", [128, 1, 2, 4, 64])
    ss = sba("ss", [128, 1, 4])
    aT = sba("aT", [128, 2, 512])
    qm = sba("qm", [128, 4, 512], BF16)
    bandm = sba("bandm", [128, 6])
    B.dma("sp", bandm[:, :], I["bandm"][:, :], w=["bandm"])
    B.dma("sp", rope[:, :, :], I["ropeC"].rearrange("a p t -> p a t"), w=["rope"])
    B.dma("pool", wt[:, :, :], I["w_tok"][li, :, :, TK_VC:TK_VC + 256], w=["wtok"])
    B.dma("pool", wo[:, :, :], I["w_out"][li, 768:1024, :].rearrange("(c p) d -> p c d", p=128), w=["wo"])
    B.memset("pool", vC[:, :, :, 64:66], 1.0, w=["vC1"])
    B.memset("pool", aT[:, :, :], 0.0, w=[("aT", 0), ("aT", 1)])
    B.tt("dve", lqk[:, :], C.srow[:, SR_LQ:SR_LQ + 64], C.srow[:, SR_LK:SR_LK + 64], ALU.mult, r=["srow"], w=["lqk"])
    P.add("dve", lambda e: e.tensor_reduce(out=lam[:, 0:2], in_=lqk[:, :].rearrange("p (a b) -> p a b", a=2), axis=AX.X, op=ALU.add),
          r=["lqk"], w=["lam"])
    B.act(lam[:, 0:2], lam[:, 0:2], AF.Exp, r=["lam"], w=["lam"])
    B.tt("dve", lam[:, 2:3], lam[:, 1:2], lam[:, 0:1], ALU.subtract, r=["lam"], w=["lam"])
    B.ts("dve", lam[:, 4:5], lam[:, 2:3], -lam_init, ALU.add, r=["lam"], w=["lam"])
    B.ts("dve", nw[:, :], C.srow[:, SR_DNW:SR_DNW + 64], 1.0 - lam_init, ALU.mult, r=["srow"], w=["nw"])
    for c in range(2):
        fm_chunks(C, li, [CH_QC + c, CH_QCR + c], rope_evac(C, qT[:, c, :], ("qT", c), rope[:, 0, :], rope[:, 1, :]))
    for c in range(2):
        fm_chunks(C, li, [CH_KC + c, CH_KCR + c], rope_evac(C, kT[:, c, :], ("kT", c), rope[:, 0, :], rope[:, 1, :]))
    for t in range(NT):
        bk = tok_proj(C, li, t, 0, 256, wt)
        B.copy("act", vC[:, t, :, 0:64], ps[:, bk, 0:256].rearrange("p (g d) -> p g d", g=4), r=[("ps", bk)], w=[("vC", t)])
    scale = 32 ** -0.5
    poolS = PsumPool([0, 1, 2])
    accs = PsumPool([3, 7, 4, 5])
    qgroups = []
    if 0 in tiles:
        qgroups.append((0, [0, 1], [0, 1]))
    for gi in range(1, 5):
        t0 = TGS[gi][0] // 128
        qgroups.append((gi, list(range(t0, t0 + 4)), list(range(NT))))
    ei = 0
    gk = 0
    ai_ = [0]
    for hc in range(4):
        cc = hc // 2
        for (gi, qts, kts) in qgroups:
            nq = len(qts)
            q0 = qts[0] * 128
            n = nq * 128
            k = 0
            gk += 1
            acc = []
            pend = []
            for i in range(2):
                j = 2 * (hc % 2) + i
                rows = slice(32 * j, 32 * j + 32)
                ab = accs.next()
                acc.append(ab)
                B.ts("pool", qm[:, j, 0:n], qT[:, cc, q0:q0 + n], bandm[:, j:j + 1], ALU.mult, r=[("qT", cc, gi), "bandm"], w=[("qm", j)])
                for ki, kt in enumerate(kts):
                    sbk = poolS.next()
                    B.mm(ps[:, sbk, 0:n], [(kT[:, cc, kt * 128:(kt + 1) * 128], qm[:, j, 0:n])],
                         r=[("kT", cc, tg_of(kt)), ("qm", j)], w=[("ps", sbk)])
                    e = ei % 4
                    ei += 1
                    B.act(E[:, e, 0:n], ps[:, sbk, 0:n], AF.Exp, scale=scale, r=[("ps", sbk)], w=[("E", e)])

                    def pv(eng, e=e, kt=kt, ab=ab, ki=ki, n=n, hc=hc, last=(ki == len(kts) - 1)):
                        return eng.matmul(ps[0:65, ab, 0:n], lhsT=vC[:, kt, hc, 0:65], rhs=E[:, e, 0:n], start=(ki == 0), stop=last)
                    pend.append((pv, [("E", e), ("vC", kt), "vC1"], [("ps", ab)]))
                    while len(pend) > 2:
                        f_, r_, w_ = pend.pop(0)
                        P.add("pe", f_, r=r_, w=w_)
            while pend:
                f_, r_, w_ = pend.pop(0)
                P.add("pe", f_, r=r_, w=w_)
            for i in range(2):
                ab = acc[i]
                m = ai_[0] % 2
                ai_[0] += 1
                B.copy("act", aT[0:65, m, 0:n], ps[0:65, ab, 0:n], r=[("ps", ab)], w=[("aT", m)])

                def trb(eng, ab=ab, m=m, nq=nq):
                    ins = None
                    for qi in range(nq):
                        ins = eng.transpose(out=ps[:, ab, qi * 66:qi * 66 + 66], in_=aT[0:66, m, qi * 128:(qi + 1) * 128],
                                            identity=C.identf[0:66, 0:66])
                    return ins
                P.add("pe", trb, r=[("aT", m), "identf"], w=[("ps", ab)])
            a1 = ps[:, acc[0], 0:nq * 66].rearrange("p (q d) -> p q d", d=66)
            a2 = ps[:, acc[1], 0:nq * 66].rearrange("p (q d) -> p q d", d=66)
            P.add("dve", lambda e_, k=k, a1=a1, nq=nq: e_.reciprocal(out=r12[:, k, 0, 0:nq], in_=a1[:, :, 64]), r=[("ps", acc[0])], w=[("r12", k)])
            P.add("dve", lambda e_, k=k, a2=a2, nq=nq: e_.reciprocal(out=r12[:, k, 1, 0:nq], in_=a2[:, :, 64]), r=[("ps", acc[1])], w=[("r12", k)])
            B.ts("dve", r12[:, k, 1, 0:nq], r12[:, k, 1, 0:nq], lam[:, 4:5], ALU.mult, r=[("r12", k), "lam"], w=[("r12", k)])
            B.tt("dve", o12[:, k, 0, 0:nq, :], a1[:, :, 0:64], r12[:, k, 0, 0:nq].unsqueeze(2).to_broadcast([128, nq, 64]), ALU.mult,
                 r=[("ps", acc[0]), ("r12", k)], w=[("o1", k)])
            B.tt("dve", o12[:, k, 1, 0:nq, :], a2[:, :, 0:64], r12[:, k, 1, 0:nq].unsqueeze(2).to_broadcast([128, nq, 64]), ALU.mult,
                 r=[("ps", acc[1]), ("r12", k)], w=[("o2", k)])
            B.tt("pool", o12[:, k, 0, 0:nq, :], o12[:, k, 0, 0:nq, :], o12[:, k, 1, 0:nq, :], ALU.add, r=[("o1", k), ("o2", k)], w=[("o1", k)])
            B.tt("pool", o12[:, k, 1, 0:nq, :], o12[:, k, 0, 0:nq, :], o12[:, k, 0, 0:nq, :], ALU.mult, r=[("o1", k)], w=[("o2", k)])
            P.add("dve", lambda e_, k=k, nq=nq: e_.tensor_reduce(out=ss[:, k, 0:nq], in_=o12[:, k, 1, 0:nq, :], axis=AX.X, op=ALU.add),
                  r=[("o2", k)], w=[("ss", k)])
            B.act(ss[:, k, 0:nq], ss[:, k, 0:nq], AF.Sqrt, scale=1.0 / 64, bias=EPS, r=[("ss", k)], w=[("ss", k)])
            P.add("dve", lambda e_, k=k, nq=nq: e_.reciprocal(out=ss[:, k, 0:nq], in_=ss[:, k, 0:nq]), r=[("ss", k)], w=[("ss", k)])
            B.tt("dve", o12[:, k, 0, 0:nq, :], o12[:, k, 0, 0:nq, :], ss[:, k, 0:nq].unsqueeze(2).to_broadcast([128, nq, 64]), ALU.mult,
                 r=[("o1", k), ("ss", k)], w=[("o1", k)])
            B.tt("pool", yd[:, qts[0]:qts[0] + nq, hc * 64:(hc + 1) * 64], o12[:, k, 0, 0:nq, :],
                 nw[:, :].unsqueeze(1).to_broadcast([128, nq, 64]), ALU.mult, r=[("o1", k), "nw"], w=[("yd", qt) for qt in qts])
    for t in tiles:
        if "mixC%d" % li in C.dbg:
            d = dbg_out(C, "mixC%d" % li, 256)
            B.dma("sp", d[t * 128:(t + 1) * 128, :], yd[:, t, :], r=[("yd", t)])
        out_proj_tile(C, t, yd[:, t, :], 2, wo, ("yd", t))


def mixer_B(C, li, tiles, sba):
    B, nc, I, ps, P = C.B, C.nc, C.I, C.psum, C.P
    C.poolO = PsumPool([4, 5])
    xsT = sba("xsT", [128, 3, T], BF16)
    BT = sba("BT", [128, 2, T], BF16)
    CT = sba("CT", [128, 2, T], BF16)
    wz = sba("wz", [128, KC, 384], BF16)
    wdt = sba("wdt", [128, KC, 12], BF16)
    wo = sba("woB", [128, 3, D], BF16)
    cst = sba("ssdc", [128, 4, 128])
    Abc = sba("Abc", [128, 12])
    hb_scr = B.dram_out("scr_hb%d" % li, [NT, 128, 384], BF16)
    B.dma("pool", wz[:, :, :], I["w_tok"][li, :, :, TK_Z:TK_Z + 384], w=["wtok"])
    B.dma("pool", wdt[:, :, :], I["w_tok"][li, :, :, TK_DT:TK_DT + 12], w=["wdt"])
    B.dma("pool", wo[:, :, :], I["w_out"][li, 384:768, :].rearrange("(c p) d -> p c d", p=128), w=["wo"])
    B.dma("sp", cst[:, :, :], I["ssdc"].rearrange("a p l -> p a l"), w=["ssdc"])
    B.act(Abc[:, :], C.srow[:, SR_ALOG:SR_ALOG + 12], AF.Exp, r=["srow"], w=["Abc"])
    B.ts("dve", Abc[:, :], Abc[:, :], -1.0, ALU.mult, r=["Abc"], w=["Abc"])
    with contextlib.ExitStack() as sc:
        sbc = lambda name, shape, dt=F32: sc.enter_context(nc.sbuf_tensor("%s_bc%d" % (name, li), list(shape), dt))
        C.wfm = sbc("wfm", [128, 4, KC, 128], BF16)
        pre = sbc("pre", [128, 2, PADT])
        acc = sbc("acc", [128, 2, 512])
        cp = sbc("convp", [128, 7, 6])
        B.dma("sp", cp[:, :, :], I["convp"][li, :, :, :], w=["convp"])
        for k in range(2):
            B.memset("pool", pre[:, k, :], 0.0, w=[("pre", k, gi) for gi in range(5)])
        dsts = [xsT[:, 0, :], xsT[:, 1, :], xsT[:, 2, :], BT[:, 0, :], BT[:, 1, :], CT[:, 0, :], CT[:, 1, :]]
        ai = 0
        for ci in range(7):
            k = ci % 2

            def evac(gi, t0, n, banks, k=k):
                off = 2 if t0 < 256 else 6
                B.copy("act", pre[:, k, off + t0:off + t0 + n], ps[:, banks[0], 0:n], r=[("ps", banks[0])], w=[("pre", k, gi)])
            fm_chunks(C, li, [CH_XBC + ci], evac)
            for gi, (t0, n) in enumerate(TGS):
                a = ai % 2
                ai += 1
                base = t0 if t0 < 256 else t0 + 4
                rd = [("pre", k, g2) for g2 in range(5)]
                B.ts("dve", acc[:, a, 0:n], pre[:, k, base:base + n], cp[:, ci, 0:1], ALU.mult, r=rd + ["convp"], w=[("acc", a)])
                for tap in range(1, 5):
                    B.stt(acc[:, a, 0:n], pre[:, k, base + tap:base + tap + n], cp[:, ci, tap:tap + 1], acc[:, a, 0:n], ALU.mult, ALU.add,
                          r=rd + ["convp", ("acc", a)], w=[("acc", a)])
                B.act(dsts[ci][:, t0:t0 + n], acc[:, a, 0:n], AF.Silu, bias=cp[:, ci, 5:6], r=[("acc", a), "convp"], w=[("u", ci, gi)])
        P.barrier()
    if B_STAGE < 1:
        return
    with contextlib.ExitStack() as sc:
        sbp = lambda name, shape, dt=F32: sc.enter_context(nc.sbuf_tensor("%s_bp%d" % (name, li), list(shape), dt))
        sc_all = sbp("sc_all", [128, 4, NT, 12])
        ect_all = sbp("ect_all", [128, NT, 12])
        AT = sbp("AT", [128, 6, 128])
        Dm = sbp("Dm", [128, 1, 3, 128])
        E = sbp("E", [128, 2, 3, 128], BF16)
        E0 = sbp("E0", [128, 2, 3, 128], BF16)
        MT = sbp("MT", [128, 1, 12, 128], BF16)
        CTp = sbp("CTp", [128, 1, 12, 128], BF16)
        xtok = sbp("xtok", [128, 2, 6, 64], BF16)
        Btok = sbp("Btok", [128, 2, 2, 128], BF16)
        Xdt = sbp("Xdt", [128, 2, 12, 64], BF16)
        Xw = sbp("Xw", [128, 2, 12, 64], BF16)
        H32 = sbp("H32", [128, 2, 6, 64])
        H16 = sbp("H16", [128, 6, 64], BF16)
        Hbin = sbp("Hbin", [128, 2, 6, 64], BF16)
        yb32 = sbp("yb32", [128, 384])
        tmp32 = sbp("tmp32", [128, 384])
        sz = tmp32
        ssq = sbp("ssq", [128, 2])
        yb16 = sbp("yb16", [128, 2, 384], BF16)
        poolP = PsumPool([0, 1, 2])
        C.poolF = PsumPool([0, 1, 2])
        st_i = [0]
        dt_a, a_a, cum_a, dtw_a = [sc_all[:, i, :, :] for i in range(4)]
        mtf = MT[:, 0, :, :].rearrange("p h l -> p (h l)").bitcast(F32)
        xb, ax, mx = [mtf[:, i * 216:(i + 1) * 216].rearrange("p (t h) -> p t h", h=12) for i in range(3)]
        fl = lambda ap: ap.rearrange("p t h -> p (t h)")
        for t in range(NT):
            B.mm(ps[:, 0, t * 12:(t + 1) * 12], [(C.xT[:, kc, t * 128:(t + 1) * 128], wdt[:, kc, :]) for kc in range(KC)],
                 r=[("xT", t), "wdt"], w=[("ps", 0)])
        B.tt("dve", xb, ps[:, 0, 0:NT * 12].rearrange("p (t h) -> p t h", h=12),
             C.srow[:, SR_DTB:SR_DTB + 12].unsqueeze(1).to_broadcast([128, NT, 12]), ALU.add, r=[("ps", 0), "srow"], w=["sc0"])
        B.act(fl(ax), fl(xb), AF.Abs, r=["sc0"], w=["sc1"])
        B.act(fl(ax), fl(ax), AF.Exp, scale=-1.0, r=["sc1"], w=["sc1"])
        B.act(fl(ax), fl(ax), AF.Ln, bias=1.0, r=["sc1"], w=["sc1"])
        B.ts("dve", fl(mx), fl(xb), 0.0, ALU.max, r=["sc0"], w=["sc2"])
        B.tt("dve", fl(dt_a), fl(mx), fl(ax), ALU.add, r=["sc1", "sc2"], w=["dt_a"])
        B.tt("dve", a_a, dt_a, Abc[:, :].unsqueeze(1).to_broadcast([128, NT, 12]), ALU.mult, r=["dt_a", "Abc"], w=["a_a"])
        for t in range(NT):
            B.mm(ps[:, 1, t * 12:t * 12 + 6], [(cst[:, 0, :], a_a[:, t, 0:6])], r=["ssdc", "a_a"], w=[("ps", 1)])
            B.mm(ps[:, 1, t * 12 + 6:t * 12 + 12], [(cst[:, 1, :], a_a[:, t, 6:12])], r=["ssdc", "a_a"], w=[("ps", 1)])
            B.mm(ps[:, 2, t * 12:(t + 1) * 12], [(C.onesf[:, :], a_a[:, t, :])], r=["onesf", "a_a"], w=[("ps", 2)])
        B.copy("act", fl(cum_a), ps[:, 1, 0:NT * 12], r=[("ps", 1)], w=["cum_a"])
        B.copy("act", fl(ect_all[:, :, :]), ps[:, 2, 0:NT * 12], r=[("ps", 2)], w=["ect"])
        B.tt("dve", fl(dtw_a), fl(ect_all[:, :, :]), fl(cum_a), ALU.subtract, r=["ect", "cum_a"], w=["dtw_a"])
        B.act(fl(dtw_a), fl(dtw_a), AF.Exp, r=["dtw_a"], w=["dtw_a"])
        B.tt("dve", fl(dtw_a), fl(dtw_a), fl(dt_a), ALU.mult, r=["dtw_a", "dt_a"], w=["dtw_a"])
        B.act(fl(ect_all[:, :, :]), fl(ect_all[:, :, :]), AF.Exp, r=["ect", "dtw_a"], w=["ect"])
        P.barrier()

        def prep(t, full):
            k = st_i[0] % 2
            st_i[0] += 1
            tgt = tg_of(t)
            av = a_a[:, t, :]
            bk = poolP.next()
            psb = ps[:, bk, :].bitcast(BF16)

            def tr(e, t=t, psb=psb):
                ins = None
                for c in range(3):
                    ins = e.transpose(out=psb[:, c * 128:(c + 1) * 128], in_=xsT[:, c, t * 128:(t + 1) * 128], identity=C.identb[:, :])
                for g in range(2):
                    ins = e.transpose(out=psb[:, 384 + g * 128:384 + (g + 1) * 128], in_=BT[:, g, t * 128:(t + 1) * 128], identity=C.identb[:, :])
                return ins
            P.add("pe", tr, r=[("u", ci, tgt) for ci in range(5)] + ["identb"], w=[("ps", bk)])
            B.copy("act", xtok[:, k, :, :].rearrange("p h d -> p (h d)"), psb[:, 0:384], r=[("ps", bk)], w=[("xtok", k)])
            B.copy("act", Btok[:, k, :, :].rearrange("p g n -> p (g n)"), psb[:, 384:640], r=[("ps", bk)], w=[("Btok", k)])
            dirs = (0, 1) if full else (1,)
            for d in dirs:
                B.tt("pool", Xw[:, k, d * 6:(d + 1) * 6, :], xtok[:, k, :, :],
                     dtw_a[:, t, d * 6:(d + 1) * 6].unsqueeze(2).to_broadcast([128, 6, 64]),
                     ALU.mult, r=[("xtok", k), "dtw_a"], w=[("Xw", k, d)])
            if not full:
                return k
            for d in range(2):
                B.tt("pool", Xdt[:, k, d * 6:(d + 1) * 6, :], xtok[:, k, :, :],
                     dt_a[:, t, d * 6:(d + 1) * 6].unsqueeze(2).to_broadcast([128, 6, 64]),
                     ALU.mult, r=[("xtok", k), "dt_a"], w=[("Xdt", k, d)])
            return k

        def prep2(t):
            tgt = tg_of(t)
            av = a_a[:, t, :]
            bg = 3
            for g in range(2):
                B.mm(ps[:, bg, g * 128:(g + 1) * 128], [(BT[:, g, t * 128:(t + 1) * 128], CT[:, g, t * 128:(t + 1) * 128])],
                     r=[("u", 3 + g, tgt), ("u", 5 + g, tgt)], w=[("ps", bg)])
            for d in range(2):
                B.tt("pool", AT[:, :, :], cst[:, d, :].unsqueeze(1).to_broadcast([128, 6, 128]),
                     av[:, d * 6:(d + 1) * 6].unsqueeze(2).to_broadcast([128, 6, 128]), ALU.mult, r=["ssdc", "a_a"], w=["AT"])
                for g in range(2):
                    hd0 = d * 6 + g * 3
                    bc = poolP.next()
                    B.mm(ps[:, bc, 0:384], [(C.onesf[:, :], AT[:, g * 3:(g + 1) * 3, :].rearrange("p h l -> p (h l)"))],
                         r=["onesf", "AT"], w=[("ps", bc)])
                    cr = ps[:, bc, 0:384].rearrange("p (h l) -> p h l", h=3)
                    m = 0
                    for i3 in range(3):
                        B.stt(Dm[:, m, i3, :], cr[:, i3, :], cum_a[:, t, hd0 + i3:hd0 + i3 + 1], cst[:, 2 + d, :], ALU.subtract, ALU.add,
                              r=[("ps", bc), "cum_a", "ssdc"], w=[("Dm", m)])
                    B.act(E[:, g, :, :], Dm[:, m, :, :], AF.Exp, r=[("Dm", m)], w=[("E", g)])
                    B.act(E0[:, g, :, :], cr, AF.Exp, r=[("ps", bc)], w=[("E0", g)])
                    for i3 in range(3):
                        B.tt("dve", MT[:, 0, hd0 + i3, :], E[:, g, i3, :], ps[:, bg, g * 128:(g + 1) * 128], ALU.mult,
                             r=[("E", g), ("ps", bg)], w=[("MT", 0, hd0)])
                    B.tt("pool", CTp[:, 0, hd0:hd0 + 3, :], E0[:, g, :, :],
                         CT[:, g, t * 128:(t + 1) * 128].unsqueeze(1).to_broadcast([128, 3, 128]), ALU.mult,
                         r=[("E0", g), ("u", 5 + g, tgt)], w=[("CTp", 0, hd0)])

        def state_update(d, k, t):
            bh = poolP.next()
            for h in range(6):
                B.mm(ps[:, bh, h * 64:(h + 1) * 64], [(Btok[:, k, h // 3, :], Xw[:, k, d * 6 + h, :])],
                     r=[("Btok", k), ("Xw", k, d)], w=[("ps", bh)])
            B.tt("pool", H32[:, d, :, :], H32[:, d, :, :], ect_all[:, t, d * 6:(d + 1) * 6].unsqueeze(2).to_broadcast([128, 6, 64]), ALU.mult,
                 r=[("H32", d), "ect"], w=[("H32", d)])
            B.tt("dve", H32[:, d, :, :], H32[:, d, :, :], ps[:, bh, 0:384].rearrange("p (h d) -> p h d", h=6), ALU.add,
                 r=[("H32", d), ("ps", bh)], w=[("H32", d)])

        B.memset("pool", H32[:, :, :, :], 0.0, w=[("H32", 0), ("H32", 1)])
        border = [1, 0] + list(range(NT - 1, 1, -1))
        kn = prep(border[0], False)
        for i, t in enumerate(border):
            k2 = t % 2
            B.copy("act", Hbin[:, k2, :, :], H32[:, 1, :, :], r=[("H32", 1)], w=[("Hbin", k2)])
            B.dma("sp", hb_scr[t, :, :], Hbin[:, k2, :, :].rearrange("p h d -> p (h d)"), r=[("Hbin", k2)], w=[("hbscr", t)])
            if t == 2:
                break
            k = kn
            if border[i + 1] != 2:
                kn = prep(border[i + 1], False)
            state_update(1, k, t)
        kn = prep(0, True)
        prep2(0)
        for t in range(NT):
            need_y = t in tiles
            k = kn
            if t + 1 < NT:
                kn = prep(t + 1, True)
            if need_y:
                k2 = t % 2
                B.dma("sp", Hbin[:, k2, :, :].rearrange("p h d -> p (h d)"), hb_scr[t, :, :], r=[("hbscr", t)], w=[("Hbin", k2)])
                B.copy("act", H16[:, :, :], H32[:, 0, :, :], r=[("H32", 0)], w=["H16"])
                by = 7
                for h in range(6):
                    B.mm(ps[:, by, h * 64:(h + 1) * 64],
                         [(MT[:, 0, h, :], Xdt[:, k, h, :]), (CTp[:, 0, h, :], H16[:, h, :]),
                          (MT[:, 0, 6 + h, :], Xdt[:, k, 6 + h, :]), (CTp[:, 0, 6 + h, :], Hbin[:, k2, h, :])],
                         r=[("MT", 0, (h // 3) * 3), ("MT", 0, 6 + (h // 3) * 3), ("CTp", 0, (h // 3) * 3), ("CTp", 0, 6 + (h // 3) * 3),
                            ("Xdt", k, 0), ("Xdt", k, 1), "H16", ("Hbin", k2)], w=[("ps", by)])
            if t + 1 < NT:
                prep2(t + 1)
            if need_y:
                B.tt("pool", tmp32[:, :].rearrange("p (h d) -> p h d", h=6), xtok[:, k, :, :],
                     C.srow[:, SR_DSKIP:SR_DSKIP + 6].unsqueeze(2).to_broadcast([128, 6, 64]), ALU.mult, r=[("xtok", k), "srow"], w=["tmp32"])
                B.tt("dve", yb32[:, :], ps[:, by, 0:384], tmp32[:, :], ALU.add, r=[("ps", by), "tmp32"], w=["yb32"])
                bz = tok_proj(C, li, t, 0, 384, wz)
                B.act(sz[:, :], ps[:, bz, 0:384], AF.Silu, r=[("ps", bz)], w=["tmp32"])
                B.tt("pool", yb32[:, :], yb32[:, :], sz[:, :], ALU.mult, r=["yb32", "tmp32"], w=["yb32"])
                B.act(tmp32[:, :], yb32[:, :], AF.Square, accum_out=ssq[:, 0:1], r=["yb32"], w=["tmp32", "ssq"])
                B.act(ssq[:, 1:2], ssq[:, 0:1], AF.Sqrt, scale=1.0 / 384, bias=EPS, r=["ssq"], w=["ssq"])
                P.add("dve", lambda e_: e_.reciprocal(out=ssq[:, 1:2], in_=ssq[:, 1:2]), r=["ssq"], w=["ssq"])
                B.stt(yb16[:, k2, :], yb32[:, :], ssq[:, 1:2], C.srow[:, SR_SNW:SR_SNW + 384], ALU.mult, ALU.mult,
                      r=["yb32", "ssq", "srow"], w=[("yb16", k2)])
                if "mixB%d" % li in C.dbg:
                    d_ = dbg_out(C, "mixB%d" % li, 384)
                    B.dma("sp", d_[t * 128:(t + 1) * 128, :], yb16[:, k2, :], r=[("yb16", k2)])
                out_proj_tile(C, t, yb16[:, k2, :], 3, wo, ("yb16", k2))
            if t < NT - 1:
                state_update(0, k, t)
        P.barrier()


_NC_CACHE = {}


def kernel(**inputs):
    inp = {k: np.asarray(v) for k, v in inputs.items()}
    if "nc" not in _NC_CACHE:
        nc = bass.Bass("TRN2", target_bir_lowering=False)
        build_program(nc, nlayers=DEPTH)
        _NC_CACHE["nc"] = nc
    nc = _NC_CACHE["nc"]
    shared = _shared_inputs(inp)
    in_maps = [host_inputs(inp, b, shared) for b in range(8)]
    res = run_bass_kernel_spmd(nc, in_maps, core_ids=list(range(8)))
    out = np.stack([np.asarray(r["out"], dtype=np.float32) for r in res.results], axis=0)
    return out
```

```python
import contextlib
import math
import numpy as np
import ml_dtypes
import concourse.bass as bass
import concourse.mybir as mybir
from concourse.bass_utils import run_bass_kernel_spmd

F32, BF16 = mybir.dt.float32, mybir.dt.bfloat16
AF = mybir.ActivationFunctionType
ALU = mybir.AluOpType
AX = mybir.AxisListType

D = 1024
NT = 18
T = NT * 128
KC = 8
DEPTH = 4
DFF = 2816
FG = 256
NFG = DFF // FG
NEXP = 8
ALPHA = (2 * DEPTH) ** 0.25
EPS = 1e-6
TGS = [(0, 256), (256, 512), (768, 512), (1280, 512), (1792, 512)]

O_QA, O_KA, O_VA, O_Z, O_XBC, O_DTF, O_DTB, O_QC, O_KC, O_VC = 0, 384, 512, 640, 1024, 1920, 1926, 1932, 2188, 2444


class Op:
    __slots__ = ("idx", "eng", "fn", "dma", "deps", "sig", "cnt", "dsem", "dval", "dprev")

    def __init__(self, idx, eng, fn, dma):
        self.idx, self.eng, self.fn, self.dma = idx, eng, fn, dma
        self.deps = set()
        self.sig = False
        self.cnt = 0
        self.dsem = None
        self.dval = 0
        self.dprev = 0


class Prog:
    ENGS = ("pe", "act", "dve", "pool", "sp")
    NDS = 20
    SEM_CAP = 30000

    def __init__(self, nc):
        self.nc = nc
        self.ops = []
        self.lastw = {}
        self.rd = {}
        self.bar_from = 0

    def add(self, eng, fn, r=(), w=(), dma=False):
        op = Op(len(self.ops), eng, fn, dma)
        deps = set()
        for t in r:
            lw = self.lastw.get(t)
            if lw is not None:
                deps.add(lw)
        for t in w:
            lw = self.lastw.get(t)
            if lw is not None:
                deps.add(lw)
            for o in self.rd.get(t, ()):
                deps.add(o)
        deps.discard(op)
        op.deps = deps
        for t in r:
            self.rd.setdefault(t, set()).add(op)
        for t in w:
            self.lastw[t] = op
            self.rd[t] = set()
        self.ops.append(op)
        return op

    def barrier(self):
        last = {}
        dmas = []
        for op in self.ops[self.bar_from:]:
            if op.dma:
                dmas.append(op)
            elif op.fn is not None:
                last[op.eng] = op
        self.bar_from = len(self.ops)
        for e in self.ENGS:
            op = Op(len(self.ops), e, None, False)
            op.deps = set(last.values()) | set(dmas)
            self.ops.append(op)

    def emit(self):
        nc = self.nc
        ops = self.ops
        for op in ops:
            for d in op.deps:
                if d.dma:
                    continue
                if d.eng == op.eng and op.eng in ("pe", "sp") and not op.dma and op.fn is not None:
                    continue
                d.sig = True
        cnt = {e: 0 for e in self.ENGS}
        for op in ops:
            if op.sig and not op.dma:
                cnt[op.eng] += 1
                op.cnt = cnt[op.eng]
        dcount = {"sp": 0, "pool": 0}
        duse = {}
        for op in ops:
            if op.dma:
                k = dcount[op.eng] % self.NDS
                dcount[op.eng] += 1
                key = (op.eng, k)
                op.dsem = key
                op.dprev = duse.get(key, 0)
                op.dval = op.dprev + 16
                duse[key] = op.dval
        with contextlib.ExitStack() as st:
            esems = {}
            for e in self.ENGS:
                n = cnt[e] // self.SEM_CAP + 1
                esems[e] = [st.enter_context(nc.semaphore(f"se_{e}_{i}")) for i in range(n)]
            dsems = {}
            for q in ("sp", "pool"):
                for k in range(min(self.NDS, dcount[q])):
                    dsems[(q, k)] = st.enter_context(nc.semaphore(f"sd_{q}_{k}"))
            block = st.enter_context(nc.Block())
            cap = self.SEM_CAP

            def run(engname, eh):
                waited = {}

                def wait(sem_key, sem, val):
                    if waited.get(sem_key, 0) >= val:
                        return
                    waited[sem_key] = val
                    eh.wait_ge(sem, val)

                for op in ops:
                    if op.eng != engname:
                        continue
                    for d in sorted(op.deps, key=lambda o: o.idx):
                        if d.dma:
                            wait(("d",) + d.dsem, dsems[d.dsem], d.dval)
                        else:
                            if d.eng == engname and engname in ("pe", "sp") and not op.dma and op.fn is not None:
                                continue
                            si, sv = divmod(d.cnt - 1, cap)
                            wait(("e", d.eng, si), esems[d.eng][si], sv + 1)
                    if op.fn is None:
                        continue
                    if op.dma:
                        if op.dprev > 0:
                            wait(("d",) + op.dsem, dsems[op.dsem], op.dprev)
                        ins = op.fn(eh)
                        ins.then_inc(dsems[op.dsem], 16)
                    else:
                        ins = op.fn(eh)
                        if op.sig:
                            si, sv = divmod(op.cnt - 1, cap)
                            ins.then_inc(esems[engname][si], 1)
                if engname == "sp":
                    for key, v in duse.items():
                        wait(("d",) + key, dsems[key], v)

            @block.tensor
            def _(t):
                run("pe", t)

            @block.scalar
            def _(s):
                run("act", s)

            @block.vector
            def _(v):
                run("dve", v)

            @block.gpsimd
            def _(g):
                run("pool", g)

            @block.sync
            def _(s):
                run("sp", s)


class PsumPool:
    def __init__(self, banks):
        self.banks = list(banks)
        self.i = 0

    def next(self):
        b = self.banks[self.i % len(self.banks)]
        self.i += 1
        return b


class Builder:
    def __init__(self, nc, dbg=None, nlayers=DEPTH):
        self.nc = nc
        self.P = Prog(nc)
        self.dbg = dbg or {}
        self.nlayers = nlayers
        self.uid = 0

    def tok(self, name):
        self.uid += 1
        return (name, self.uid)

    def dram_in(self, name, shape, dt=F32):
        return self.nc.dram_tensor(name, list(shape), dt, kind="ExternalInput").ap()

    def dram_out(self, name, shape, dt=F32):
        return self.nc.dram_tensor(name, list(shape), dt, kind="ExternalOutput").ap()

    def dma(self, q, out, in_, r=(), w=()):
        return self.P.add(q, lambda e: e.dma_start(out=out, in_=in_), r, w, dma=True)

    def mm(self, out, pairs, r=(), w=(), tile_position=None, start=True):
        def fn(e):
            n = len(pairs)
            ins = None
            for i, (l, rh) in enumerate(pairs):
                kw = {}
                if tile_position is not None:
                    kw["tile_position"] = tile_position
                ins = e.matmul(out, lhsT=l, rhs=rh, start=(start and i == 0), stop=(i == n - 1), **kw)
            return ins
        return self.P.add("pe", fn, r, w)

    def act(self, out, in_, func, r=(), w=(), bias=None, scale=None, accum_out=None):
        def fn(e):
            kw = {}
            if bias is not None:
                kw["bias"] = bias
            if scale is not None:
                kw["scale"] = scale
            if accum_out is not None:
                kw["accum_out"] = accum_out
            return e.activation(out=out, in_=in_, func=func, **kw)
        return self.P.add("act", fn, r, w)

    def tt(self, eng, out, in0, in1, op, r=(), w=()):
        return self.P.add(eng, lambda e: e.tensor_tensor(out=out, in0=in0, in1=in1, op=op), r, w)

    def ts(self, eng, out, in0, s1, op0, s2=None, op1=None, r=(), w=()):
        def fn(e):
            if op1 is None:
                return e.tensor_scalar(out=out, in0=in0, scalar1=s1, scalar2=None, op0=op0)
            return e.tensor_scalar(out=out, in0=in0, scalar1=s1, scalar2=s2, op0=op0, op1=op1)
        return self.P.add(eng, fn, r, w)

    def stt(self, out, in0, scalar, in1, op0, op1, r=(), w=()):
        return self.P.add("dve", lambda e: e.scalar_tensor_tensor(out=out, in0=in0, scalar=scalar, in1=in1,
                                                                   op0=op0, op1=op1), r, w)

    def copy(self, eng, out, in_, r=(), w=()):
        if eng == "act":
            return self.P.add("act", lambda e: e.copy(out=out, in_=in_), r, w)
        return self.P.add(eng, lambda e: e.tensor_copy(out=out, in_=in_), r, w)

    def memset(self, eng, ap, val, w=()):
        return self.P.add(eng, lambda e: e.memset(ap, val), (), w)


class Ctx:
    pass


def declare_inputs(B):
    I = {}
    I["xin"] = B.dram_in("xin", [T, D])
    I["cvec"] = B.dram_in("cvec", [128, KC, 2])
    I["ident"] = B.dram_in("ident", [128, 128])
    I["w_ada"] = B.dram_in("w_ada", [DEPTH, D, 6 * D])
    I["b_adaT"] = B.dram_in("b_adaT", [DEPTH, 128, 48])
    I["lnrows"] = B.dram_in("lnrows", [DEPTH, 4, 128, D])
    I["ffn_gu"] = B.dram_in("ffn_gu", [2, NFG, 2, 128, KC, FG])
    I["ffn_dn"] = B.dram_in("ffn_dn", [2, DFF, D])
    I["exp_gu"] = B.dram_in("exp_gu", [2, NEXP, NFG, 2, 128, KC, FG])
    I["exp_dn"] = B.dram_in("exp_dn", [2, NEXP, DFF, D])
    I["router_w"] = B.dram_in("router_w", [2, 128, KC, NEXP])
    I["router_b"] = B.dram_in("router_b", [2, 128, NEXP])
    I["w_fm"] = B.dram_in("w_fm", [DEPTH, NCH, 128, KC, 128])
    I["w_tok"] = B.dram_in("w_tok", [DEPTH, 128, KC, NTK])
    I["w_out"] = B.dram_in("w_out", [DEPTH, D, D])
    I["srow"] = B.dram_in("srow", [DEPTH, 128, NSR])
    I["convp"] = B.dram_in("convp", [DEPTH, 128, 7, 6])
    I["ropeA"] = B.dram_in("ropeA", [2, 128, T], BF16)
    I["ropeC"] = B.dram_in("ropeC", [2, 128, T], BF16)
    I["bmask"] = B.dram_in("bmask", [128, 3, 128], BF16)
    I["ssdc"] = B.dram_in("ssdc", [4, 128, 128])
    I["bandm"] = B.dram_in("bandm", [128, 6])
    return I


def build_program(nc, nlayers=DEPTH, dbg=(), inject_x1=False):
    B = Builder(nc)
    P = B.P
    I = declare_inputs(B)
    out_d = B.dram_out("out", [2048, D])
    dbg_d = {}
    st = contextlib.ExitStack()
    sb = lambda name, shape, dt=F32: st.enter_context(nc.sbuf_tensor(name, list(shape), dt))
    C = Ctx()
    C.B, C.P, C.I, C.nc = B, P, I, nc
    C.x = sb("x", [128, NT, D])
    C.xT = sb("xT", [128, KC, T], BF16)
    C.identf = sb("identf", [128, 128])
    C.identb = sb("identb", [128, 128], BF16)
    C.onesf = sb("onesf", [128, 128])
    C.cact = sb("cact", [128, KC, 2])
    C.adaT = sb("adaT", [128, 48, 2])
    C.scp = sb("scp", [128, 2, KC, 2])
    C.mv = sb("mv", [128, NT, 2])
    C.rstd = sb("rstd", [128, NT])
    C.bnst = sb("bnst", [128, NT, 2, 6])
    C.psum = st.enter_context(nc.psum_tensor("psum", [128, 8, 512], F32))
    ps = C.psum

    xin_v = I["xin"].rearrange("(t p) d -> p t d", p=128)
    for t in range(NT):
        B.dma("sp", C.x[:, t, :], xin_v[:, t, :], w=[("x", t)])
    B.dma("sp", C.identf[:, :], I["ident"][:, :], w=["identf"])
    B.dma("sp", C.cact[:, :, :], I["cvec"][:, :, :], w=["cact"])
    B.copy("dve", C.identb[:, :], C.identf[:, :], r=["identf"], w=["identb"])
    B.memset("pool", C.onesf[:, :], 1.0, w=["onesf"])
    B.act(C.cact[:, :, :], C.cact[:, :, :], AF.Silu, r=["cact"], w=["cact"])
    for t in range(NT):
        B.ts("pool", C.x[:, t, :], C.x[:, t, :], ALPHA, ALU.mult, r=[("x", t)], w=[("x", t)])

    C.nlayers = nlayers
    C.dbg = dbg
    C.dbgd = {}
    for li in range(nlayers):
        last = (li == DEPTH - 1)
        tiles = list(range(2, NT)) if last else list(range(NT))
        mk = lambda sc, tag, li=li: (lambda name, shape, dt=F32: sc.enter_context(nc.sbuf_tensor("%s_%s%d" % (name, tag, li), list(shape), dt)))
        with contextlib.ExitStack() as sc:
            C.sb = mk(sc, "m")
            C.dg = C.sb("dg", [128, 2, 128])
            with contextlib.ExitStack() as sc2:
                C.sb2 = mk(sc2, "m0")
                C.xn = C.sb2("xn", [128, 2, D], BF16)
                ada_params(C, li)
                ln_modulate(C, which_sc=0, sh_base=0)
                P.barrier()
            if "xT%d" % li in dbg:
                d = B.dram_out("dbg_xT%d" % li, [128, KC, T], BF16)
                B.dma("sp", d[:, :, :], C.xT[:, :, :], r=[("xT", t) for t in range(NT)])
            if inject_x1:
                d = B.dram_in("dbg_x1_%d" % li, [T, D])
                dv = d.rearrange("(t p) d -> p t d", p=128)
                for t in range(NT):
                    B.dma("sp", C.x[:, t, :], dv[:, t, :], w=[("x", t)])
            else:
                mixer_phase(C, li, tiles)
                with contextlib.ExitStack() as sc2:
                    C.rows = mk(sc2, "m9")("lnrows", [128, 2, D])
                    post_ln(C, li, 0, tiles, out_scale=ALPHA)
                    P.barrier()
            if "x1_%d" % li in dbg:
                dump_x(C, "dbg_x1_%d" % li)
            P.barrier()
        with contextlib.ExitStack() as sc:
            C.sb = lambda name, shape, dt=F32, li=li, sc=sc: sc.enter_context(nc.sbuf_tensor("%s_f%d" % (name, li), list(shape), dt))
            C.xn = C.sb("xn", [128, 2, D], BF16)
            C.rows = C.sb("lnrows", [128, 2, D])
            C.dg = C.sb("dg", [128, 2, 128])
            ffn_phase(C, li, tiles)
            if "xpre%d" % li in dbg:
                dump_x(C, "dbg_xpre%d" % li)
            post_ln(C, li, 1, tiles, out_scale=(1.0 if li == nlayers - 1 else ALPHA))
            if "x2_%d" % li in dbg:
                dump_x(C, "dbg_x2_%d" % li)
            P.barrier()
    ov = out_d.rearrange("(t p) d -> p t d", p=128)
    for t in range(2, NT):
        B.dma("sp", ov[:, t - 2, :], C.x[:, t, :], r=[("x", t)])
    P.emit()
    st.close()
    return nc


def dbg_out(C, name, w):
    if name not in C.dbgd:
        C.dbgd[name] = C.B.dram_out("dbg_" + name, [T, w], BF16)
    return C.dbgd[name]


def dump_x(C, name):
    d = C.B.dram_out(name, [T, D])
    dv = d.rearrange("(t p) d -> p t d", p=128)
    for t in range(NT):
        C.B.dma("sp", dv[:, t, :], C.x[:, t, :], r=[("x", t)])


def ada_params(C, li):
    B, nc, I, ps = C.B, C.nc, C.I, C.psum
    wb = C.sb2("adaw", [128, 2, KC, 256])
    bT = C.sb2("adab", [128, 48])
    B.dma("sp", bT[:, :], I["b_adaT"][li, :, :], w=["adab"])
    wv = I["w_ada"][li].rearrange("(kc p) f -> p kc f", p=128)
    bank = 7
    for piece in range(24):
        s = piece % 2
        B.dma("sp", wb[:, s, :, :], wv[:, :, piece * 256:(piece + 1) * 256], w=[("adaw", s)])
        for j in range(2):
            fc = piece * 2 + j
            B.mm(ps[:, bank, fc * 2:fc * 2 + 2],
                 [(wb[:, s, kc, j * 128:(j + 1) * 128], C.cact[:, kc, :]) for kc in range(KC)],
                 r=[("adaw", s), "cact"], w=[("ps", bank)])
    B.tt("dve", C.adaT[:, :, :], ps[:, bank, 0:96].rearrange("p (f w) -> p f w", w=2),
         bT[:, :].unsqueeze(2).to_broadcast([128, 48, 2]), ALU.add,
         r=[("ps", bank), "adab"], w=["adaT"])
    B.ts("pool", C.scp[:, 0, :, :], C.adaT[:, 8:16, :], 1.0, ALU.add, r=["adaT"], w=["scp"])
    B.ts("pool", C.scp[:, 1, :, :], C.adaT[:, 32:40, :], 1.0, ALU.add, r=["adaT"], w=["scp"])


def ln_stats(C, tiles, eps=EPS):
    B = C.B
    for t in tiles:
        for h in range(2):
            C.P.add("dve", lambda e, t=t, h=h: e.bn_stats(out=C.bnst[:, t, h, :], in_=C.x[:, t, h * 512:(h + 1) * 512]),
                    r=[("x", t), ("x", t, h)], w=[("bnst", t)])
        C.P.add("dve", lambda e, t=t: e.bn_aggr(out=C.mv[:, t, :], in_=C.bnst[:, t, :, :].rearrange("p a b -> p (a b)")),
                r=[("bnst", t)], w=[("mv", t)])
    t0, t1 = tiles[0], tiles[-1] + 1
    B.act(C.rstd[:, t0:t1], C.mv[:, t0:t1, 1], AF.Sqrt, bias=eps, r=[("mv", t) for t in tiles], w=[("rstd", t) for t in tiles])
    C.P.add("dve", lambda e: e.reciprocal(out=C.rstd[:, t0:t1], in_=C.rstd[:, t0:t1]),
            r=[("rstd", t) for t in tiles], w=[("rstd", t) for t in tiles])


def ln_modulate(C, which_sc, sh_base, ctx_tiles=True):
    B, nc, ps = C.B, C.nc, C.psum
    tiles = list(range(NT)) if ctx_tiles else list(range(2, NT))
    ln_stats(C, tiles, eps=EPS * ALPHA * ALPHA)
    if True:
        xn = C.xn
        pool = PsumPool([0, 1])
        for i, t in enumerate(tiles):
            s = i % 2
            wch = 1 if t < 2 else 0
            B.ts("dve", xn[:, s, :], C.x[:, t, :], C.mv[:, t, 0:1], ALU.subtract, C.rstd[:, t:t + 1], ALU.mult,
                 r=[("x", t), ("mv", t), ("rstd", t)], w=[("xn", s)])
            bk = pool.next()
            psb = ps[:, bk, :].bitcast(BF16)

            def tr(e, s=s, psb=psb):
                ins = None
                for kc in range(KC):
                    ins = e.transpose(out=psb[:, kc * 128:(kc + 1) * 128], in_=xn[:, s, kc * 128:(kc + 1) * 128],
                                      identity=C.identb[:, :])
                return ins
            C.P.add("pe", tr, r=[("xn", s), "identb"], w=[("ps", bk)])
            for kc in range(KC):
                B.act(C.xT[:, kc, t * 128:(t + 1) * 128], psb[:, kc * 128:(kc + 1) * 128], AF.Identity,
                      scale=C.scp[:, which_sc, kc, wch:wch + 1], bias=C.adaT[:, sh_base + kc, wch:wch + 1],
                      r=[("ps", bk), "scp", "adaT"], w=[("xT", t)])


def _shared_inputs(inp):
    f32 = np.float32
    S = {}
    S["ident"] = np.eye(128, dtype=f32)
    S["w_ada"] = np.ascontiguousarray(inp["w_ada"], dtype=f32)
    S["b_adaT"] = np.ascontiguousarray(inp["b_ada"].reshape(DEPTH, 48, 128).transpose(0, 2, 1), dtype=f32)
    ln = np.stack([inp["ln1_g"], inp["ln1_b"], inp["ln2_g"], inp["ln2_b"]], axis=1)
    S["lnrows"] = np.ascontiguousarray(np.broadcast_to(ln[:, :, None, :], (DEPTH, 4, 128, D)), dtype=f32)
    g = inp["ffn_w_gu"].reshape(2, KC, 128, 2, NFG, FG)
    S["ffn_gu"] = np.ascontiguousarray(g.transpose(0, 4, 3, 2, 1, 5), dtype=f32)
    S["ffn_dn"] = np.ascontiguousarray(inp["ffn_w_down"], dtype=f32)
    g = inp["exp_w_gu"].reshape(2, NEXP, KC, 128, 2, NFG, FG)
    S["exp_gu"] = np.ascontiguousarray(g.transpose(0, 1, 5, 4, 3, 2, 6), dtype=f32)
    S["exp_dn"] = np.ascontiguousarray(inp["exp_w_down"], dtype=f32)
    S["router_w"] = np.ascontiguousarray(inp["router_w"].reshape(2, KC, 128, NEXP).transpose(0, 2, 1, 3), dtype=f32)
    S["router_b"] = np.ascontiguousarray(np.broadcast_to(inp["router_b"][:, None, :], (2, 128, NEXP)), dtype=f32)
    i128 = np.arange(128)

    def rot(i, hd):
        d = i % hd
        return (i // hd) * hd + np.where(d < hd // 2, d + hd // 2, d - hd // 2)
    cols = []
    for c in range(3):
        cols.append(O_QA + c * 128 + i128)
    for c in range(3):
        cols.append(O_QA + c * 128 + rot(i128, 64))
    for g in range(2):
        cols.append(O_KA + g * 64 + (i128 % 64))
    for g in range(2):
        cols.append(O_KA + g * 64 + rot(i128 % 64, 64))
    for base in (O_QC, O_KC):
        for c in range(2):
            cols.append(base + c * 128 + i128)
        for c in range(2):
            cols.append(base + c * 128 + rot(i128, 32))
    for ci in range(7):
        cols.append(O_XBC + ci * 128 + i128)
    cols = np.stack(cols)
    w_in = inp["w_in"]
    wf = w_in[:, :, cols]
    S["w_fm"] = np.ascontiguousarray(wf.reshape(DEPTH, KC, 128, NCH, 128).transpose(0, 3, 2, 1, 4), dtype=f32)
    tcols = np.concatenate([O_VA + np.arange(128), O_VC + np.arange(256), O_DTF + np.arange(12), O_Z + np.arange(384)])
    wt = w_in[:, :, tcols]
    S["w_tok"] = np.ascontiguousarray(wt.reshape(DEPTH, KC, 128, NTK).transpose(0, 2, 1, 3), dtype=f32)
    S["w_out"] = np.ascontiguousarray(inp["w_out"], dtype=f32)
    sr = np.concatenate([inp["attn_sink"], inp["dt_bias"].reshape(DEPTH, 12), inp["a_log"].reshape(DEPTH, 12), inp["d_skip"],
                         inp["lam_q"].reshape(DEPTH, 64), inp["lam_k"].reshape(DEPTH, 64), inp["diff_norm_w"], inp["ssm_norm_w"]], axis=1)
    S["srow"] = np.ascontiguousarray(np.broadcast_to(sr[:, None, :], (DEPTH, 128, NSR)), dtype=f32)
    cw = np.concatenate([inp["conv_w"], inp["conv_b"][:, None, :]], axis=1)
    S["convp"] = np.ascontiguousarray(cw.reshape(DEPTH, 6, 7, 128).transpose(0, 3, 2, 1), dtype=f32)
    tt = np.arange(2048)
    rows, colsg = (tt // 64).astype(f32), (tt % 64).astype(f32)

    def rope_tab(hd):
        quarter = hd // 4
        inv = (10000.0 ** (-np.arange(quarter, dtype=f32) / quarter)).astype(f32)
        ang = np.concatenate([rows[:, None] * inv, colsg[:, None] * inv], axis=-1).astype(f32)
        d = i128 % hd
        jj = d % (hd // 2)
        sign = np.where(d < hd // 2, -1.0, 1.0).astype(f32)
        cos = np.ones((128, T), f32)
        sin = np.zeros((128, T), f32)
        cos[:, 256:] = np.cos(ang).astype(f32)[:, jj].T
        sin[:, 256:] = np.sin(ang).astype(f32)[:, jj].T * sign[:, None]
        return np.stack([cos, sin]).astype(ml_dtypes.bfloat16)
    S["ropeA"] = rope_tab(64)
    S["ropeC"] = rope_tab(32)
    kk, qq = np.meshgrid(np.arange(128), np.arange(128), indexing="ij")
    S["bmask"] = np.stack([(qq <= kk), np.ones_like(kk, bool), (qq >= kk)], axis=1).astype(f32).astype(ml_dtypes.bfloat16)
    jj, ll = np.meshgrid(np.arange(128), np.arange(128), indexing="ij")
    pp = np.arange(128)
    S["bandm"] = np.stack([(pp // 32 == 0), (pp // 32 == 1), (pp // 32 == 2), (pp // 32 == 3), (pp // 64 == 0), (pp // 64 == 1)], axis=1).astype(f32)
    S["ssdc"] = np.stack([(jj <= ll).astype(f32), (jj >= ll).astype(f32),
                          np.where(ll >= jj, 0.0, -30000.0).astype(f32), np.where(ll <= jj, 0.0, -30000.0).astype(f32)]).astype(f32)
    return S


def host_inputs(inp, b, shared=None):
    f32 = np.float32
    m = dict(shared if shared is not None else _shared_inputs(inp))
    m["xin"] = np.ascontiguousarray(np.concatenate([inp["ctx"][b], inp["x"][b]], axis=0), dtype=f32)
    cc = np.stack([inp["c"][b], inp["c_ctx"]], axis=0)
    m["cvec"] = np.ascontiguousarray(cc.reshape(2, KC, 128).transpose(2, 1, 0), dtype=f32)
    return m


def bcast_rows(C, dst, src_fc0, name):
    B, nc, ps = C.B, C.nc, C.psum
    if True:
        dg = C.dg
        k = 0
        for wch in range(2):
            for half in range(2):
                bank = 6 + (k % 2)
                for q in range(4):
                    kc = half * 4 + q
                    s = (k * 4 + q) % 2
                    B.ts("dve", dg[:, s, :], C.identf[:, :], C.adaT[:, src_fc0 + kc, wch:wch + 1], ALU.mult,
                         r=["identf", "adaT"], w=[("diag", s)])
                    B.mm(ps[:, bank, q * 128:(q + 1) * 128], [(C.onesf[:, :], dg[:, s, :])],
                         r=["onesf", ("diag", s)], w=[("ps", bank)])
                B.copy("act", dst[:, wch, half * 512:(half + 1) * 512], ps[:, bank, :], r=[("ps", bank)], w=[name])
                k += 1


def post_ln(C, li, idx, tiles, out_scale=1.0):
    B, nc = C.B, C.nc
    ln_stats(C, tiles)
    if True:
        rows = C.rows
        for j in range(2):
            B.dma("sp", rows[:, j, :], C.I["lnrows"][li, 2 * idx + j, :, :], w=[("lnrow", j)])
            if out_scale != 1.0:
                B.ts("pool", rows[:, j, :], rows[:, j, :], out_scale, ALU.mult, r=[("lnrow", j)], w=[("lnrow", j)])
        for t in tiles:
            B.ts("dve", C.x[:, t, :], C.x[:, t, :], C.mv[:, t, 0:1], ALU.subtract, C.rstd[:, t:t + 1], ALU.mult,
                 r=[("x", t), ("mv", t), ("rstd", t)], w=[("x", t)])
            B.tt("pool", C.x[:, t, :], C.x[:, t, :], rows[:, 0, :], ALU.mult, r=[("x", t), ("lnrow", 0)], w=[("x", t)])
            B.tt("pool", C.x[:, t, :], C.x[:, t, :], rows[:, 1, :], ALU.add, r=[("x", t), ("lnrow", 1)], w=[("x", t)])


def ffn_phase(C, li, tiles):
    B, nc, I, ps, P = C.B, C.nc, C.I, C.psum, C.P
    moe = (li % 2 == 1)
    j = li // 2
    use_ctx = 0 in tiles
    ln_modulate(C, which_sc=1, sh_base=24, ctx_tiles=use_ctx)
    if "xTf%d" % li in C.dbg:
        d = B.dram_out("dbg_xTf%d" % li, [128, KC, T], BF16)
        B.dma("sp", d[:, :, :], C.xT[:, :, :], r=[("xT", t) for t in range(NT)])
    tgs = TGS if use_ctx else TGS[1:]
    if True:
        sbt = C.sb
        g2bc = sbt("g2bc", [128, 2, D])
        wgu = sbt("wgu", [128, 2, 2, KC, FG], BF16)
        wdf = sbt("wdf", [128, 1, 2, D])
        wdl = sbt("wdl", [128, 2, 2, D], BF16)
        wdc = sbt("wdc", [128, 2, 2, D], BF16)
        hT = sbt("hT", [128, 2, 2, T], BF16)
        sg = sbt("sg", [128, 3, 512], BF16)
        etmp = sbt("etmp", [128, 2, 512])
        evi = [0]
        bcast_rows(C, g2bc, 40, "g2bc")
        gates = None
        if moe:
            wr = sbt("wr", [128, KC, NEXP])
            wrb = sbt("wrb", [128, KC, NEXP], BF16)
            rb = sbt("rb", [128, NEXP])
            lg = sbt("lg", [128, NT, NEXP])
            mx8 = sbt("mx8", [128, NT, 8])
            mk1 = sbt("mk1", [128, NT, NEXP])
            mk2 = sbt("mk2", [128, NT, NEXP])
            gates = sbt("gates", [128, NT, NEXP])
            w12 = sbt("w12", [128, 3, NT])
            B.dma("sp", wr[:, :, :], I["router_w"][j, :, :, :], w=["wr"])
            B.dma("sp", rb[:, :], I["router_b"][j, :, :], w=["rb"])
            B.copy("dve", wrb[:, :, :], wr[:, :, :], r=["wr"], w=["wrb"])
            bank = 6
            for t in tiles:
                B.mm(ps[:, bank, t * 8:(t + 1) * 8],
                     [(C.xT[:, kc, t * 128:(t + 1) * 128], wrb[:, kc, :]) for kc in range(KC)],
                     r=[("xT", t), "wrb"], w=[("ps", bank)])
            t0, t1 = tiles[0], tiles[-1] + 1
            n = t1 - t0
            B.tt("dve", lg[:, t0:t1, :], ps[:, bank, t0 * 8:t1 * 8].rearrange("p (t e) -> p t e", e=8),
                 rb[:, :].unsqueeze(1).to_broadcast([128, n, NEXP]), ALU.add, r=[("ps", bank), "rb"], w=["lg"])
            for t in tiles:
                P.add("dve", lambda e, t=t: e.max(out=mx8[:, t, :], in_=lg[:, t, :]), r=["lg"], w=["mx8"])
            B.tt("dve", mk1[:, t0:t1, :], lg[:, t0:t1, :], mx8[:, t0:t1, 0:1].to_broadcast([128, n, NEXP]), ALU.is_equal,
                 r=["lg", "mx8"], w=["mk1"])
            B.tt("dve", mk2[:, t0:t1, :], lg[:, t0:t1, :], mx8[:, t0:t1, 1:2].to_broadcast([128, n, NEXP]), ALU.is_equal,
                 r=["lg", "mx8"], w=["mk2"])
            B.tt("dve", w12[:, 0, t0:t1], mx8[:, t0:t1, 1], mx8[:, t0:t1, 0], ALU.subtract, r=["mx8"], w=["w12a"])
            B.act(w12[:, 0, t0:t1], w12[:, 0, t0:t1], AF.Exp, r=["w12a"], w=["w12a"])
            B.ts("dve", w12[:, 1, t0:t1], w12[:, 0, t0:t1], 1.0, ALU.add, r=["w12a"], w=["w12b"])
            P.add("dve", lambda e: e.reciprocal(out=w12[:, 1, t0:t1], in_=w12[:, 1, t0:t1]), r=["w12b"], w=["w12b"])
            B.tt("dve", w12[:, 2, t0:t1], w12[:, 0, t0:t1], w12[:, 1, t0:t1], ALU.mult, r=["w12a", "w12b"], w=["w12c"])
            B.tt("dve", mk1[:, t0:t1, :], mk1[:, t0:t1, :], w12[:, 1, t0:t1].unsqueeze(2).to_broadcast([128, n, NEXP]),
                 ALU.mult, r=["mk1", "w12b"], w=["mk1"])
            B.tt("dve", mk2[:, t0:t1, :], mk2[:, t0:t1, :], w12[:, 2, t0:t1].unsqueeze(2).to_broadcast([128, n, NEXP]),
                 ALU.mult, r=["mk2", "w12c"], w=["mk2"])
            B.tt("dve", gates[:, t0:t1, :], mk1[:, t0:t1, :], mk2[:, t0:t1, :], ALU.add, r=["mk1", "mk2"], w=["gates"])
        groups = [(e, fg) for e in range(NEXP if moe else 1) for fg in range(NFG)]
        poolA = PsumPool([0, 1, 2, 3])
        poolB = PsumPool([4, 5, 6, 7])
        sgi = [0]

        def loads_gu(i):
            e, fg = groups[i]
            s = i % 2
            src_gu = I["exp_gu"][j, e, fg] if moe else I["ffn_gu"][j, fg]
            B.dma("pool", wgu[:, s, :, :, :], src_gu.rearrange("g p k f -> p g k f"), w=[("wgu", s)])

        def loads_dn(i):
            e, fg = groups[i]
            s = i % 2
            src_dn = I["exp_dn"][j, e, fg * FG:(fg + 1) * FG, :] if moe else I["ffn_dn"][j, fg * FG:(fg + 1) * FG, :]
            B.dma("sp", wdf[:, 0, :, :], src_dn.rearrange("(c p) d -> p c d", p=128), w=["wdf"])
            B.tt("pool", wdl[:, s, :, :], wdf[:, 0, :, :], g2bc[:, 0:1, :].to_broadcast([128, 2, D]), ALU.mult,
                 r=["wdf", "g2bc"], w=[("wdl", s)])
            if use_ctx:
                B.tt("pool", wdc[:, s, :, :], wdf[:, 0, :, :], g2bc[:, 1:2, :].to_broadcast([128, 2, D]), ALU.mult,
                     r=["wdf", "g2bc"], w=[("wdc", s)])

        def phaseA(i):
            s = i % 2
            for (t0, n) in tgs:
                tl = [("xT", t) for t in range(t0 // 128, (t0 + n) // 128)]
                for fc in range(2):
                    bg, bu = poolA.next(), poolA.next()
                    B.mm(ps[:, bg, 0:n], [(wgu[:, s, 0, kc, fc * 128:(fc + 1) * 128], C.xT[:, kc, t0:t0 + n]) for kc in range(KC)],
                         r=[("wgu", s)] + tl, w=[("ps", bg)])
                    B.mm(ps[:, bu, 0:n], [(wgu[:, s, 1, kc, fc * 128:(fc + 1) * 128], C.xT[:, kc, t0:t0 + n]) for kc in range(KC)],
                         r=[("wgu", s)] + tl, w=[("ps", bu)])
                    k = sgi[0] % 3
                    sgi[0] += 1
                    B.act(sg[:, k, 0:n], ps[:, bg, 0:n], AF.Silu, r=[("ps", bg)], w=[("sg", k)])
                    B.tt("dve", hT[:, s, fc, t0:t0 + n], sg[:, k, 0:n], ps[:, bu, 0:n], ALU.mult,
                         r=[("sg", k), ("ps", bu)], w=[("hT", s, fc, t0)])

        def phaseB(i):
            e, fg = groups[i]
            s = i % 2
            for t in tiles:
                wd = wdc if t < 2 else wdl
                tg0 = [t0 for (t0, n) in TGS if t0 <= t * 128 < t0 + n][0]
                for half in range(2):
                    bo = poolB.next()
                    B.mm(ps[:, bo, :], [(hT[:, s, fc, t * 128:(t + 1) * 128], wd[:, s, fc, half * 512:(half + 1) * 512])
                                        for fc in range(2)],
                         r=[("hT", s, 0, tg0), ("hT", s, 1, tg0), ("wdc" if t < 2 else "wdl", s)], w=[("ps", bo)])
                    xs = C.x[:, t, half * 512:(half + 1) * 512]
                    sc = gates[:, t, e:e + 1] if moe else 1.0
                    if half == 0:
                        B.stt(xs, ps[:, bo, :], sc, xs, ALU.mult, ALU.add,
                              r=[("ps", bo), ("x", t)] + (["gates"] if moe else []), w=[("x", t, 0)])
                    else:
                        kq = evi[0] % 2
                        evi[0] += 1
                        B.act(etmp[:, kq, :], ps[:, bo, :], AF.Identity, scale=sc,
                              r=[("ps", bo)] + (["gates"] if moe else []), w=[("etmp", kq)])
                        B.tt("pool", xs, xs, etmp[:, kq, :], ALU.add, r=[("etmp", kq), ("x", t)], w=[("x", t, 1)])

        n = len(groups)
        loads_gu(0)
        if n > 1:
            loads_gu(1)
        loads_dn(0)
        for i in range(n):
            phaseA(i)
            if i + 2 < n:
                loads_gu(i + 2)
            if i >= 1:
                phaseB(i - 1)
            if i + 1 < n:
                loads_dn(i + 1)
        phaseB(n - 1)


CH_QA, CH_QAR, CH_KA, CH_KAR = 0, 3, 6, 8
CH_QC, CH_QCR, CH_KC, CH_KCR = 10, 12, 14, 16
CH_XBC = 18
NCH = 25
MIXERS = "ACB"
B_STAGE = 9
B_SUB = 9
TK_VA, TK_VC, TK_DT, TK_Z, NTK = 0, 128, 384, 396, 780
SR_SINK, SR_DTB, SR_ALOG, SR_DSKIP, SR_LQ, SR_LK, SR_DNW, SR_SNW, NSR = 0, 6, 18, 30, 36, 100, 164, 228, 612
PADT = T + 8


def fm_chunks(C, li, ci_list, evac):
    B, ps, I = C.B, C.psum, C.I
    ws = []
    for ci in ci_list:
        s = C.wfm_i % 4
        C.wfm_i += 1
        B.dma("pool", C.wfm[:, s, :, :], I["w_fm"][li, ci, :, :, :], w=[("wfm", s)])
        ws.append(s)
    for gi, (t0, n) in enumerate(TGS):
        tl = [("xT", t) for t in range(t0 // 128, (t0 + n) // 128)]
        banks = []
        for s in ws:
            bk = C.poolF.next()
            B.mm(ps[:, bk, 0:n], [(C.wfm[:, s, kc, :], C.xT[:, kc, t0:t0 + n]) for kc in range(KC)],
                 r=[("wfm", s)] + tl, w=[("ps", bk)])
            banks.append(bk)
        evac(gi, t0, n, banks)


def rope_evac(C, dst, dtok, cos, sin):
    B, ps = C.B, C.psum

    def evac(gi, t0, n, banks):
        bx, br = banks
        if t0 < 256:
            B.copy("act", dst[:, t0:t0 + n], ps[:, bx, 0:n], r=[("ps", bx), ("ps", br)], w=[dtok + (gi,)])
            return
        k = C.rt_i % C.rt_n
        C.rt_i += 1
        B.tt("dve", C.rtmp[:, k, 0, 0:n], ps[:, bx, 0:n], cos[:, t0:t0 + n], ALU.mult, r=[("ps", bx), "rope"], w=[("rtmp", k, 0)])
        B.tt("dve", C.rtmp[:, k, 1, 0:n], ps[:, br, 0:n], sin[:, t0:t0 + n], ALU.mult, r=[("ps", br), "rope"], w=[("rtmp", k, 1)])
        B.tt("pool", dst[:, t0:t0 + n], C.rtmp[:, k, 0, 0:n], C.rtmp[:, k, 1, 0:n], ALU.add,
             r=[("rtmp", k, 0), ("rtmp", k, 1)], w=[dtok + (gi,)])
    return evac


def tg_of(t):
    return [i for i, (t0, n) in enumerate(TGS) if t0 <= t * 128 < t0 + n][0]


def out_proj_tile(C, t, y_bf, nchunk, wo, ytok):
    B, ps = C.B, C.psum
    wch = 1 if t < 2 else 0
    bk = C.poolT.next()
    psb = ps[:, bk, :].bitcast(BF16)

    def tr(e):
        ins = None
        for c in range(nchunk):
            ins = e.transpose(out=psb[:, c * 128:(c + 1) * 128], in_=y_bf[:, c * 128:(c + 1) * 128], identity=C.identb[:, :])
        return ins
    C.P.add("pe", tr, r=[ytok, "identb"], w=[("ps", bk)])
    k = C.yT_i % 2
    C.yT_i += 1
    B.copy("act", C.yT[:, k, 0:nchunk * 128], psb[:, 0:nchunk * 128], r=[("ps", bk)], w=[("yT", k)])
    for half in range(2):
        bo = C.poolO.next()
        B.mm(ps[:, bo, :], [(C.yT[:, k, c * 128:(c + 1) * 128], wo[:, c, half * 512:(half + 1) * 512]) for c in range(nchunk)],
             r=[("yT", k), "wo"], w=[("ps", bo)])
        m = 0
        B.tt("dve", C.otmp[:, m, :], ps[:, bo, :], C.g1bc[:, wch, half * 512:(half + 1) * 512], ALU.mult,
             r=[("ps", bo), "g1bc"], w=[("otmp", m)])
        xs = C.x[:, t, half * 512:(half + 1) * 512]
        B.tt("pool", xs, xs, C.otmp[:, m, :], ALU.add, r=[("otmp", m), ("x", t)], w=[("x", t)])


def mixer_phase(C, li, tiles):
    B, nc, I, ps, P = C.B, C.nc, C.I, C.psum, C.P
    lam_init = 0.8 - 0.6 * math.exp(-0.3 * li)
    C.g1bc = C.sb("g1bc", [128, 2, D])
    C.srow = C.sb("srow", [128, NSR])
    C.yT = C.sb("yT", [128, 2, 384], BF16)
    C.otmp = C.sb("otmp", [128, 1, 512])
    C.wfm_i = C.rt_i = C.yT_i = C.ot_i = 0
    C.poolF = PsumPool([0, 1, 2, 3])
    C.poolT = PsumPool([6])
    C.poolO = PsumPool([4, 5])
    bcast_rows(C, C.g1bc, 16, "g1bc")
    B.dma("sp", C.srow[:, :], I["srow"][li, :, :], w=["srow"])
    with contextlib.ExitStack() as sc:
        sba = lambda name, shape, dt=F32: sc.enter_context(nc.sbuf_tensor("%s_a%d" % (name, li), list(shape), dt))
        if "A" in MIXERS:
            mixer_A(C, li, tiles, sba)
        P.barrier()
    with contextlib.ExitStack() as sc:
        sba = lambda name, shape, dt=F32: sc.enter_context(nc.sbuf_tensor("%s_c%d" % (name, li), list(shape), dt))
        if "C" in MIXERS:
            mixer_C(C, li, tiles, sba, lam_init)
        P.barrier()
    with contextlib.ExitStack() as sc:
        sba = lambda name, shape, dt=F32: sc.enter_context(nc.sbuf_tensor("%s_b%d" % (name, li), list(shape), dt))
        if "B" in MIXERS:
            mixer_B(C, li, tiles, sba)
        P.barrier()


def tok_proj(C, li, t, col0, ncol, wt):
    B, ps = C.B, C.psum
    bk = C.poolF.next()
    B.mm(ps[:, bk, 0:ncol], [(C.xT[:, kc, t * 128:(t + 1) * 128], wt[:, kc, col0:col0 + ncol]) for kc in range(KC)],
         r=[("xT", t), "wtok"], w=[("ps", bk)])
    return bk


def mixer_A(C, li, tiles, sba):
    B, nc, I, ps, P = C.B, C.nc, C.I, C.psum, C.P
    C.wfm = sba("wfm", [128, 4, KC, 128], BF16)
    qT = sba("qT", [128, 3, T], BF16)
    kT = sba("kT", [128, 2, T], BF16)
    vA = sba("vA", [128, NT, 2, 66], BF16)
    rope = sba("rope", [128, 2, T], BF16)
    C.rtmp = sba("rtmp", [128, 2, 2, 512])
    C.rt_n = 2
    wt = sba("wtA", [128, KC, 128], BF16)
    wo = sba("woA", [128, 3, D], BF16)
    bm = sba("bmask", [128, 3, 128], BF16)
    E1 = sba("E1", [128, 3, 384], BF16)
    E2 = sba("E2", [128, 3, 256], BF16)
    esink = sba("esink", [128, 6])
    den = sba("den", [128, 2, 6])
    ya = sba("ya", [128, 2, 384], BF16)
    qm = sba("qm", [128, 3, 128], BF16)
    bandm = sba("bandm", [128, 6])
    B.dma("sp", bandm[:, :], I["bandm"][:, :], w=["bandm"])
    B.dma("sp", rope[:, :, :], I["ropeA"].rearrange("a p t -> p a t"), w=["rope"])
    B.dma("sp", bm[:, :, :], I["bmask"][:, :, :], w=["bmask"])
    B.dma("pool", wt[:, :, :], I["w_tok"][li, :, :, TK_VA:TK_VA + 128], w=["wtok"])
    B.dma("pool", wo[:, :, :], I["w_out"][li, 0:384, :].rearrange("(c p) d -> p c d", p=128), w=["wo"])
    B.memset("pool", vA[:, :, :, 64:66], 1.0, w=["vA1"])
    B.act(esink[:, :], C.srow[:, SR_SINK:SR_SINK + 6], AF.Exp, r=["srow"], w=["esink"])
    for c in range(3):
        fm_chunks(C, li, [CH_QA + c, CH_QAR + c], rope_evac(C, qT[:, c, :], ("qT", c), rope[:, 0, :], rope[:, 1, :]))
    for g in range(2):
        fm_chunks(C, li, [CH_KA + g, CH_KAR + g], rope_evac(C, kT[:, g, :], ("kT", g), rope[:, 0, :], rope[:, 1, :]))
    for t in range(NT):
        bk = tok_proj(C, li, t, 0, 128, wt)
        B.copy("act", vA[:, t, :, 0:64], ps[:, bk, 0:128].rearrange("p (g d) -> p g d", g=2), r=[("ps", bk)], w=[("vA", t)])
    scale = 64 ** -0.5
    poolS = PsumPool([0, 1, 2, 4])
    C.poolO = PsumPool([5])
    ei = 0
    for qi, t in enumerate(tiles):
        if t < 2:
            loc = []
        else:
            loc = [(j, t - 1 + j) for j in range(3) if 2 <= t - 1 + j < NT]
        bo = 7 if qi % 2 == 0 else 3
        pend = []
        for h in range(6):
            g, b, c = h // 3, h % 2, h // 2
            rows = slice(b * 64, (b + 1) * 64)
            qtok = ("qT", c, tg_of(t))
            e = ei % 3
            ei += 1
            pv = []
            if loc:
                b1 = poolS.next()
                for (j, kt) in loc:
                    B.mm(ps[:, b1, j * 128:(j + 1) * 128], [(kT[rows, g, kt * 128:(kt + 1) * 128], qT[rows, c, t * 128:(t + 1) * 128])],
                         r=[("kT", g, tg_of(kt)), qtok], w=[("ps", b1)])
                j0, j1 = loc[0][0], loc[-1][0] + 1
                B.act(E1[:, e, j0 * 128:j1 * 128], ps[:, b1, j0 * 128:j1 * 128], AF.Exp, scale=scale, r=[("ps", b1)], w=[("E1", e)])
                B.tt("dve", E1[:, e, j0 * 128:j1 * 128], E1[:, e, j0 * 128:j1 * 128],
                     bm[:, j0:j1, :].rearrange("p a b -> p (a b)"), ALU.mult, r=[("E1", e), "bmask"], w=[("E1", e)])
                pv += [(E1[:, e, j * 128:(j + 1) * 128], vA[:, kt, g, 0:65], ("E1", e), kt) for (j, kt) in loc]
            b2 = poolS.next()
            for kt in range(2):
                B.mm(ps[:, b2, kt * 128:(kt + 1) * 128], [(kT[rows, g, kt * 128:(kt + 1) * 128], qT[rows, c, t * 128:(t + 1) * 128])],
                     r=[("kT", g, 0), qtok], w=[("ps", b2)])
            B.act(E2[:, e, :], ps[:, b2, 0:256], AF.Exp, scale=scale, r=[("ps", b2)], w=[("E2", e)])
            pv += [(E2[:, e, kt * 128:(kt + 1) * 128], vA[:, kt, g, 0:65], ("E2", e), kt) for kt in range(2)]
            pend.append((ps[:, bo, h * 66:h * 66 + 65], [(l, r_) for (l, r_, _, _) in pv],
                         list({x[2] for x in pv}) + [("vA", x[3]) for x in pv] + ["vA1"]))
            while len(pend) > 1:
                o_, p_, r_ = pend.pop(0)
                B.mm(o_, p_, r=r_, w=[("ps", bo)])
        while pend:
            o_, p_, r_ = pend.pop(0)
            B.mm(o_, p_, r=r_, w=[("ps", bo)])
        k = qi % 2
        pv3 = ps[:, bo, 0:396].rearrange("p (h d) -> p h d", d=66)
        B.tt("dve", den[:, k, :], pv3[:, :, 64], esink[:, :], ALU.add, r=[("ps", bo), "esink"], w=[("den", k)])
        P.add("dve", lambda e_, k=k: e_.reciprocal(out=den[:, k, :], in_=den[:, k, :]), r=[("den", k)], w=[("den", k)])
        B.tt("dve", ya[:, k, :].rearrange("p (h d) -> p h d", d=64), pv3[:, :, 0:64],
             den[:, k, :].unsqueeze(2).to_broadcast([128, 6, 64]), ALU.mult, r=[("ps", bo), ("den", k)], w=[("ya", k)])
        if "mixA%d" % li in C.dbg:
            d = dbg_out(C, "mixA%d" % li, 384)
            B.dma("sp", d[t * 128:(t + 1) * 128, :], ya[:, k, :], r=[("ya", k)])
        out_proj_tile(C, t, ya[:, k, :], 3, wo, ("ya", k))


def mixer_C(C, li, tiles, sba, lam_init):
    B, nc, I, ps, P = C.B, C.nc, C.I, C.psum, C.P
    C.poolO = PsumPool([4, 5])
    C.wfm = sba("wfm", [128, 4, KC, 128], BF16)
    qT = sba("qT", [128, 2, T], BF16)
    kT = sba("kT", [128, 2, T], BF16)
    vC = sba("vC", [128, NT, 4, 66], BF16)
    rope = sba("rope", [128, 2, T], BF16)
    C.rtmp = sba("rtmp", [128, 1, 2, 512])
    C.rt_n = 1
    wt = sba("wtC", [128, KC, 256], BF16)
    wo = sba("woC", [128, 2, D], BF16)
    E = sba("E", [128, 5, 512], BF16)
    yd = sba("yd", [128, NT, 256], BF16)
    lam = sba("lam", [128, 8])
    lqk = sba("lqk", [128, 64])
    nw = sba("nw", [128, 64])
    r12 = sba("r12", [128, 1, 2, 4])
    o12 = sba("o12", [128, 1, 2, 4, 64])
    ss = sba("ss", [128, 1, 4])
    aT = sba("aT", [128, 2, 512])
    qm = sba("qm", [128, 2, 512], BF16)
    bandm = sba("bandm", [128, 6])
    B.dma("sp", bandm[:, :], I["bandm"][:, :], w=["bandm"])
    B.dma("sp", rope[:, :, :], I["ropeC"].rearrange("a p t -> p a t"), w=["rope"])
    B.dma("pool", wt[:, :, :], I["w_tok"][li, :, :, TK_VC:TK_VC + 256], w=["wtok"])
    B.dma("pool", wo[:, :, :], I["w_out"][li, 768:1024, :].rearrange("(c p) d -> p c d", p=128), w=["wo"])
    B.memset("pool", vC[:, :, :, 64:66], 1.0, w=["vC1"])
    B.memset("pool", aT[:, :, :], 0.0, w=[("aT", 0), ("aT", 1)])
    B.tt("dve", lqk[:, :], C.srow[:, SR_LQ:SR_LQ + 64], C.srow[:, SR_LK:SR_LK + 64], ALU.mult, r=["srow"], w=["lqk"])
    P.add("dve", lambda e: e.tensor_reduce(out=lam[:, 0:2], in_=lqk[:, :].rearrange("p (a b) -> p a b", a=2), axis=AX.X, op=ALU.add),
          r=["lqk"], w=["lam"])
    B.act(lam[:, 0:2], lam[:, 0:2], AF.Exp, r=["lam"], w=["lam"])
    B.tt("dve", lam[:, 2:3], lam[:, 1:2], lam[:, 0:1], ALU.subtract, r=["lam"], w=["lam"])
    B.ts("dve", lam[:, 4:5], lam[:, 2:3], -lam_init, ALU.add, r=["lam"], w=["lam"])
    B.ts("dve", nw[:, :], C.srow[:, SR_DNW:SR_DNW + 64], 1.0 - lam_init, ALU.mult, r=["srow"], w=["nw"])
    for c in range(2):
        fm_chunks(C, li, [CH_QC + c, CH_QCR + c], rope_evac(C, qT[:, c, :], ("qT", c), rope[:, 0, :], rope[:, 1, :]))
    for c in range(2):
        fm_chunks(C, li, [CH_KC + c, CH_KCR + c], rope_evac(C, kT[:, c, :], ("kT", c), rope[:, 0, :], rope[:, 1, :]))
    for t in range(NT):
        bk = tok_proj(C, li, t, 0, 256, wt)
        B.copy("act", vC[:, t, :, 0:64], ps[:, bk, 0:256].rearrange("p (g d) -> p g d", g=4), r=[("ps", bk)], w=[("vC", t)])
    scale = 32 ** -0.5
    poolS = PsumPool([0, 1, 2, 6])
    accs = PsumPool([3, 7, 4, 5])
    qgroups = []
    if 0 in tiles:
        qgroups.append((0, [0, 1], [0, 1]))
    for gi in range(1, 5):
        t0 = TGS[gi][0] // 128
        qgroups.append((gi, list(range(t0, t0 + 4)), list(range(NT))))
    ei = 0
    gk = 0
    ai_ = [0]
    for hc in range(4):
        cc = hc // 2
        for (gi, qts, kts) in qgroups:
            nq = len(qts)
            q0 = qts[0] * 128
            n = nq * 128
            k = 0
            gk += 1
            acc = []
            pend = []
            for i in range(2):
                j = 2 * (hc % 2) + i
                rows = slice(32 * j, 32 * j + 32)
                ab = accs.next()
                acc.append(ab)
                B.ts("dve", qm[:, i, 0:n], qT[:, cc, q0:q0 + n], bandm[:, j:j + 1], ALU.mult, r=[("qT", cc, gi), "bandm"], w=[("qm", i)])
                for ki, kt in enumerate(kts):
                    sbk = poolS.next()
                    B.mm(ps[:, sbk, 0:n], [(kT[:, cc, kt * 128:(kt + 1) * 128], qm[:, i, 0:n])],
                         r=[("kT", cc, tg_of(kt)), ("qm", i)], w=[("ps", sbk)])
                    e = ei % 5
                    ei += 1
                    B.act(E[:, e, 0:n], ps[:, sbk, 0:n], AF.Exp, scale=scale, r=[("ps", sbk)], w=[("E", e)])

                    def pv(eng, e=e, kt=kt, ab=ab, ki=ki, n=n, hc=hc, last=(ki == len(kts) - 1)):
                        return eng.matmul(ps[0:65, ab, 0:n], lhsT=vC[:, kt, hc, 0:65], rhs=E[:, e, 0:n], start=(ki == 0), stop=last)
                    pend.append((pv, [("E", e), ("vC", kt), "vC1"], [("ps", ab)]))
                    while len(pend) > 3:
                        f_, r_, w_ = pend.pop(0)
                        P.add("pe", f_, r=r_, w=w_)
            while pend:
                f_, r_, w_ = pend.pop(0)
                P.add("pe", f_, r=r_, w=w_)
            for i in range(2):
                ab = acc[i]
                m = ai_[0] % 2
                ai_[0] += 1
                B.copy("act", aT[0:65, m, 0:n], ps[0:65, ab, 0:n], r=[("ps", ab)], w=[("aT", m)])

                def trb(eng, ab=ab, m=m, nq=nq):
                    ins = None
                    for qi in range(nq):
                        ins = eng.transpose(out=ps[:, ab, qi * 66:qi * 66 + 66], in_=aT[0:66, m, qi * 128:(qi + 1) * 128],
                                            identity=C.identf[0:66, 0:66])
                    return ins
                P.add("pe", trb, r=[("aT", m), "identf"], w=[("ps", ab)])
            a1 = ps[:, acc[0], 0:nq * 66].rearrange("p (q d) -> p q d", d=66)
            a2 = ps[:, acc[1], 0:nq * 66].rearrange("p (q d) -> p q d", d=66)
            P.add("dve", lambda e_, k=k, a1=a1, nq=nq: e_.reciprocal(out=r12[:, k, 0, 0:nq], in_=a1[:, :, 64]), r=[("ps", acc[0])], w=[("r12", k)])
            P.add("dve", lambda e_, k=k, a2=a2, nq=nq: e_.reciprocal(out=r12[:, k, 1, 0:nq], in_=a2[:, :, 64]), r=[("ps", acc[1])], w=[("r12", k)])
            B.ts("dve", r12[:, k, 1, 0:nq], r12[:, k, 1, 0:nq], lam[:, 4:5], ALU.mult, r=[("r12", k), "lam"], w=[("r12", k)])
            B.tt("dve", o12[:, k, 0, 0:nq, :], a1[:, :, 0:64], r12[:, k, 0, 0:nq].unsqueeze(2).to_broadcast([128, nq, 64]), ALU.mult,
                 r=[("ps", acc[0]), ("r12", k)], w=[("o1", k)])
            B.tt("dve", o12[:, k, 1, 0:nq, :], a2[:, :, 0:64], r12[:, k, 1, 0:nq].unsqueeze(2).to_broadcast([128, nq, 64]), ALU.mult,
                 r=[("ps", acc[1]), ("r12", k)], w=[("o2", k)])
            B.tt("pool", o12[:, k, 0, 0:nq, :], o12[:, k, 0, 0:nq, :], o12[:, k, 1, 0:nq, :], ALU.add, r=[("o1", k), ("o2", k)], w=[("o1", k)])
            B.tt("pool", o12[:, k, 1, 0:nq, :], o12[:, k, 0, 0:nq, :], o12[:, k, 0, 0:nq, :], ALU.mult, r=[("o1", k)], w=[("o2", k)])
            P.add("dve", lambda e_, k=k, nq=nq: e_.tensor_reduce(out=ss[:, k, 0:nq], in_=o12[:, k, 1, 0:nq, :], axis=AX.X, op=ALU.add),
                  r=[("o2", k)], w=[("ss", k)])
            B.act(ss[:, k, 0:nq], ss[:, k, 0:nq], AF.Sqrt, scale=1.0 / 64, bias=EPS, r=[("ss", k)], w=[("ss", k)])
            P.add("dve", lambda e_, k=k, nq=nq: e_.reciprocal(out=ss[:, k, 0:nq], in_=ss[:, k, 0:nq]), r=[("ss", k)], w=[("ss", k)])
            B.tt("dve", o12[:, k, 0, 0:nq, :], o12[:, k, 0, 0:nq, :], ss[:, k, 0:nq].unsqueeze(2).to_broadcast([128, nq, 64]), ALU.mult,
                 r=[("o1", k), ("ss", k)], w=[("o1", k)])
            B.tt("pool", yd[:, qts[0]:qts[0] + nq, hc * 64:(hc + 1) * 64], o12[:, k, 0, 0:nq, :],
                 nw[:, :].unsqueeze(1).to_broadcast([128, nq, 64]), ALU.mult, r=[("o1", k), "nw"], w=[("yd", qt) for qt in qts])
    for t in tiles:
        if "mixC%d" % li in C.dbg:
            d = dbg_out(C, "mixC%d" % li, 256)
            B.dma("sp", d[t * 128:(t + 1) * 128, :], yd[:, t, :], r=[("yd", t)])
        out_proj_tile(C, t, yd[:, t, :], 2, wo, ("yd", t))


def mixer_B(C, li, tiles, sba):
    B, nc, I, ps, P = C.B, C.nc, C.I, C.psum, C.P
    C.poolO = PsumPool([4, 5])
    xsT = sba("xsT", [128, 3, T], BF16)
    BT = sba("BT", [128, 2, T], BF16)
    CT = sba("CT", [128, 2, T], BF16)
    wz = sba("wz", [128, KC, 384], BF16)
    wdt = sba("wdt", [128, KC, 12], BF16)
    wo = sba("woB", [128, 3, D], BF16)
    cst = sba("ssdc", [128, 4, 128])
    Abc = sba("Abc", [128, 12])
    hb_scr = B.dram_out("scr_hb%d" % li, [NT, 128, 384], BF16)
    B.dma("pool", wz[:, :, :], I["w_tok"][li, :, :, TK_Z:TK_Z + 384], w=["wtok"])
    B.dma("pool", wdt[:, :, :], I["w_tok"][li, :, :, TK_DT:TK_DT + 12], w=["wdt"])
    B.dma("pool", wo[:, :, :], I["w_out"][li, 384:768, :].rearrange("(c p) d -> p c d", p=128), w=["wo"])
    B.dma("sp", cst[:, :, :], I["ssdc"].rearrange("a p l -> p a l"), w=["ssdc"])
    B.act(Abc[:, :], C.srow[:, SR_ALOG:SR_ALOG + 12], AF.Exp, r=["srow"], w=["Abc"])
    B.ts("dve", Abc[:, :], Abc[:, :], -1.0, ALU.mult, r=["Abc"], w=["Abc"])
    with contextlib.ExitStack() as sc:
        sbc = lambda name, shape, dt=F32: sc.enter_context(nc.sbuf_tensor("%s_bc%d" % (name, li), list(shape), dt))
        C.wfm = sbc("wfm", [128, 4, KC, 128], BF16)
        pre = sbc("pre", [128, 2, PADT])
        acc = sbc("acc", [128, 2, 512])
        cp = sbc("convp", [128, 7, 6])
        B.dma("sp", cp[:, :, :], I["convp"][li, :, :, :], w=["convp"])
        for k in range(2):
            B.memset("pool", pre[:, k, :], 0.0, w=[("pre", k, gi) for gi in range(5)])
        dsts = [xsT[:, 0, :], xsT[:, 1, :], xsT[:, 2, :], BT[:, 0, :], BT[:, 1, :], CT[:, 0, :], CT[:, 1, :]]
        ai = 0
        for ci in range(7):
            k = ci % 2

            def evac(gi, t0, n, banks, k=k):
                off = 2 if t0 < 256 else 6
                B.copy("act", pre[:, k, off + t0:off + t0 + n], ps[:, banks[0], 0:n], r=[("ps", banks[0])], w=[("pre", k, gi)])
            fm_chunks(C, li, [CH_XBC + ci], evac)
            for gi, (t0, n) in enumerate(TGS):
                a = ai % 2
                ai += 1
                base = t0 if t0 < 256 else t0 + 4
                rd = [("pre", k, g2) for g2 in range(5)]
                B.ts("dve", acc[:, a, 0:n], pre[:, k, base:base + n], cp[:, ci, 0:1], ALU.mult, r=rd + ["convp"], w=[("acc", a)])
                for tap in range(1, 5):
                    B.stt(acc[:, a, 0:n], pre[:, k, base + tap:base + tap + n], cp[:, ci, tap:tap + 1], acc[:, a, 0:n], ALU.mult, ALU.add,
                          r=rd + ["convp", ("acc", a)], w=[("acc", a)])
                B.act(dsts[ci][:, t0:t0 + n], acc[:, a, 0:n], AF.Silu, bias=cp[:, ci, 5:6], r=[("acc", a), "convp"], w=[("u", ci, gi)])
        P.barrier()
    if B_STAGE < 1:
        return
    with contextlib.ExitStack() as sc:
        sbp = lambda name, shape, dt=F32: sc.enter_context(nc.sbuf_tensor("%s_bp%d" % (name, li), list(shape), dt))
        sc_all = sbp("sc_all", [128, 4, NT, 12])
        ect_all = sbp("ect_all", [128, NT, 12])
        AT = sbp("AT", [128, 6, 128])
        Dm = sbp("Dm", [128, 1, 3, 128])
        E = sbp("E", [128, 2, 3, 128], BF16)
        E0 = sbp("E0", [128, 2, 3, 128], BF16)
        MT = sbp("MT", [128, 1, 12, 128], BF16)
        CTp = sbp("CTp", [128, 1, 12, 128], BF16)
        xtok = sbp("xtok", [128, 2, 6, 64], BF16)
        Btok = sbp("Btok", [128, 2, 2, 128], BF16)
        Xdt = sbp("Xdt", [128, 2, 12, 64], BF16)
        Xw = sbp("Xw", [128, 2, 12, 64], BF16)
        H32 = sbp("H32", [128, 2, 6, 64])
        H16 = sbp("H16", [128, 6, 64], BF16)
        Hbin = sbp("Hbin", [128, 2, 6, 64], BF16)
        yb32 = sbp("yb32", [128, 384])
        tmp32 = sbp("tmp32", [128, 384])
        sz = tmp32
        ssq = sbp("ssq", [128, 2])
        yb16 = sbp("yb16", [128, 2, 384], BF16)
        poolP = PsumPool([0, 1, 2])
        C.poolF = PsumPool([0, 1, 2])
        st_i = [0]
        dt_a, a_a, cum_a, dtw_a = [sc_all[:, i, :, :] for i in range(4)]
        mtf = MT[:, 0, :, :].rearrange("p h l -> p (h l)").bitcast(F32)
        xb, ax, mx = [mtf[:, i * 216:(i + 1) * 216].rearrange("p (t h) -> p t h", h=12) for i in range(3)]
        fl = lambda ap: ap.rearrange("p t h -> p (t h)")
        for t in range(NT):
            B.mm(ps[:, 0, t * 12:(t + 1) * 12], [(C.xT[:, kc, t * 128:(t + 1) * 128], wdt[:, kc, :]) for kc in range(KC)],
                 r=[("xT", t), "wdt"], w=[("ps", 0)])
        B.tt("dve", xb, ps[:, 0, 0:NT * 12].rearrange("p (t h) -> p t h", h=12),
             C.srow[:, SR_DTB:SR_DTB + 12].unsqueeze(1).to_broadcast([128, NT, 12]), ALU.add, r=[("ps", 0), "srow"], w=["sc0"])
        B.act(fl(ax), fl(xb), AF.Abs, r=["sc0"], w=["sc1"])
        B.act(fl(ax), fl(ax), AF.Exp, scale=-1.0, r=["sc1"], w=["sc1"])
        B.act(fl(ax), fl(ax), AF.Ln, bias=1.0, r=["sc1"], w=["sc1"])
        B.ts("dve", fl(mx), fl(xb), 0.0, ALU.max, r=["sc0"], w=["sc2"])
        B.tt("dve", fl(dt_a), fl(mx), fl(ax), ALU.add, r=["sc1", "sc2"], w=["dt_a"])
        B.tt("dve", a_a, dt_a, Abc[:, :].unsqueeze(1).to_broadcast([128, NT, 12]), ALU.mult, r=["dt_a", "Abc"], w=["a_a"])
        for t in range(NT):
            B.mm(ps[:, 1, t * 12:t * 12 + 6], [(cst[:, 0, :], a_a[:, t, 0:6])], r=["ssdc", "a_a"], w=[("ps", 1)])
            B.mm(ps[:, 1, t * 12 + 6:t * 12 + 12], [(cst[:, 1, :], a_a[:, t, 6:12])], r=["ssdc", "a_a"], w=[("ps", 1)])
            B.mm(ps[:, 2, t * 12:(t + 1) * 12], [(C.onesf[:, :], a_a[:, t, :])], r=["onesf", "a_a"], w=[("ps", 2)])
        B.copy("act", fl(cum_a), ps[:, 1, 0:NT * 12], r=[("ps", 1)], w=["cum_a"])
        B.copy("act", fl(ect_all[:, :, :]), ps[:, 2, 0:NT * 12], r=[("ps", 2)], w=["ect"])
        B.tt("dve", fl(dtw_a), fl(ect_all[:, :, :]), fl(cum_a), ALU.subtract, r=["ect", "cum_a"], w=["dtw_a"])
        B.act(fl(dtw_a), fl(dtw_a), AF.Exp, r=["dtw_a"], w=["dtw_a"])
        B.tt("dve", fl(dtw_a), fl(dtw_a), fl(dt_a), ALU.mult, r=["dtw_a", "dt_a"], w=["dtw_a"])
        B.act(fl(ect_all[:, :, :]), fl(ect_all[:, :, :]), AF.Exp, r=["ect", "dtw_a"], w=["ect"])
        P.barrier()

        def prep(t, full):
            k = st_i[0] % 2
            st_i[0] += 1
            tgt = tg_of(t)
            av = a_a[:, t, :]
            bk = poolP.next()
            psb = ps[:, bk, :].bitcast(BF16)

            def tr(e, t=t, psb=psb):
                ins = None
                for c in range(3):
                    ins = e.transpose(out=psb[:, c * 128:(c + 1) * 128], in_=xsT[:, c, t * 128:(t + 1) * 128], identity=C.identb[:, :])
                for g in range(2):
                    ins = e.transpose(out=psb[:, 384 + g * 128:384 + (g + 1) * 128], in_=BT[:, g, t * 128:(t + 1) * 128], identity=C.identb[:, :])
                return ins
            P.add("pe", tr, r=[("u", ci, tgt) for ci in range(5)] + ["identb"], w=[("ps", bk)])
            B.copy("act", xtok[:, k, :, :].rearrange("p h d -> p (h d)"), psb[:, 0:384], r=[("ps", bk)], w=[("xtok", k)])
            B.copy("act", Btok[:, k, :, :].rearrange("p g n -> p (g n)"), psb[:, 384:640], r=[("ps", bk)], w=[("Btok", k)])
            dirs = (0, 1) if full else (1,)
            for d in dirs:
                B.tt("pool", Xw[:, k, d * 6:(d + 1) * 6, :], xtok[:, k, :, :],
                     dtw_a[:, t, d * 6:(d + 1) * 6].unsqueeze(2).to_broadcast([128, 6, 64]),
                     ALU.mult, r=[("xtok", k), "dtw_a"], w=[("Xw", k, d)])
            if not full:
                return k
            for d in range(2):
                B.tt("pool", Xdt[:, k, d * 6:(d + 1) * 6, :], xtok[:, k, :, :],
                     dt_a[:, t, d * 6:(d + 1) * 6].unsqueeze(2).to_broadcast([128, 6, 64]),
                     ALU.mult, r=[("xtok", k), "dt_a"], w=[("Xdt", k, d)])
            return k

        def prep2(t):
            tgt = tg_of(t)
            av = a_a[:, t, :]
            bg = 3
            for g in range(2):
                B.mm(ps[:, bg, g * 128:(g + 1) * 128], [(BT[:, g, t * 128:(t + 1) * 128], CT[:, g, t * 128:(t + 1) * 128])],
                     r=[("u", 3 + g, tgt), ("u", 5 + g, tgt)], w=[("ps", bg)])
            for d in range(2):
                B.tt("dve", AT[:, :, :], cst[:, d, :].unsqueeze(1).to_broadcast([128, 6, 128]),
                     av[:, d * 6:(d + 1) * 6].unsqueeze(2).to_broadcast([128, 6, 128]), ALU.mult, r=["ssdc", "a_a"], w=["AT"])
                for g in range(2):
                    hd0 = d * 6 + g * 3
                    bc = poolP.next()
                    B.mm(ps[:, bc, 0:384], [(C.onesf[:, :], AT[:, g * 3:(g + 1) * 3, :].rearrange("p h l -> p (h l)"))],
                         r=["onesf", "AT"], w=[("ps", bc)])
                    cr = ps[:, bc, 0:384].rearrange("p (h l) -> p h l", h=3)
                    m = 0
                    for i3 in range(3):
                        B.stt(Dm[:, m, i3, :], cr[:, i3, :], cum_a[:, t, hd0 + i3:hd0 + i3 + 1], cst[:, 2 + d, :], ALU.subtract, ALU.add,
                              r=[("ps", bc), "cum_a", "ssdc"], w=[("Dm", m)])
                    B.act(E[:, g, :, :], Dm[:, m, :, :], AF.Exp, r=[("Dm", m)], w=[("E", g)])
                    B.act(E0[:, g, :, :], cr, AF.Exp, r=[("ps", bc)], w=[("E0", g)])
                    for i3 in range(3):
                        B.tt("dve", MT[:, 0, hd0 + i3, :], E[:, g, i3, :], ps[:, bg, g * 128:(g + 1) * 128], ALU.mult,
                             r=[("E", g), ("ps", bg)], w=[("MT", 0, hd0)])
                    B.tt("dve", CTp[:, 0, hd0:hd0 + 3, :], E0[:, g, :, :],
                         CT[:, g, t * 128:(t + 1) * 128].unsqueeze(1).to_broadcast([128, 3, 128]), ALU.mult,
                         r=[("E0", g), ("u", 5 + g, tgt)], w=[("CTp", 0, hd0)])

        def state_update(d, k, t):
            bh = poolP.next()
            for h in range(6):
                B.mm(ps[:, bh, h * 64:(h + 1) * 64], [(Btok[:, k, h // 3, :], Xw[:, k, d * 6 + h, :])],
                     r=[("Btok", k), ("Xw", k, d)], w=[("ps", bh)])
            B.tt("pool", H32[:, d, :, :], H32[:, d, :, :], ect_all[:, t, d * 6:(d + 1) * 6].unsqueeze(2).to_broadcast([128, 6, 64]), ALU.mult,
                 r=[("H32", d), "ect"], w=[("H32", d)])
            B.tt("dve", H32[:, d, :, :], H32[:, d, :, :], ps[:, bh, 0:384].rearrange("p (h d) -> p h d", h=6), ALU.add,
                 r=[("H32", d), ("ps", bh)], w=[("H32", d)])

        B.memset("pool", H32[:, :, :, :], 0.0, w=[("H32", 0), ("H32", 1)])
        border = [1, 0] + list(range(NT - 1, 1, -1))
        kn = prep(border[0], False)
        for i, t in enumerate(border):
            k2 = t % 2
            B.copy("act", Hbin[:, k2, :, :], H32[:, 1, :, :], r=[("H32", 1)], w=[("Hbin", k2)])
            B.dma("sp", hb_scr[t, :, :], Hbin[:, k2, :, :].rearrange("p h d -> p (h d)"), r=[("Hbin", k2)], w=[("hbscr", t)])
            if t == 2:
                break
            k = kn
            if border[i + 1] != 2:
                kn = prep(border[i + 1], False)
            state_update(1, k, t)
        kn = prep(0, True)
        prep2(0)
        for t in range(NT):
            need_y = t in tiles
            k = kn
            if t + 1 < NT:
                kn = prep(t + 1, True)
            if need_y:
                k2 = t % 2
                B.dma("sp", Hbin[:, k2, :, :].rearrange("p h d -> p (h d)"), hb_scr[t, :, :], r=[("hbscr", t)], w=[("Hbin", k2)])
                B.copy("act", H16[:, :, :], H32[:, 0, :, :], r=[("H32", 0)], w=["H16"])
                by = 7
                for h in range(6):
                    B.mm(ps[:, by, h * 64:(h + 1) * 64],
                         [(MT[:, 0, h, :], Xdt[:, k, h, :]), (CTp[:, 0, h, :], H16[:, h, :]),
                          (MT[:, 0, 6 + h, :], Xdt[:, k, 6 + h, :]), (CTp[:, 0, 6 + h, :], Hbin[:, k2, h, :])],
                         r=[("MT", 0, (h // 3) * 3), ("MT", 0, 6 + (h // 3) * 3), ("CTp", 0, (h // 3) * 3), ("CTp", 0, 6 + (h // 3) * 3),
                            ("Xdt", k, 0), ("Xdt", k, 1), "H16", ("Hbin", k2)], w=[("ps", by)])
            if t + 1 < NT:
                prep2(t + 1)
            if need_y:
                B.tt("pool", tmp32[:, :].rearrange("p (h d) -> p h d", h=6), xtok[:, k, :, :],
                     C.srow[:, SR_DSKIP:SR_DSKIP + 6].unsqueeze(2).to_broadcast([128, 6, 64]), ALU.mult, r=[("xtok", k), "srow"], w=["tmp32"])
                B.tt("dve", yb32[:, :], ps[:, by, 0:384], tmp32[:, :], ALU.add, r=[("ps", by), "tmp32"], w=["yb32"])
                bz = tok_proj(C, li, t, 0, 384, wz)
                B.act(sz[:, :], ps[:, bz, 0:384], AF.Silu, r=[("ps", bz)], w=["tmp32"])
                B.tt("pool", yb32[:, :], yb32[:, :], sz[:, :], ALU.mult, r=["yb32", "tmp32"], w=["yb32"])
                B.act(tmp32[:, :], yb32[:, :], AF.Square, accum_out=ssq[:, 0:1], r=["yb32"], w=["tmp32", "ssq"])
                B.act(ssq[:, 1:2], ssq[:, 0:1], AF.Sqrt, scale=1.0 / 384, bias=EPS, r=["ssq"], w=["ssq"])
                P.add("dve", lambda e_: e_.reciprocal(out=ssq[:, 1:2], in_=ssq[:, 1:2]), r=["ssq"], w=["ssq"])
                B.stt(yb16[:, k2, :], yb32[:, :], ssq[:, 1:2], C.srow[:, SR_SNW:SR_SNW + 384], ALU.mult, ALU.mult,
                      r=["yb32", "ssq", "srow"], w=[("yb16", k2)])
                if "mixB%d" % li in C.dbg:
                    d_ = dbg_out(C, "mixB%d" % li, 384)
                    B.dma("sp", d_[t * 128:(t + 1) * 128, :], yb16[:, k2, :], r=[("yb16", k2)])
                out_proj_tile(C, t, yb16[:, k2, :], 3, wo, ("yb16", k2))
            if t < NT - 1:
                state_update(0, k, t)
        P.barrier()


_NC_CACHE = {}


def kernel(**inputs):
    inp = {k: np.asarray(v) for k, v in inputs.items()}
    if "nc" not in _NC_CACHE:
        nc = bass.Bass("TRN2", target_bir_lowering=False)
        build_program(nc, nlayers=DEPTH)
        _NC_CACHE["nc"] = nc
    nc = _NC_CACHE["nc"]
    shared = _shared_inputs(inp)
    in_maps = [host_inputs(inp, b, shared) for b in range(8)]
    res = run_bass_kernel_spmd(nc, in_maps, core_ids=list(range(8)))
    out = np.stack([np.asarray(r["out"], dtype=np.float32) for r in res.results], axis=0)
    return out
```

```python
import contextlib
import math
import numpy as np
import ml_dtypes
import concourse.bass as bass
import concourse.mybir as mybir
from concourse.bass_utils import run_bass_kernel_spmd

F32, BF16 = mybir.dt.float32, mybir.dt.bfloat16
AF = mybir.ActivationFunctionType
ALU = mybir.AluOpType
AX = mybir.AxisListType

D = 1024
NT = 18
T = NT * 128
KC = 8
DEPTH = 4
DFF = 2816
FG = 256
NFG = DFF // FG
NEXP = 8
ALPHA = (2 * DEPTH) ** 0.25
EPS = 1e-6
TGS = [(0, 256), (256, 512), (768, 512), (1280, 512), (1792, 512)]

O_QA, O_KA, O_VA, O_Z, O_XBC, O_DTF, O_DTB, O_QC, O_KC, O_VC = 0, 384, 512, 640, 1024, 1920, 1926, 1932, 2188, 2444


class Op:
    __slots__ = ("idx", "eng", "fn", "dma", "deps", "sig", "cnt", "dsem", "dval", "dprev")

    def __init__(self, idx, eng, fn, dma):
        self.idx, self.eng, self.fn, self.dma = idx, eng, fn, dma
        self.deps = set()
        self.sig = False
        self.cnt = 0
        self.dsem = None
        self.dval = 0
        self.dprev = 0


class Prog:
    ENGS = ("pe", "act", "dve", "pool", "sp")
    NDS = 20
    SEM_CAP = 30000

    def __init__(self, nc):
        self.nc = nc
        self.ops = []
        self.lastw = {}
        self.rd = {}
        self.bar_from = 0

    def add(self, eng, fn, r=(), w=(), dma=False):
        op = Op(len(self.ops), eng, fn, dma)
        deps = set()
        for t in r:
            lw = self.lastw.get(t)
            if lw is not None:
                deps.add(lw)
        for t in w:
            lw = self.lastw.get(t)
            if lw is not None:
                deps.add(lw)
            for o in self.rd.get(t, ()):
                deps.add(o)
        deps.discard(op)
        op.deps = deps
        for t in r:
            self.rd.setdefault(t, set()).add(op)
        for t in w:
            self.lastw[t] = op
            self.rd[t] = set()
        self.ops.append(op)
        return op

    def barrier(self):
        last = {}
        dmas = []
        for op in self.ops[self.bar_from:]:
            if op.dma:
                dmas.append(op)
            elif op.fn is not None:
                last[op.eng] = op
        self.bar_from = len(self.ops)
        for e in self.ENGS:
            op = Op(len(self.ops), e, None, False)
            op.deps = set(last.values()) | set(dmas)
            self.ops.append(op)

    def emit(self):
        nc = self.nc
        ops = self.ops
        for op in ops:
            for d in op.deps:
                if d.dma:
                    continue
                if d.eng == op.eng and op.eng in ("pe", "sp") and not op.dma and op.fn is not None:
                    continue
                d.sig = True
        cnt = {e: 0 for e in self.ENGS}
        for op in ops:
            if op.sig and not op.dma:
                cnt[op.eng] += 1
                op.cnt = cnt[op.eng]
        dcount = {"sp": 0, "pool": 0}
        duse = {}
        for op in ops:
            if op.dma:
                k = dcount[op.eng] % self.NDS
                dcount[op.eng] += 1
                key = (op.eng, k)
                op.dsem = key
                op.dprev = duse.get(key, 0)
                op.dval = op.dprev + 16
                duse[key] = op.dval
        with contextlib.ExitStack() as st:
            esems = {}
            for e in self.ENGS:
                n = cnt[e] // self.SEM_CAP + 1
                esems[e] = [st.enter_context(nc.semaphore(f"se_{e}_{i}")) for i in range(n)]
            dsems = {}
            for q in ("sp", "pool"):
                for k in range(min(self.NDS, dcount[q])):
                    dsems[(q, k)] = st.enter_context(nc.semaphore(f"sd_{q}_{k}"))
            block = st.enter_context(nc.Block())
            cap = self.SEM_CAP

            def run(engname, eh):
                waited = {}

                def wait(sem_key, sem, val):
                    if waited.get(sem_key, 0) >= val:
                        return
                    waited[sem_key] = val
                    eh.wait_ge(sem, val)

                for op in ops:
                    if op.eng != engname:
                        continue
                    for d in sorted(op.deps, key=lambda o: o.idx):
                        if d.dma:
                            wait(("d",) + d.dsem, dsems[d.dsem], d.dval)
                        else:
                            if d.eng == engname and engname in ("pe", "sp") and not op.dma and op.fn is not None:
                                continue
                            si, sv = divmod(d.cnt - 1, cap)
                            wait(("e", d.eng, si), esems[d.eng][si], sv + 1)
                    if op.fn is None:
                        continue
                    if op.dma:
                        if op.dprev > 0:
                            wait(("d",) + op.dsem, dsems[op.dsem], op.dprev)
                        ins = op.fn(eh)
                        ins.then_inc(dsems[op.dsem], 16)
                    else:
                        ins = op.fn(eh)
                        if op.sig:
                            si, sv = divmod(op.cnt - 1, cap)
                            ins.then_inc(esems[engname][si], 1)
                if engname == "sp":
                    for key, v in duse.items():
                        wait(("d",) + key, dsems[key], v)

            @block.tensor
            def _(t):
                run("pe", t)

            @block.scalar
            def _(s):
                run("act", s)

            @block.vector
            def _(v):
                run("dve", v)

            @block.gpsimd
            def _(g):
                run("pool", g)

            @block.sync
            def _(s):
                run("sp", s)


class PsumPool:
    def __init__(self, banks):
        self.banks = list(banks)
        self.i = 0

    def next(self):
        b = self.banks[self.i % len(self.banks)]
        self.i += 1
        return b


class Builder:
    def __init__(self, nc, dbg=None, nlayers=DEPTH):
        self.nc = nc
        self.P = Prog(nc)
        self.dbg = dbg or {}
        self.nlayers = nlayers
        self.uid = 0

    def tok(self, name):
        self.uid += 1
        return (name, self.uid)

    def dram_in(self, name, shape, dt=F32):
        return self.nc.dram_tensor(name, list(shape), dt, kind="ExternalInput").ap()

    def dram_out(self, name, shape, dt=F32):
        return self.nc.dram_tensor(name, list(shape), dt, kind="ExternalOutput").ap()

    def dma(self, q, out, in_, r=(), w=()):
        return self.P.add(q, lambda e: e.dma_start(out=out, in_=in_), r, w, dma=True)

    def mm(self, out, pairs, r=(), w=(), tile_position=None, start=True):
        def fn(e):
            n = len(pairs)
            ins = None
            for i, (l, rh) in enumerate(pairs):
                kw = {}
                if tile_position is not None:
                    kw["tile_position"] = tile_position
                ins = e.matmul(out, lhsT=l, rhs=rh, start=(start and i == 0), stop=(i == n - 1), **kw)
            return ins
        return self.P.add("pe", fn, r, w)

    def act(self, out, in_, func, r=(), w=(), bias=None, scale=None, accum_out=None):
        def fn(e):
            kw = {}
            if bias is not None:
                kw["bias"] = bias
            if scale is not None:
                kw["scale"] = scale
            if accum_out is not None:
                kw["accum_out"] = accum_out
            return e.activation(out=out, in_=in_, func=func, **kw)
        return self.P.add("act", fn, r, w)

    def tt(self, eng, out, in0, in1, op, r=(), w=()):
        return self.P.add(eng, lambda e: e.tensor_tensor(out=out, in0=in0, in1=in1, op=op), r, w)

    def ts(self, eng, out, in0, s1, op0, s2=None, op1=None, r=(), w=()):
        def fn(e):
            if op1 is None:
                return e.tensor_scalar(out=out, in0=in0, scalar1=s1, scalar2=None, op0=op0)
            return e.tensor_scalar(out=out, in0=in0, scalar1=s1, scalar2=s2, op0=op0, op1=op1)
        return self.P.add(eng, fn, r, w)

    def stt(self, out, in0, scalar, in1, op0, op1, r=(), w=()):
        return self.P.add("dve", lambda e: e.scalar_tensor_tensor(out=out, in0=in0, scalar=scalar, in1=in1,
                                                                   op0=op0, op1=op1), r, w)

    def copy(self, eng, out, in_, r=(), w=()):
        if eng == "act":
            return self.P.add("act", lambda e: e.copy(out=out, in_=in_), r, w)
        return self.P.add(eng, lambda e: e.tensor_copy(out=out, in_=in_), r, w)

    def memset(self, eng, ap, val, w=()):
        return self.P.add(eng, lambda e: e.memset(ap, val), (), w)


class Ctx:
    pass


def declare_inputs(B):
    I = {}
    I["xin"] = B.dram_in("xin", [T, D])
    I["cvec"] = B.dram_in("cvec", [128, KC, 2])
    I["ident"] = B.dram_in("ident", [128, 128])
    I["w_ada"] = B.dram_in("w_ada", [DEPTH, D, 6 * D])
    I["b_adaT"] = B.dram_in("b_adaT", [DEPTH, 128, 48])
    I["lnrows"] = B.dram_in("lnrows", [DEPTH, 4, 128, D])
    I["ffn_gu"] = B.dram_in("ffn_gu", [2, NFG, 2, 128, KC, FG])
    I["ffn_dn"] = B.dram_in("ffn_dn", [2, DFF, D])
    I["exp_gu"] = B.dram_in("exp_gu", [2, NEXP, NFG, 2, 128, KC, FG])
    I["exp_dn"] = B.dram_in("exp_dn", [2, NEXP, DFF, D])
    I["router_w"] = B.dram_in("router_w", [2, 128, KC, NEXP])
    I["router_b"] = B.dram_in("router_b", [2, 128, NEXP])
    I["w_fm"] = B.dram_in("w_fm", [DEPTH, NCH, 128, KC, 128])
    I["w_tok"] = B.dram_in("w_tok", [DEPTH, 128, KC, NTK])
    I["w_out"] = B.dram_in("w_out", [DEPTH, D, D])
    I["srow"] = B.dram_in("srow", [DEPTH, 128, NSR])
    I["convp"] = B.dram_in("convp", [DEPTH, 128, 7, 6])
    I["ropeA"] = B.dram_in("ropeA", [2, 128, T], BF16)
    I["ropeC"] = B.dram_in("ropeC", [2, 128, T], BF16)
    I["bmask"] = B.dram_in("bmask", [128, 3, 128], BF16)
    I["ssdc"] = B.dram_in("ssdc", [4, 128, 128])
    I["bandm"] = B.dram_in("bandm", [128, 6])
    return I


def build_program(nc, nlayers=DEPTH, dbg=(), inject_x1=False):
    B = Builder(nc)
    P = B.P
    I = declare_inputs(B)
    out_d = B.dram_out("out", [2048, D])
    dbg_d = {}
    st = contextlib.ExitStack()
    sb = lambda name, shape, dt=F32: st.enter_context(nc.sbuf_tensor(name, list(shape), dt))
    C = Ctx()
    C.B, C.P, C.I, C.nc = B, P, I, nc
    C.x = sb("x", [128, NT, D])
    C.xT = sb("xT", [128, KC, T], BF16)
    C.identf = sb("identf", [128, 128])
    C.identb = sb("identb", [128, 128], BF16)
    C.onesf = sb("onesf", [128, 128])
    C.cact = sb("cact", [128, KC, 2])
    C.adaT = sb("adaT", [128, 48, 2])
    C.scp = sb("scp", [128, 2, KC, 2])
    C.mv = sb("mv", [128, NT, 2])
    C.rstd = sb("rstd", [128, NT])
    C.nmr = sb("nmr", [128, NT])
    C.bnst = sb("bnst", [128, NT, 2, 6])
    C.psum = st.enter_context(nc.psum_tensor("psum", [128, 8, 512], F32))
    ps = C.psum

    xin_v = I["xin"].rearrange("(t p) d -> p t d", p=128)
    for t in range(NT):
        B.dma("sp", C.x[:, t, :], xin_v[:, t, :], w=[("x", t)])
    B.dma("sp", C.identf[:, :], I["ident"][:, :], w=["identf"])
    B.dma("sp", C.cact[:, :, :], I["cvec"][:, :, :], w=["cact"])
    B.copy("dve", C.identb[:, :], C.identf[:, :], r=["identf"], w=["identb"])
    B.memset("pool", C.onesf[:, :], 1.0, w=["onesf"])
    B.act(C.cact[:, :, :], C.cact[:, :, :], AF.Silu, r=["cact"], w=["cact"])
    for t in range(NT):
        B.ts("pool", C.x[:, t, :], C.x[:, t, :], ALPHA, ALU.mult, r=[("x", t)], w=[("x", t)])

    C.nlayers = nlayers
    C.dbg = dbg
    C.dbgd = {}
    for li in range(nlayers):
        last = (li == DEPTH - 1)
        tiles = list(range(2, NT)) if last else list(range(NT))
        mk = lambda sc, tag, li=li: (lambda name, shape, dt=F32: sc.enter_context(nc.sbuf_tensor("%s_%s%d" % (name, tag, li), list(shape), dt)))
        with contextlib.ExitStack() as sc:
            C.sb = mk(sc, "m")
            C.dg = C.sb("dg", [128, 2, 128])
            with contextlib.ExitStack() as sc2:
                C.sb2 = mk(sc2, "m0")
                C.xn = C.sb2("xn", [128, 2, D], BF16)
                ada_params(C, li)
                ln_modulate(C, which_sc=0, sh_base=0)
                P.barrier()
            if "xT%d" % li in dbg:
                d = B.dram_out("dbg_xT%d" % li, [128, KC, T], BF16)
                B.dma("sp", d[:, :, :], C.xT[:, :, :], r=[("xT", t) for t in range(NT)])
            if inject_x1:
                d = B.dram_in("dbg_x1_%d" % li, [T, D])
                dv = d.rearrange("(t p) d -> p t d", p=128)
                for t in range(NT):
                    B.dma("sp", C.x[:, t, :], dv[:, t, :], w=[("x", t)])
            else:
                mixer_phase(C, li, tiles)
                with contextlib.ExitStack() as sc2:
                    C.rows = mk(sc2, "m9")("lnrows", [128, 2, D])
                    post_ln(C, li, 0, tiles, out_scale=ALPHA)
                    P.barrier()
            if "x1_%d" % li in dbg:
                dump_x(C, "dbg_x1_%d" % li)
            P.barrier()
        with contextlib.ExitStack() as sc:
            C.sb = lambda name, shape, dt=F32, li=li, sc=sc: sc.enter_context(nc.sbuf_tensor("%s_f%d" % (name, li), list(shape), dt))
            C.xn = C.sb("xn", [128, 2, D], BF16)
            C.rows = C.sb("lnrows", [128, 2, D])
            C.dg = C.sb("dg", [128, 2, 128])
            ffn_phase(C, li, tiles)
            if "xpre%d" % li in dbg:
                dump_x(C, "dbg_xpre%d" % li)
            post_ln(C, li, 1, tiles, out_scale=(1.0 if li == nlayers - 1 else ALPHA))
            if "x2_%d" % li in dbg:
                dump_x(C, "dbg_x2_%d" % li)
            P.barrier()
    ov = out_d.rearrange("(t p) d -> p t d", p=128)
    for t in range(2, NT):
        B.dma("sp", ov[:, t - 2, :], C.x[:, t, :], r=[("x", t)])
    P.emit()
    st.close()
    return nc


def dbg_out(C, name, w):
    if name not in C.dbgd:
        C.dbgd[name] = C.B.dram_out("dbg_" + name, [T, w], BF16)
    return C.dbgd[name]


def dump_x(C, name):
    d = C.B.dram_out(name, [T, D])
    dv = d.rearrange("(t p) d -> p t d", p=128)
    for t in range(NT):
        C.B.dma("sp", dv[:, t, :], C.x[:, t, :], r=[("x", t)])


def ada_params(C, li):
    B, nc, I, ps = C.B, C.nc, C.I, C.psum
    wb = C.sb2("adaw", [128, 2, KC, 256])
    bT = C.sb2("adab", [128, 48])
    B.dma("sp", bT[:, :], I["b_adaT"][li, :, :], w=["adab"])
    wv = I["w_ada"][li].rearrange("(kc p) f -> p kc f", p=128)
    bank = 7
    for piece in range(24):
        s = piece % 2
        B.dma("sp", wb[:, s, :, :], wv[:, :, piece * 256:(piece + 1) * 256], w=[("adaw", s)])
        for j in range(2):
            fc = piece * 2 + j
            B.mm(ps[:, bank, fc * 2:fc * 2 + 2],
                 [(wb[:, s, kc, j * 128:(j + 1) * 128], C.cact[:, kc, :]) for kc in range(KC)],
                 r=[("adaw", s), "cact"], w=[("ps", bank)])
    B.tt("dve", C.adaT[:, :, :], ps[:, bank, 0:96].rearrange("p (f w) -> p f w", w=2),
         bT[:, :].unsqueeze(2).to_broadcast([128, 48, 2]), ALU.add,
         r=[("ps", bank), "adab"], w=["adaT"])
    B.ts("pool", C.scp[:, 0, :, :], C.adaT[:, 8:16, :], 1.0, ALU.add, r=["adaT"], w=["scp"])
    B.ts("pool", C.scp[:, 1, :, :], C.adaT[:, 32:40, :], 1.0, ALU.add, r=["adaT"], w=["scp"])


def ln_stats(C, tiles, eps=EPS):
    B = C.B
    for t in tiles:
        for h in range(2):
            C.P.add("dve", lambda e, t=t, h=h: e.bn_stats(out=C.bnst[:, t, h, :], in_=C.x[:, t, h * 512:(h + 1) * 512]),
                    r=[("x", t), ("x", t, h)], w=[("bnst", t)])
        C.P.add("dve", lambda e, t=t: e.bn_aggr(out=C.mv[:, t, :], in_=C.bnst[:, t, :, :].rearrange("p a b -> p (a b)")),
                r=[("bnst", t)], w=[("mv", t)])
    t0, t1 = tiles[0], tiles[-1] + 1
    B.act(C.rstd[:, t0:t1], C.mv[:, t0:t1, 1], AF.Sqrt, bias=eps, r=[("mv", t) for t in tiles], w=[("rstd", t) for t in tiles])
    C.P.add("dve", lambda e: e.reciprocal(out=C.rstd[:, t0:t1], in_=C.rstd[:, t0:t1]),
            r=[("rstd", t) for t in tiles], w=[("rstd", t) for t in tiles])


def ln_modulate(C, which_sc, sh_base, ctx_tiles=True):
    B, nc, ps = C.B, C.nc, C.psum
    tiles = list(range(NT)) if ctx_tiles else list(range(2, NT))
    ln_stats(C, tiles, eps=EPS * ALPHA * ALPHA)
    if True:
        xn = C.xn
        pool = PsumPool([0, 1])
        for i, t in enumerate(tiles):
            s = i % 2
            wch = 1 if t < 2 else 0
            B.ts("dve", xn[:, s, :], C.x[:, t, :], C.mv[:, t, 0:1], ALU.subtract, C.rstd[:, t:t + 1], ALU.mult,
                 r=[("x", t), ("mv", t), ("rstd", t)], w=[("xn", s)])
            bk = pool.next()
            psb = ps[:, bk, :].bitcast(BF16)

            def tr(e, s=s, psb=psb):
                ins = None
                for kc in range(KC):
                    ins = e.transpose(out=psb[:, kc * 128:(kc + 1) * 128], in_=xn[:, s, kc * 128:(kc + 1) * 128],
                                      identity=C.identb[:, :])
                return ins
            C.P.add("pe", tr, r=[("xn", s), "identb"], w=[("ps", bk)])
            for kc in range(KC):
                B.act(C.xT[:, kc, t * 128:(t + 1) * 128], psb[:, kc * 128:(kc + 1) * 128], AF.Identity,
                      scale=C.scp[:, which_sc, kc, wch:wch + 1], bias=C.adaT[:, sh_base + kc, wch:wch + 1],
                      r=[("ps", bk), "scp", "adaT"], w=[("xT", t)])


def _shared_inputs(inp):
    f32 = np.float32
    S = {}
    S["ident"] = np.eye(128, dtype=f32)
    S["w_ada"] = np.ascontiguousarray(inp["w_ada"], dtype=f32)
    S["b_adaT"] = np.ascontiguousarray(inp["b_ada"].reshape(DEPTH, 48, 128).transpose(0, 2, 1), dtype=f32)
    ln = np.stack([inp["ln1_g"], inp["ln1_b"], inp["ln2_g"], inp["ln2_b"]], axis=1)
    S["lnrows"] = np.ascontiguousarray(np.broadcast_to(ln[:, :, None, :], (DEPTH, 4, 128, D)), dtype=f32)
    g = inp["ffn_w_gu"].reshape(2, KC, 128, 2, NFG, FG)
    S["ffn_gu"] = np.ascontiguousarray(g.transpose(0, 4, 3, 2, 1, 5), dtype=f32)
    S["ffn_dn"] = np.ascontiguousarray(inp["ffn_w_down"], dtype=f32)
    g = inp["exp_w_gu"].reshape(2, NEXP, KC, 128, 2, NFG, FG)
    S["exp_gu"] = np.ascontiguousarray(g.transpose(0, 1, 5, 4, 3, 2, 6), dtype=f32)
    S["exp_dn"] = np.ascontiguousarray(inp["exp_w_down"], dtype=f32)
    S["router_w"] = np.ascontiguousarray(inp["router_w"].reshape(2, KC, 128, NEXP).transpose(0, 2, 1, 3), dtype=f32)
    S["router_b"] = np.ascontiguousarray(np.broadcast_to(inp["router_b"][:, None, :], (2, 128, NEXP)), dtype=f32)
    i128 = np.arange(128)

    def rot(i, hd):
        d = i % hd
        return (i // hd) * hd + np.where(d < hd // 2, d + hd // 2, d - hd // 2)
    cols = []
    for c in range(3):
        cols.append(O_QA + c * 128 + i128)
    for c in range(3):
        cols.append(O_QA + c * 128 + rot(i128, 64))
    for g in range(2):
        cols.append(O_KA + g * 64 + (i128 % 64))
    for g in range(2):
        cols.append(O_KA + g * 64 + rot(i128 % 64, 64))
    for base in (O_QC, O_KC):
        for c in range(2):
            cols.append(base + c * 128 + i128)
        for c in range(2):
            cols.append(base + c * 128 + rot(i128, 32))
    for ci in range(7):
        cols.append(O_XBC + ci * 128 + i128)
    cols = np.stack(cols)
    w_in = inp["w_in"]
    wf = w_in[:, :, cols]
    S["w_fm"] = np.ascontiguousarray(wf.reshape(DEPTH, KC, 128, NCH, 128).transpose(0, 3, 2, 1, 4), dtype=f32)
    tcols = np.concatenate([O_VA + np.arange(128), O_VC + np.arange(256), O_DTF + np.arange(12), O_Z + np.arange(384)])
    wt = w_in[:, :, tcols]
    S["w_tok"] = np.ascontiguousarray(wt.reshape(DEPTH, KC, 128, NTK).transpose(0, 2, 1, 3), dtype=f32)
    S["w_out"] = np.ascontiguousarray(inp["w_out"], dtype=f32)
    sr = np.concatenate([inp["attn_sink"], inp["dt_bias"].reshape(DEPTH, 12), inp["a_log"].reshape(DEPTH, 12), inp["d_skip"],
                         inp["lam_q"].reshape(DEPTH, 64), inp["lam_k"].reshape(DEPTH, 64), inp["diff_norm_w"], inp["ssm_norm_w"]], axis=1)
    S["srow"] = np.ascontiguousarray(np.broadcast_to(sr[:, None, :], (DEPTH, 128, NSR)), dtype=f32)
    cw = np.concatenate([inp["conv_w"], inp["conv_b"][:, None, :]], axis=1)
    S["convp"] = np.ascontiguousarray(cw.reshape(DEPTH, 6, 7, 128).transpose(0, 3, 2, 1), dtype=f32)
    tt = np.arange(2048)
    rows, colsg = (tt // 64).astype(f32), (tt % 64).astype(f32)

    def rope_tab(hd):
        quarter = hd // 4
        inv = (10000.0 ** (-np.arange(quarter, dtype=f32) / quarter)).astype(f32)
        ang = np.concatenate([rows[:, None] * inv, colsg[:, None] * inv], axis=-1).astype(f32)
        d = i128 % hd
        jj = d % (hd // 2)
        sign = np.where(d < hd // 2, -1.0, 1.0).astype(f32)
        cos = np.ones((128, T), f32)
        sin = np.zeros((128, T), f32)
        cos[:, 256:] = np.cos(ang).astype(f32)[:, jj].T
        sin[:, 256:] = np.sin(ang).astype(f32)[:, jj].T * sign[:, None]
        return np.stack([cos, sin]).astype(ml_dtypes.bfloat16)
    S["ropeA"] = rope_tab(64)
    S["ropeC"] = rope_tab(32)
    kk, qq = np.meshgrid(np.arange(128), np.arange(128), indexing="ij")
    S["bmask"] = np.stack([(qq <= kk), np.ones_like(kk, bool), (qq >= kk)], axis=1).astype(f32).astype(ml_dtypes.bfloat16)
    jj, ll = np.meshgrid(np.arange(128), np.arange(128), indexing="ij")
    pp = np.arange(128)
    S["bandm"] = np.stack([(pp // 32 == 0), (pp // 32 == 1), (pp // 32 == 2), (pp // 32 == 3), (pp // 64 == 0), (pp // 64 == 1)], axis=1).astype(f32)
    S["ssdc"] = np.stack([(jj <= ll).astype(f32), (jj >= ll).astype(f32),
                          np.where(ll >= jj, 0.0, -30000.0).astype(f32), np.where(ll <= jj, 0.0, -30000.0).astype(f32)]).astype(f32)
    return S


def host_inputs(inp, b, shared=None):
    f32 = np.float32
    m = dict(shared if shared is not None else _shared_inputs(inp))
    m["xin"] = np.ascontiguousarray(np.concatenate([inp["ctx"][b], inp["x"][b]], axis=0), dtype=f32)
    cc = np.stack([inp["c"][b], inp["c_ctx"]], axis=0)
    m["cvec"] = np.ascontiguousarray(cc.reshape(2, KC, 128).transpose(2, 1, 0), dtype=f32)
    return m


def bcast_rows(C, dst, src_fc0, name):
    B, nc, ps = C.B, C.nc, C.psum
    if True:
        dg = C.dg
        k = 0
        for wch in range(2):
            for half in range(2):
                bank = 6 + (k % 2)
                for q in range(4):
                    kc = half * 4 + q
                    s = (k * 4 + q) % 2
                    B.ts("dve", dg[:, s, :], C.identf[:, :], C.adaT[:, src_fc0 + kc, wch:wch + 1], ALU.mult,
                         r=["identf", "adaT"], w=[("diag", s)])
                    B.mm(ps[:, bank, q * 128:(q + 1) * 128], [(C.onesf[:, :], dg[:, s, :])],
                         r=["onesf", ("diag", s)], w=[("ps", bank)])
                B.copy("act", dst[:, wch, half * 512:(half + 1) * 512], ps[:, bank, :], r=[("ps", bank)], w=[name])
                k += 1


def post_ln(C, li, idx, tiles, out_scale=1.0):
    B, nc = C.B, C.nc
    ln_stats(C, tiles)
    if True:
        rows = C.rows
        for j in range(2):
            B.dma("sp", rows[:, j, :], C.I["lnrows"][li, 2 * idx + j, :, :], w=[("lnrow", j)])
            if out_scale != 1.0:
                B.ts("pool", rows[:, j, :], rows[:, j, :], out_scale, ALU.mult, r=[("lnrow", j)], w=[("lnrow", j)])
        t0_, t1_ = tiles[0], tiles[-1] + 1
        B.stt(C.nmr[:, t0_:t1_], C.mv[:, t0_:t1_, 0], -1.0, C.rstd[:, t0_:t1_], ALU.mult, ALU.mult,
              r=[("mv", t) for t in tiles] + [("rstd", t) for t in tiles], w=["nmr"])
        for t in tiles:
            B.act(C.x[:, t, :], C.x[:, t, :], AF.Identity, scale=C.rstd[:, t:t + 1], bias=C.nmr[:, t:t + 1],
                  r=[("x", t), ("x", t, 0), ("x", t, 1), "nmr", ("rstd", t)], w=[("x", t)])
            B.tt("dve", C.x[:, t, :], C.x[:, t, :], rows[:, 0, :], ALU.mult, r=[("x", t), ("lnrow", 0)], w=[("x", t)])
            B.tt("dve", C.x[:, t, :], C.x[:, t, :], rows[:, 1, :], ALU.add, r=[("x", t), ("lnrow", 1)], w=[("x", t)])


def ffn_phase(C, li, tiles):
    B, nc, I, ps, P = C.B, C.nc, C.I, C.psum, C.P
    moe = (li % 2 == 1)
    j = li // 2
    use_ctx = 0 in tiles
    ln_modulate(C, which_sc=1, sh_base=24, ctx_tiles=use_ctx)
    if "xTf%d" % li in C.dbg:
        d = B.dram_out("dbg_xTf%d" % li, [128, KC, T], BF16)
        B.dma("sp", d[:, :, :], C.xT[:, :, :], r=[("xT", t) for t in range(NT)])
    tgs = TGS if use_ctx else TGS[1:]
    if True:
        sbt = C.sb
        g2bc = sbt("g2bc", [128, 2, D])
        wgu = sbt("wgu", [128, 2, 2, KC, FG], BF16)
        wdf = sbt("wdf", [128, 1, 2, D])
        wdl = sbt("wdl", [128, 2, 2, D], BF16)
        wdc = sbt("wdc", [128, 2, 2, D], BF16)
        hT = sbt("hT", [128, 2, 2, T], BF16)
        sg = sbt("sg", [128, 3, 512], BF16)
        etmp = sbt("etmp", [128, 2, 512])
        evi = [0]
        bcast_rows(C, g2bc, 40, "g2bc")
        gates = None
        if moe:
            wr = sbt("wr", [128, KC, NEXP])
            wrb = sbt("wrb", [128, KC, NEXP], BF16)
            rb = sbt("rb", [128, NEXP])
            lg = sbt("lg", [128, NT, NEXP])
            mx8 = sbt("mx8", [128, NT, 8])
            mk1 = sbt("mk1", [128, NT, NEXP])
            mk2 = sbt("mk2", [128, NT, NEXP])
            gates = sbt("gates", [128, NT, NEXP])
            w12 = sbt("w12", [128, 3, NT])
            B.dma("sp", wr[:, :, :], I["router_w"][j, :, :, :], w=["wr"])
            B.dma("sp", rb[:, :], I["router_b"][j, :, :], w=["rb"])
            B.copy("dve", wrb[:, :, :], wr[:, :, :], r=["wr"], w=["wrb"])
            bank = 6
            for t in tiles:
                B.mm(ps[:, bank, t * 8:(t + 1) * 8],
                     [(C.xT[:, kc, t * 128:(t + 1) * 128], wrb[:, kc, :]) for kc in range(KC)],
                     r=[("xT", t), "wrb"], w=[("ps", bank)])
            t0, t1 = tiles[0], tiles[-1] + 1
            n = t1 - t0
            B.tt("dve", lg[:, t0:t1, :], ps[:, bank, t0 * 8:t1 * 8].rearrange("p (t e) -> p t e", e=8),
                 rb[:, :].unsqueeze(1).to_broadcast([128, n, NEXP]), ALU.add, r=[("ps", bank), "rb"], w=["lg"])
            for t in tiles:
                P.add("dve", lambda e, t=t: e.max(out=mx8[:, t, :], in_=lg[:, t, :]), r=["lg"], w=["mx8"])
            B.tt("dve", mk1[:, t0:t1, :], lg[:, t0:t1, :], mx8[:, t0:t1, 0:1].to_broadcast([128, n, NEXP]), ALU.is_equal,
                 r=["lg", "mx8"], w=["mk1"])
            B.tt("dve", mk2[:, t0:t1, :], lg[:, t0:t1, :], mx8[:, t0:t1, 1:2].to_broadcast([128, n, NEXP]), ALU.is_equal,
                 r=["lg", "mx8"], w=["mk2"])
            B.tt("dve", w12[:, 0, t0:t1], mx8[:, t0:t1, 1], mx8[:, t0:t1, 0], ALU.subtract, r=["mx8"], w=["w12a"])
            B.act(w12[:, 0, t0:t1], w12[:, 0, t0:t1], AF.Exp, r=["w12a"], w=["w12a"])
            B.ts("dve", w12[:, 1, t0:t1], w12[:, 0, t0:t1], 1.0, ALU.add, r=["w12a"], w=["w12b"])
            P.add("dve", lambda e: e.reciprocal(out=w12[:, 1, t0:t1], in_=w12[:, 1, t0:t1]), r=["w12b"], w=["w12b"])
            B.tt("dve", w12[:, 2, t0:t1], w12[:, 0, t0:t1], w12[:, 1, t0:t1], ALU.mult, r=["w12a", "w12b"], w=["w12c"])
            B.tt("dve", mk1[:, t0:t1, :], mk1[:, t0:t1, :], w12[:, 1, t0:t1].unsqueeze(2).to_broadcast([128, n, NEXP]),
                 ALU.mult, r=["mk1", "w12b"], w=["mk1"])
            B.tt("dve", mk2[:, t0:t1, :], mk2[:, t0:t1, :], w12[:, 2, t0:t1].unsqueeze(2).to_broadcast([128, n, NEXP]),
                 ALU.mult, r=["mk2", "w12c"], w=["mk2"])
            B.tt("dve", gates[:, t0:t1, :], mk1[:, t0:t1, :], mk2[:, t0:t1, :], ALU.add, r=["mk1", "mk2"], w=["gates"])
        groups = [(e, fg) for e in range(NEXP if moe else 1) for fg in range(NFG)]
        poolA = PsumPool([0, 1, 2, 3])
        poolB = PsumPool([4, 5, 6, 7])
        sgi = [0]

        def loads_gu(i):
            e, fg = groups[i]
            s = i % 2
            src_gu = I["exp_gu"][j, e, fg] if moe else I["ffn_gu"][j, fg]
            B.dma("pool", wgu[:, s, :, :, :], src_gu.rearrange("g p k f -> p g k f"), w=[("wgu", s)])

        def loads_dn(i):
            e, fg = groups[i]
            s = i % 2
            src_dn = I["exp_dn"][j, e, fg * FG:(fg + 1) * FG, :] if moe else I["ffn_dn"][j, fg * FG:(fg + 1) * FG, :]
            B.dma("sp", wdf[:, 0, :, :], src_dn.rearrange("(c p) d -> p c d", p=128), w=["wdf"])
            B.tt("pool", wdl[:, s, :, :], wdf[:, 0, :, :], g2bc[:, 0:1, :].to_broadcast([128, 2, D]), ALU.mult,
                 r=["wdf", "g2bc"], w=[("wdl", s)])
            if use_ctx:
                B.tt("pool", wdc[:, s, :, :], wdf[:, 0, :, :], g2bc[:, 1:2, :].to_broadcast([128, 2, D]), ALU.mult,
                     r=["wdf", "g2bc"], w=[("wdc", s)])

        def phaseA(i):
            s = i % 2
            for (t0, n) in tgs:
                tl = [("xT", t) for t in range(t0 // 128, (t0 + n) // 128)]
                for fc in range(2):
                    bg, bu = poolA.next(), poolA.next()
                    B.mm(ps[:, bg, 0:n], [(wgu[:, s, 0, kc, fc * 128:(fc + 1) * 128], C.xT[:, kc, t0:t0 + n]) for kc in range(KC)],
                         r=[("wgu", s)] + tl, w=[("ps", bg)])
                    B.mm(ps[:, bu, 0:n], [(wgu[:, s, 1, kc, fc * 128:(fc + 1) * 128], C.xT[:, kc, t0:t0 + n]) for kc in range(KC)],
                         r=[("wgu", s)] + tl, w=[("ps", bu)])
                    k = sgi[0] % 3
                    sgi[0] += 1
                    B.act(sg[:, k, 0:n], ps[:, bg, 0:n], AF.Silu, r=[("ps", bg)], w=[("sg", k)])
                    B.tt("dve", hT[:, s, fc, t0:t0 + n], sg[:, k, 0:n], ps[:, bu, 0:n], ALU.mult,
                         r=[("sg", k), ("ps", bu)], w=[("hT", s, fc, t0)])

        def phaseB(i):
            e, fg = groups[i]
            s = i % 2
            for t in tiles:
                wd = wdc if t < 2 else wdl
                tg0 = [t0 for (t0, n) in TGS if t0 <= t * 128 < t0 + n][0]
                for half in range(2):
                    bo = poolB.next()
                    B.mm(ps[:, bo, :], [(hT[:, s, fc, t * 128:(t + 1) * 128], wd[:, s, fc, half * 512:(half + 1) * 512])
                                        for fc in range(2)],
                         r=[("hT", s, 0, tg0), ("hT", s, 1, tg0), ("wdc" if t < 2 else "wdl", s)], w=[("ps", bo)])
                    xs = C.x[:, t, half * 512:(half + 1) * 512]
                    sc = gates[:, t, e:e + 1] if moe else 1.0
                    if half == 0:
                        B.stt(xs, ps[:, bo, :], sc, xs, ALU.mult, ALU.add,
                              r=[("ps", bo), ("x", t)] + (["gates"] if moe else []), w=[("x", t, 0)])
                    else:
                        kq = evi[0] % 2
                        evi[0] += 1
                        B.act(etmp[:, kq, :], ps[:, bo, :], AF.Identity, scale=sc,
                              r=[("ps", bo)] + (["gates"] if moe else []), w=[("etmp", kq)])
                        B.tt("pool", xs, xs, etmp[:, kq, :], ALU.add, r=[("etmp", kq), ("x", t)], w=[("x", t, 1)])

        n = len(groups)
        loads_gu(0)
        if n > 1:
            loads_gu(1)
        loads_dn(0)
        for i in range(n):
            phaseA(i)
            if i + 2 < n:
                loads_gu(i + 2)
            if i >= 1:
                phaseB(i - 1)
            if i + 1 < n:
                loads_dn(i + 1)
        phaseB(n - 1)


CH_QA, CH_QAR, CH_KA, CH_KAR = 0, 3, 6, 8
CH_QC, CH_QCR, CH_KC, CH_KCR = 10, 12, 14, 16
CH_XBC = 18
NCH = 25
MIXERS = "ACB"
B_STAGE = 9
B_SUB = 9
TK_VA, TK_VC, TK_DT, TK_Z, NTK = 0, 128, 384, 396, 780
SR_SINK, SR_DTB, SR_ALOG, SR_DSKIP, SR_LQ, SR_LK, SR_DNW, SR_SNW, NSR = 0, 6, 18, 30, 36, 100, 164, 228, 612
PADT = T + 8


def fm_chunks(C, li, ci_list, evac):
    B, ps, I = C.B, C.psum, C.I
    ws = []
    for ci in ci_list:
        s = C.wfm_i % 4
        C.wfm_i += 1
        B.dma("pool", C.wfm[:, s, :, :], I["w_fm"][li, ci, :, :, :], w=[("wfm", s)])
        ws.append(s)
    for gi, (t0, n) in enumerate(TGS):
        tl = [("xT", t) for t in range(t0 // 128, (t0 + n) // 128)]
        banks = []
        for s in ws:
            bk = C.poolF.next()
            B.mm(ps[:, bk, 0:n], [(C.wfm[:, s, kc, :], C.xT[:, kc, t0:t0 + n]) for kc in range(KC)],
                 r=[("wfm", s)] + tl, w=[("ps", bk)])
            banks.append(bk)
        evac(gi, t0, n, banks)


def rope_evac(C, dst, dtok, cos, sin):
    B, ps = C.B, C.psum

    def evac(gi, t0, n, banks):
        bx, br = banks
        if t0 < 256:
            B.copy("act", dst[:, t0:t0 + n], ps[:, bx, 0:n], r=[("ps", bx), ("ps", br)], w=[dtok + (gi,)])
            return
        k = C.rt_i % C.rt_n
        C.rt_i += 1
        B.tt("dve", C.rtmp[:, k, 0, 0:n], ps[:, bx, 0:n], cos[:, t0:t0 + n], ALU.mult, r=[("ps", bx), "rope"], w=[("rtmp", k, 0)])
        B.tt("dve", C.rtmp[:, k, 1, 0:n], ps[:, br, 0:n], sin[:, t0:t0 + n], ALU.mult, r=[("ps", br), "rope"], w=[("rtmp", k, 1)])
        B.tt("dve", dst[:, t0:t0 + n], C.rtmp[:, k, 0, 0:n], C.rtmp[:, k, 1, 0:n], ALU.add,
             r=[("rtmp", k, 0), ("rtmp", k, 1)], w=[dtok + (gi,)])
    return evac


def tg_of(t):
    return [i for i, (t0, n) in enumerate(TGS) if t0 <= t * 128 < t0 + n][0]


def out_proj_tile(C, t, y_bf, nchunk, wo, ytok):
    B, ps = C.B, C.psum
    wch = 1 if t < 2 else 0
    bk = C.poolT.next()
    psb = ps[:, bk, :].bitcast(BF16)

    def tr(e):
        ins = None
        for c in range(nchunk):
            ins = e.transpose(out=psb[:, c * 128:(c + 1) * 128], in_=y_bf[:, c * 128:(c + 1) * 128], identity=C.identb[:, :])
        return ins
    C.P.add("pe", tr, r=[ytok, "identb"], w=[("ps", bk)])
    k = C.yT_i % 2
    C.yT_i += 1
    B.copy("act", C.yT[:, k, 0:nchunk * 128], psb[:, 0:nchunk * 128], r=[("ps", bk)], w=[("yT", k)])
    for half in range(2):
        bo = C.poolO.next()
        B.mm(ps[:, bo, :], [(C.yT[:, k, c * 128:(c + 1) * 128], wo[:, c, half * 512:(half + 1) * 512]) for c in range(nchunk)],
             r=[("yT", k), "wo"], w=[("ps", bo)])
        m = 0
        B.tt("dve", C.otmp[:, m, :], ps[:, bo, :], C.g1bc[:, wch, half * 512:(half + 1) * 512], ALU.mult,
             r=[("ps", bo), "g1bc"], w=[("otmp", m)])
        xs = C.x[:, t, half * 512:(half + 1) * 512]
        B.tt("dve", xs, xs, C.otmp[:, m, :], ALU.add, r=[("otmp", m), ("x", t)], w=[("x", t)])


def mixer_phase(C, li, tiles):
    B, nc, I, ps, P = C.B, C.nc, C.I, C.psum, C.P
    lam_init = 0.8 - 0.6 * math.exp(-0.3 * li)
    C.g1bc = C.sb("g1bc", [128, 2, D])
    C.srow = C.sb("srow", [128, NSR])
    C.yT = C.sb("yT", [128, 2, 384], BF16)
    C.otmp = C.sb("otmp", [128, 1, 512])
    C.wfm_i = C.rt_i = C.yT_i = C.ot_i = 0
    C.poolF = PsumPool([0, 1, 2, 3])
    C.poolT = PsumPool([6])
    C.poolO = PsumPool([4, 5])
    bcast_rows(C, C.g1bc, 16, "g1bc")
    B.dma("sp", C.srow[:, :], I["srow"][li, :, :], w=["srow"])
    with contextlib.ExitStack() as sc:
        sba = lambda name, shape, dt=F32: sc.enter_context(nc.sbuf_tensor("%s_a%d" % (name, li), list(shape), dt))
        if "A" in MIXERS:
            mixer_A(C, li, tiles, sba)
        P.barrier()
    with contextlib.ExitStack() as sc:
        sba = lambda name, shape, dt=F32: sc.enter_context(nc.sbuf_tensor("%s_c%d" % (name, li), list(shape), dt))
        if "C" in MIXERS:
            mixer_C(C, li, tiles, sba, lam_init)
        P.barrier()
    with contextlib.ExitStack() as sc:
        sba = lambda name, shape, dt=F32: sc.enter_context(nc.sbuf_tensor("%s_b%d" % (name, li), list(shape), dt))
        if "B" in MIXERS:
            mixer_B(C, li, tiles, sba)
        P.barrier()


def tok_proj(C, li, t, col0, ncol, wt):
    B, ps = C.B, C.psum
    bk = C.poolF.next()
    B.mm(ps[:, bk, 0:ncol], [(C.xT[:, kc, t * 128:(t + 1) * 128], wt[:, kc, col0:col0 + ncol]) for kc in range(KC)],
         r=[("xT", t), "wtok"], w=[("ps", bk)])
    return bk


def mixer_A(C, li, tiles, sba):
    B, nc, I, ps, P = C.B, C.nc, C.I, C.psum, C.P
    C.wfm = sba("wfm", [128, 4, KC, 128], BF16)
    qT = sba("qT", [128, 3, T], BF16)
    kT = sba("kT", [128, 2, T], BF16)
    vA = sba("vA", [128, NT, 2, 66], BF16)
    rope = sba("rope", [128, 2, T], BF16)
    C.rtmp = sba("rtmp", [128, 2, 2, 512])
    C.rt_n = 2
    wt = sba("wtA", [128, KC, 128], BF16)
    wo = sba("woA", [128, 3, D], BF16)
    bm = sba("bmask", [128, 3, 128], BF16)
    E1 = sba("E1", [128, 3, 384], BF16)
    E2 = sba("E2", [128, 3, 256], BF16)
    esink = sba("esink", [128, 6])
    den = sba("den", [128, 2, 6])
    ya = sba("ya", [128, 2, 384], BF16)
    qm = sba("qm", [128, 3, 128], BF16)
    bandm = sba("bandm", [128, 6])
    B.dma("sp", bandm[:, :], I["bandm"][:, :], w=["bandm"])
    B.dma("sp", rope[:, :, :], I["ropeA"].rearrange("a p t -> p a t"), w=["rope"])
    B.dma("sp", bm[:, :, :], I["bmask"][:, :, :], w=["bmask"])
    B.dma("pool", wt[:, :, :], I["w_tok"][li, :, :, TK_VA:TK_VA + 128], w=["wtok"])
    B.dma("pool", wo[:, :, :], I["w_out"][li, 0:384, :].rearrange("(c p) d -> p c d", p=128), w=["wo"])
    B.memset("pool", vA[:, :, :, 64:66], 1.0, w=["vA1"])
    B.act(esink[:, :], C.srow[:, SR_SINK:SR_SINK + 6], AF.Exp, r=["srow"], w=["esink"])
    for c in range(3):
        fm_chunks(C, li, [CH_QA + c, CH_QAR + c], rope_evac(C, qT[:, c, :], ("qT", c), rope[:, 0, :], rope[:, 1, :]))
    for g in range(2):
        fm_chunks(C, li, [CH_KA + g, CH_KAR + g], rope_evac(C, kT[:, g, :], ("kT", g), rope[:, 0, :], rope[:, 1, :]))
    for t in range(NT):
        bk = tok_proj(C, li, t, 0, 128, wt)
        B.copy("act", vA[:, t, :, 0:64], ps[:, bk, 0:128].rearrange("p (g d) -> p g d", g=2), r=[("ps", bk)], w=[("vA", t)])
    scale = 64 ** -0.5
    poolS = PsumPool([0, 1, 2, 4])
    C.poolO = PsumPool([5])
    ei = 0
    for qi, t in enumerate(tiles):
        if t < 2:
            loc = []
        else:
            loc = [(j, t - 1 + j) for j in range(3) if 2 <= t - 1 + j < NT]
        bo = 7 if qi % 2 == 0 else 3
        pend = []
        for h in range(6):
            g, b, c = h // 3, h % 2, h // 2
            rows = slice(b * 64, (b + 1) * 64)
            qtok = ("qT", c, tg_of(t))
            e = ei % 3
            ei += 1
            pv = []
            if loc:
                b1 = poolS.next()
                for (j, kt) in loc:
                    B.mm(ps[:, b1, j * 128:(j + 1) * 128], [(kT[rows, g, kt * 128:(kt + 1) * 128], qT[rows, c, t * 128:(t + 1) * 128])],
                         r=[("kT", g, tg_of(kt)), qtok], w=[("ps", b1)])
                j0, j1 = loc[0][0], loc[-1][0] + 1
                B.act(E1[:, e, j0 * 128:j1 * 128], ps[:, b1, j0 * 128:j1 * 128], AF.Exp, scale=scale, r=[("ps", b1)], w=[("E1", e)])
                B.tt("dve", E1[:, e, j0 * 128:j1 * 128], E1[:, e, j0 * 128:j1 * 128],
                     bm[:, j0:j1, :].rearrange("p a b -> p (a b)"), ALU.mult, r=[("E1", e), "bmask"], w=[("E1", e)])
                pv += [(E1[:, e, j * 128:(j + 1) * 128], vA[:, kt, g, 0:65], ("E1", e), kt) for (j, kt) in loc]
            b2 = poolS.next()
            for kt in range(2):
                B.mm(ps[:, b2, kt * 128:(kt + 1) * 128], [(kT[rows, g, kt * 128:(kt + 1) * 128], qT[rows, c, t * 128:(t + 1) * 128])],
                     r=[("kT", g, 0), qtok], w=[("ps", b2)])
            B.act(E2[:, e, :], ps[:, b2, 0:256], AF.Exp, scale=scale, r=[("ps", b2)], w=[("E2", e)])
            pv += [(E2[:, e, kt * 128:(kt + 1) * 128], vA[:, kt, g, 0:65], ("E2", e), kt) for kt in range(2)]
            pend.append((ps[:, bo, h * 66:h * 66 + 65], [(l, r_) for (l, r_, _, _) in pv],
                         list({x[2] for x in pv}) + [("vA", x[3]) for x in pv] + ["vA1"]))
            while len(pend) > 1:
                o_, p_, r_ = pend.pop(0)
                B.mm(o_, p_, r=r_, w=[("ps", bo)])
        while pend:
            o_, p_, r_ = pend.pop(0)
            B.mm(o_, p_, r=r_, w=[("ps", bo)])
        k = qi % 2
        pv3 = ps[:, bo, 0:396].rearrange("p (h d) -> p h d", d=66)
        B.tt("dve", den[:, k, :], pv3[:, :, 64], esink[:, :], ALU.add, r=[("ps", bo), "esink"], w=[("den", k)])
        P.add("dve", lambda e_, k=k: e_.reciprocal(out=den[:, k, :], in_=den[:, k, :]), r=[("den", k)], w=[("den", k)])
        B.tt("dve", ya[:, k, :].rearrange("p (h d) -> p h d", d=64), pv3[:, :, 0:64],
             den[:, k, :].unsqueeze(2).to_broadcast([128, 6, 64]), ALU.mult, r=[("ps", bo), ("den", k)], w=[("ya", k)])
        if "mixA%d" % li in C.dbg:
            d = dbg_out(C, "mixA%d" % li, 384)
            B.dma("sp", d[t * 128:(t + 1) * 128, :], ya[:, k, :], r=[("ya", k)])
        out_proj_tile(C, t, ya[:, k, :], 3, wo, ("ya", k))


def mixer_C(C, li, tiles, sba, lam_init):
    B, nc, I, ps, P = C.B, C.nc, C.I, C.psum, C.P
    C.poolO = PsumPool([4, 5])
    C.wfm = sba("wfm", [128, 4, KC, 128], BF16)
    qT = sba("qT", [128, 2, T], BF16)
    kT = sba("kT", [128, 2, T], BF16)
    vC = sba("vC", [128, NT, 4, 66], BF16)
    rope = sba("rope", [128, 2, T], BF16)
    C.rtmp = sba("rtmp", [128, 1, 2, 512])
    C.rt_n = 1
    wt = sba("wtC", [128, KC, 256], BF16)
    wo = sba("woC", [128, 2, D], BF16)
    E = sba("E", [128, 5, 512], BF16)
    yd = sba("yd", [128, NT, 256], BF16)
    lam = sba("lam", [128, 8])
    lqk = sba("lqk", [128, 64])
    nw = sba("nw", [128, 64])
    r12 = sba("r12", [128, 1, 2, 4])
    o12 = sba("o12", [128, 1, 2, 4, 64])
    ss = sba("ss", [128, 1, 4])
    aT = sba("aT", [128, 2, 512])
    qm = sba("qm", [128, 2, 512], BF16)
    bandm = sba("bandm", [128, 6])
    B.dma("sp", bandm[:, :], I["bandm"][:, :], w=["bandm"])
    B.dma("sp", rope[:, :, :], I["ropeC"].rearrange("a p t -> p a t"), w=["rope"])
    B.dma("pool", wt[:, :, :], I["w_tok"][li, :, :, TK_VC:TK_VC + 256], w=["wtok"])
    B.dma("pool", wo[:, :, :], I["w_out"][li, 768:1024, :].rearrange("(c p) d -> p c d", p=128), w=["wo"])
    B.memset("pool", vC[:, :, :, 64:66], 1.0, w=["vC1"])
    B.memset("pool", aT[:, :, :], 0.0, w=[("aT", 0), ("aT", 1)])
    B.tt("dve", lqk[:, :], C.srow[:, SR_LQ:SR_LQ + 64], C.srow[:, SR_LK:SR_LK + 64], ALU.mult, r=["srow"], w=["lqk"])
    P.add("dve", lambda e: e.tensor_reduce(out=lam[:, 0:2], in_=lqk[:, :].rearrange("p (a b) -> p a b", a=2), axis=AX.X, op=ALU.add),
          r=["lqk"], w=["lam"])
    B.act(lam[:, 0:2], lam[:, 0:2], AF.Exp, r=["lam"], w=["lam"])
    B.tt("dve", lam[:, 2:3], lam[:, 1:2], lam[:, 0:1], ALU.subtract, r=["lam"], w=["lam"])
    B.ts("dve", lam[:, 4:5], lam[:, 2:3], -lam_init, ALU.add, r=["lam"], w=["lam"])
    B.ts("dve", nw[:, :], C.srow[:, SR_DNW:SR_DNW + 64], 1.0 - lam_init, ALU.mult, r=["srow"], w=["nw"])
    for c in range(2):
        fm_chunks(C, li, [CH_QC + c, CH_QCR + c], rope_evac(C, qT[:, c, :], ("qT", c), rope[:, 0, :], rope[:, 1, :]))
    for c in range(2):
        fm_chunks(C, li, [CH_KC + c, CH_KCR + c], rope_evac(C, kT[:, c, :], ("kT", c), rope[:, 0, :], rope[:, 1, :]))
    for t in range(NT):
        bk = tok_proj(C, li, t, 0, 256, wt)
        B.copy("act", vC[:, t, :, 0:64], ps[:, bk, 0:256].rearrange("p (g d) -> p g d", g=4), r=[("ps", bk)], w=[("vC", t)])
    scale = 32 ** -0.5
    poolS = PsumPool([0, 1, 2, 6])
    accs = PsumPool([3, 7, 4, 5])
    qgroups = []
    if 0 in tiles:
        qgroups.append((0, [0, 1], [0, 1]))
    for gi in range(1, 5):
        t0 = TGS[gi][0] // 128
        qgroups.append((gi, list(range(t0, t0 + 4)), list(range(NT))))
    ei = 0
    gk = 0
    ai_ = [0]
    for hc in range(4):
        cc = hc // 2
        for (gi, qts, kts) in qgroups:
            nq = len(qts)
            q0 = qts[0] * 128
            n = nq * 128
            k = 0
            gk += 1
            acc = []
            pend = []
            for i in range(2):
                j = 2 * (hc % 2) + i
                rows = slice(32 * j, 32 * j + 32)
                ab = accs.next()
                acc.append(ab)
                B.ts("dve", qm[:, i, 0:n], qT[:, cc, q0:q0 + n], bandm[:, j:j + 1], ALU.mult, r=[("qT", cc, gi), "bandm"], w=[("qm", i)])
                for ki, kt in enumerate(kts):
                    sbk = poolS.next()
                    B.mm(ps[:, sbk, 0:n], [(kT[:, cc, kt * 128:(kt + 1) * 128], qm[:, i, 0:n])],
                         r=[("kT", cc, tg_of(kt)), ("qm", i)], w=[("ps", sbk)])
                    e = ei % 5
                    ei += 1
                    B.act(E[:, e, 0:n], ps[:, sbk, 0:n], AF.Exp, scale=scale, r=[("ps", sbk)], w=[("E", e)])

                    def pv(eng, e=e, kt=kt, ab=ab, ki=ki, n=n, hc=hc, last=(ki == len(kts) - 1)):
                        return eng.matmul(ps[0:65, ab, 0:n], lhsT=vC[:, kt, hc, 0:65], rhs=E[:, e, 0:n], start=(ki == 0), stop=last)
                    pend.append((pv, [("E", e), ("vC", kt), "vC1"], [("ps", ab)]))
                    while len(pend) > 3:
                        f_, r_, w_ = pend.pop(0)
                        P.add("pe", f_, r=r_, w=w_)
            while pend:
                f_, r_, w_ = pend.pop(0)
                P.add("pe", f_, r=r_, w=w_)
            for i in range(2):
                ab = acc[i]
                m = ai_[0] % 2
                ai_[0] += 1
                B.copy("act", aT[0:65, m, 0:n], ps[0:65, ab, 0:n], r=[("ps", ab)], w=[("aT", m)])

                def trb(eng, ab=ab, m=m, nq=nq):
                    ins = None
                    for qi in range(nq):
                        ins = eng.transpose(out=ps[:, ab, qi * 66:qi * 66 + 66], in_=aT[0:66, m, qi * 128:(qi + 1) * 128],
                                            identity=C.identf[0:66, 0:66])
                    return ins
                P.add("pe", trb, r=[("aT", m), "identf"], w=[("ps", ab)])
            a1 = ps[:, acc[0], 0:nq * 66].rearrange("p (q d) -> p q d", d=66)
            a2 = ps[:, acc[1], 0:nq * 66].rearrange("p (q d) -> p q d", d=66)
            P.add("dve", lambda e_, k=k, a1=a1, nq=nq: e_.reciprocal(out=r12[:, k, 0, 0:nq], in_=a1[:, :, 64]), r=[("ps", acc[0])], w=[("r12", k)])
            P.add("dve", lambda e_, k=k, a2=a2, nq=nq: e_.reciprocal(out=r12[:, k, 1, 0:nq], in_=a2[:, :, 64]), r=[("ps", acc[1])], w=[("r12", k)])
            B.ts("dve", r12[:, k, 1, 0:nq], r12[:, k, 1, 0:nq], lam[:, 4:5], ALU.mult, r=[("r12", k), "lam"], w=[("r12", k)])
            B.tt("dve", o12[:, k, 0, 0:nq, :], a1[:, :, 0:64], r12[:, k, 0, 0:nq].unsqueeze(2).to_broadcast([128, nq, 64]), ALU.mult,
                 r=[("ps", acc[0]), ("r12", k)], w=[("o1", k)])
            B.tt("dve", o12[:, k, 1, 0:nq, :], a2[:, :, 0:64], r12[:, k, 1, 0:nq].unsqueeze(2).to_broadcast([128, nq, 64]), ALU.mult,
                 r=[("ps", acc[1]), ("r12", k)], w=[("o2", k)])
            B.tt("dve", o12[:, k, 0, 0:nq, :], o12[:, k, 0, 0:nq, :], o12[:, k, 1, 0:nq, :], ALU.add, r=[("o1", k), ("o2", k)], w=[("o1", k)])
            B.tt("dve", o12[:, k, 1, 0:nq, :], o12[:, k, 0, 0:nq, :], o12[:, k, 0, 0:nq, :], ALU.mult, r=[("o1", k)], w=[("o2", k)])
            P.add("dve", lambda e_, k=k, nq=nq: e_.tensor_reduce(out=ss[:, k, 0:nq], in_=o12[:, k, 1, 0:nq, :], axis=AX.X, op=ALU.add),
                  r=[("o2", k)], w=[("ss", k)])
            B.act(ss[:, k, 0:nq], ss[:, k, 0:nq], AF.Sqrt, scale=1.0 / 64, bias=EPS, r=[("ss", k)], w=[("ss", k)])
            P.add("dve", lambda e_, k=k, nq=nq: e_.reciprocal(out=ss[:, k, 0:nq], in_=ss[:, k, 0:nq]), r=[("ss", k)], w=[("ss", k)])
            B.tt("dve", o12[:, k, 0, 0:nq, :], o12[:, k, 0, 0:nq, :], ss[:, k, 0:nq].unsqueeze(2).to_broadcast([128, nq, 64]), ALU.mult,
                 r=[("o1", k), ("ss", k)], w=[("o1", k)])
            B.tt("dve", yd[:, qts[0]:qts[0] + nq, hc * 64:(hc + 1) * 64], o12[:, k, 0, 0:nq, :],
                 nw[:, :].unsqueeze(1).to_broadcast([128, nq, 64]), ALU.mult, r=[("o1", k), "nw"], w=[("yd", qt) for qt in qts])
    for t in tiles:
        if "mixC%d" % li in C.dbg:
            d = dbg_out(C, "mixC%d" % li, 256)
            B.dma("sp", d[t * 128:(t + 1) * 128, :], yd[:, t, :], r=[("yd", t)])
        out_proj_tile(C, t, yd[:, t, :], 2, wo, ("yd", t))


def mixer_B(C, li, tiles, sba):
    B, nc, I, ps, P = C.B, C.nc, C.I, C.psum, C.P
    C.poolO = PsumPool([4, 5])
    xsT = sba("xsT", [128, 3, T], BF16)
    BT = sba("BT", [128, 2, T], BF16)
    CT = sba("CT", [128, 2, T], BF16)
    wz = sba("wz", [128, KC, 384], BF16)
    wdt = sba("wdt", [128, KC, 12], BF16)
    wo = sba("woB", [128, 3, D], BF16)
    cst = sba("ssdc", [128, 4, 128])
    Abc = sba("Abc", [128, 12])
    hb_scr = B.dram_out("scr_hb%d" % li, [NT, 128, 384], BF16)
    B.dma("pool", wz[:, :, :], I["w_tok"][li, :, :, TK_Z:TK_Z + 384], w=["wtok"])
    B.dma("pool", wdt[:, :, :], I["w_tok"][li, :, :, TK_DT:TK_DT + 12], w=["wdt"])
    B.dma("pool", wo[:, :, :], I["w_out"][li, 384:768, :].rearrange("(c p) d -> p c d", p=128), w=["wo"])
    B.dma("sp", cst[:, :, :], I["ssdc"].rearrange("a p l -> p a l"), w=["ssdc"])
    B.act(Abc[:, :], C.srow[:, SR_ALOG:SR_ALOG + 12], AF.Exp, r=["srow"], w=["Abc"])
    B.ts("dve", Abc[:, :], Abc[:, :], -1.0, ALU.mult, r=["Abc"], w=["Abc"])
    with contextlib.ExitStack() as sc:
        sbc = lambda name, shape, dt=F32: sc.enter_context(nc.sbuf_tensor("%s_bc%d" % (name, li), list(shape), dt))
        C.wfm = sbc("wfm", [128, 4, KC, 128], BF16)
        pre = sbc("pre", [128, 2, PADT])
        acc = sbc("acc", [128, 2, 512])
        cp = sbc("convp", [128, 7, 6])
        B.dma("sp", cp[:, :, :], I["convp"][li, :, :, :], w=["convp"])
        for k in range(2):
            B.memset("pool", pre[:, k, :], 0.0, w=[("pre", k, gi) for gi in range(5)])
        dsts = [xsT[:, 0, :], xsT[:, 1, :], xsT[:, 2, :], BT[:, 0, :], BT[:, 1, :], CT[:, 0, :], CT[:, 1, :]]
        ai = 0
        for ci in range(7):
            k = ci % 2

            def evac(gi, t0, n, banks, k=k):
                off = 2 if t0 < 256 else 6
                B.copy("act", pre[:, k, off + t0:off + t0 + n], ps[:, banks[0], 0:n], r=[("ps", banks[0])], w=[("pre", k, gi)])
            fm_chunks(C, li, [CH_XBC + ci], evac)
            for gi, (t0, n) in enumerate(TGS):
                a = ai % 2
                ai += 1
                base = t0 if t0 < 256 else t0 + 4
                rd = [("pre", k, g2) for g2 in range(5)]
                B.ts("dve", acc[:, a, 0:n], pre[:, k, base:base + n], cp[:, ci, 0:1], ALU.mult, r=rd + ["convp"], w=[("acc", a)])
                for tap in range(1, 5):
                    B.stt(acc[:, a, 0:n], pre[:, k, base + tap:base + tap + n], cp[:, ci, tap:tap + 1], acc[:, a, 0:n], ALU.mult, ALU.add,
                          r=rd + ["convp", ("acc", a)], w=[("acc", a)])
                B.act(dsts[ci][:, t0:t0 + n], acc[:, a, 0:n], AF.Silu, bias=cp[:, ci, 5:6], r=[("acc", a), "convp"], w=[("u", ci, gi)])
        P.barrier()
    if B_STAGE < 1:
        return
    with contextlib.ExitStack() as sc:
        sbp = lambda name, shape, dt=F32: sc.enter_context(nc.sbuf_tensor("%s_bp%d" % (name, li), list(shape), dt))
        sc_all = sbp("sc_all", [128, 4, NT, 12])
        ect_all = sbp("ect_all", [128, NT, 12])
        AT = sbp("AT", [128, 6, 128])
        Dm = sbp("Dm", [128, 1, 3, 128])
        E = sbp("E", [128, 2, 3, 128], BF16)
        E0 = sbp("E0", [128, 2, 3, 128], BF16)
        MT = sbp("MT", [128, 1, 12, 128], BF16)
        CTp = sbp("CTp", [128, 1, 12, 128], BF16)
        xtok = sbp("xtok", [128, 2, 6, 64], BF16)
        Btok = sbp("Btok", [128, 2, 2, 128], BF16)
        Xdt = sbp("Xdt", [128, 2, 12, 64], BF16)
        Xw = sbp("Xw", [128, 2, 12, 64], BF16)
        H32 = sbp("H32", [128, 2, 6, 64])
        H16 = sbp("H16", [128, 6, 64], BF16)
        Hbin = sbp("Hbin", [128, 2, 6, 64], BF16)
        yb32 = sbp("yb32", [128, 384])
        tmp32 = sbp("tmp32", [128, 384])
        sz = tmp32
        ssq = sbp("ssq", [128, 2])
        yb16 = sbp("yb16", [128, 2, 384], BF16)
        poolP = PsumPool([0, 1, 2])
        C.poolF = PsumPool([0, 1, 2])
        st_i = [0]
        dt_a, a_a, cum_a, dtw_a = [sc_all[:, i, :, :] for i in range(4)]
        mtf = MT[:, 0, :, :].rearrange("p h l -> p (h l)").bitcast(F32)
        xb, ax, mx = [mtf[:, i * 216:(i + 1) * 216].rearrange("p (t h) -> p t h", h=12) for i in range(3)]
        fl = lambda ap: ap.rearrange("p t h -> p (t h)")
        for t in range(NT):
            B.mm(ps[:, 0, t * 12:(t + 1) * 12], [(C.xT[:, kc, t * 128:(t + 1) * 128], wdt[:, kc, :]) for kc in range(KC)],
                 r=[("xT", t), "wdt"], w=[("ps", 0)])
        B.tt("dve", xb, ps[:, 0, 0:NT * 12].rearrange("p (t h) -> p t h", h=12),
             C.srow[:, SR_DTB:SR_DTB + 12].unsqueeze(1).to_broadcast([128, NT, 12]), ALU.add, r=[("ps", 0), "srow"], w=["sc0"])
        B.act(fl(ax), fl(xb), AF.Abs, r=["sc0"], w=["sc1"])
        B.act(fl(ax), fl(ax), AF.Exp, scale=-1.0, r=["sc1"], w=["sc1"])
        B.act(fl(ax), fl(ax), AF.Ln, bias=1.0, r=["sc1"], w=["sc1"])
        B.ts("dve", fl(mx), fl(xb), 0.0, ALU.max, r=["sc0"], w=["sc2"])
        B.tt("dve", fl(dt_a), fl(mx), fl(ax), ALU.add, r=["sc1", "sc2"], w=["dt_a"])
        B.tt("dve", a_a, dt_a, Abc[:, :].unsqueeze(1).to_broadcast([128, NT, 12]), ALU.mult, r=["dt_a", "Abc"], w=["a_a"])
        for t in range(NT):
            B.mm(ps[:, 1, t * 12:t * 12 + 6], [(cst[:, 0, :], a_a[:, t, 0:6])], r=["ssdc", "a_a"], w=[("ps", 1)])
            B.mm(ps[:, 1, t * 12 + 6:t * 12 + 12], [(cst[:, 1, :], a_a[:, t, 6:12])], r=["ssdc", "a_a"], w=[("ps", 1)])
            B.mm(ps[:, 2, t * 12:(t + 1) * 12], [(C.onesf[:, :], a_a[:, t, :])], r=["onesf", "a_a"], w=[("ps", 2)])
        B.copy("act", fl(cum_a), ps[:, 1, 0:NT * 12], r=[("ps", 1)], w=["cum_a"])
        B.copy("act", fl(ect_all[:, :, :]), ps[:, 2, 0:NT * 12], r=[("ps", 2)], w=["ect"])
        B.tt("dve", fl(dtw_a), fl(ect_all[:, :, :]), fl(cum_a), ALU.subtract, r=["ect", "cum_a"], w=["dtw_a"])
        B.act(fl(dtw_a), fl(dtw_a), AF.Exp, r=["dtw_a"], w=["dtw_a"])
        B.tt("dve", fl(dtw_a), fl(dtw_a), fl(dt_a), ALU.mult, r=["dtw_a", "dt_a"], w=["dtw_a"])
        B.act(fl(ect_all[:, :, :]), fl(ect_all[:, :, :]), AF.Exp, r=["ect", "dtw_a"], w=["ect"])
        P.barrier()

        def prep(t, full):
            k = st_i[0] % 2
            st_i[0] += 1
            tgt = tg_of(t)
            av = a_a[:, t, :]
            bk = poolP.next()
            psb = ps[:, bk, :].bitcast(BF16)

            def tr(e, t=t, psb=psb):
                ins = None
                for c in range(3):
                    ins = e.transpose(out=psb[:, c * 128:(c + 1) * 128], in_=xsT[:, c, t * 128:(t + 1) * 128], identity=C.identb[:, :])
                for g in range(2):
                    ins = e.transpose(out=psb[:, 384 + g * 128:384 + (g + 1) * 128], in_=BT[:, g, t * 128:(t + 1) * 128], identity=C.identb[:, :])
                return ins
            P.add("pe", tr, r=[("u", ci, tgt) for ci in range(5)] + ["identb"], w=[("ps", bk)])
            B.copy("act", xtok[:, k, :, :].rearrange("p h d -> p (h d)"), psb[:, 0:384], r=[("ps", bk)], w=[("xtok", k)])
            B.copy("act", Btok[:, k, :, :].rearrange("p g n -> p (g n)"), psb[:, 384:640], r=[("ps", bk)], w=[("Btok", k)])
            dirs = (0, 1) if full else (1,)
            for d in dirs:
                B.tt("pool", Xw[:, k, d * 6:(d + 1) * 6, :], xtok[:, k, :, :],
                     dtw_a[:, t, d * 6:(d + 1) * 6].unsqueeze(2).to_broadcast([128, 6, 64]),
                     ALU.mult, r=[("xtok", k), "dtw_a"], w=[("Xw", k, d)])
            if not full:
                return k
            for d in range(2):
                B.tt("dve", Xdt[:, k, d * 6:(d + 1) * 6, :], xtok[:, k, :, :],
                     dt_a[:, t, d * 6:(d + 1) * 6].unsqueeze(2).to_broadcast([128, 6, 64]),
                     ALU.mult, r=[("xtok", k), "dt_a"], w=[("Xdt", k, d)])
            return k

        def prep2(t):
            tgt = tg_of(t)
            av = a_a[:, t, :]
            bg = 3
            for g in range(2):
                B.mm(ps[:, bg, g * 128:(g + 1) * 128], [(BT[:, g, t * 128:(t + 1) * 128], CT[:, g, t * 128:(t + 1) * 128])],
                     r=[("u", 3 + g, tgt), ("u", 5 + g, tgt)], w=[("ps", bg)])
            for d in range(2):
                B.tt("dve", AT[:, :, :], cst[:, d, :].unsqueeze(1).to_broadcast([128, 6, 128]),
                     av[:, d * 6:(d + 1) * 6].unsqueeze(2).to_broadcast([128, 6, 128]), ALU.mult, r=["ssdc", "a_a"], w=["AT"])
                for g in range(2):
                    hd0 = d * 6 + g * 3
                    bc = poolP.next()
                    B.mm(ps[:, bc, 0:384], [(C.onesf[:, :], AT[:, g * 3:(g + 1) * 3, :].rearrange("p h l -> p (h l)"))],
                         r=["onesf", "AT"], w=[("ps", bc)])
                    cr = ps[:, bc, 0:384].rearrange("p (h l) -> p h l", h=3)
                    m = 0
                    for i3 in range(3):
                        B.stt(Dm[:, m, i3, :], cr[:, i3, :], cum_a[:, t, hd0 + i3:hd0 + i3 + 1], cst[:, 2 + d, :], ALU.subtract, ALU.add,
                              r=[("ps", bc), "cum_a", "ssdc"], w=[("Dm", m)])
                    B.act(E[:, g, :, :], Dm[:, m, :, :], AF.Exp, r=[("Dm", m)], w=[("E", g)])
                    B.act(E0[:, g, :, :], cr, AF.Exp, r=[("ps", bc)], w=[("E0", g)])
                    for i3 in range(3):
                        B.tt("dve", MT[:, 0, hd0 + i3, :], E[:, g, i3, :], ps[:, bg, g * 128:(g + 1) * 128], ALU.mult,
                             r=[("E", g), ("ps", bg)], w=[("MT", 0, hd0)])
                    B.tt("dve", CTp[:, 0, hd0:hd0 + 3, :], E0[:, g, :, :],
                         CT[:, g, t * 128:(t + 1) * 128].unsqueeze(1).to_broadcast([128, 3, 128]), ALU.mult,
                         r=[("E0", g), ("u", 5 + g, tgt)], w=[("CTp", 0, hd0)])

        def state_update(d, k, t):
            bh = poolP.next()
            for h in range(6):
                B.mm(ps[:, bh, h * 64:(h + 1) * 64], [(Btok[:, k, h // 3, :], Xw[:, k, d * 6 + h, :])],
                     r=[("Btok", k), ("Xw", k, d)], w=[("ps", bh)])
            B.tt("pool", H32[:, d, :, :], H32[:, d, :, :], ect_all[:, t, d * 6:(d + 1) * 6].unsqueeze(2).to_broadcast([128, 6, 64]), ALU.mult,
                 r=[("H32", d), "ect"], w=[("H32", d)])
            B.tt("dve", H32[:, d, :, :], H32[:, d, :, :], ps[:, bh, 0:384].rearrange("p (h d) -> p h d", h=6), ALU.add,
                 r=[("H32", d), ("ps", bh)], w=[("H32", d)])

        B.memset("pool", H32[:, :, :, :], 0.0, w=[("H32", 0), ("H32", 1)])
        border = [1, 0] + list(range(NT - 1, 1, -1))
        kn = prep(border[0], False)
        for i, t in enumerate(border):
            k2 = t % 2
            B.copy("act", Hbin[:, k2, :, :], H32[:, 1, :, :], r=[("H32", 1)], w=[("Hbin", k2)])
            B.dma("sp", hb_scr[t, :, :], Hbin[:, k2, :, :].rearrange("p h d -> p (h d)"), r=[("Hbin", k2)], w=[("hbscr", t)])
            if t == 2:
                break
            k = kn
            if border[i + 1] != 2:
                kn = prep(border[i + 1], False)
            state_update(1, k, t)
        kn = prep(0, True)
        prep2(0)
        for t in range(NT):
            need_y = t in tiles
            k = kn
            if t + 1 < NT:
                kn = prep(t + 1, True)
            if need_y:
                k2 = t % 2
                B.dma("sp", Hbin[:, k2, :, :].rearrange("p h d -> p (h d)"), hb_scr[t, :, :], r=[("hbscr", t)], w=[("Hbin", k2)])
                B.copy("act", H16[:, :, :], H32[:, 0, :, :], r=[("H32", 0)], w=["H16"])
                by = 7
                for h in range(6):
                    B.mm(ps[:, by, h * 64:(h + 1) * 64],
                         [(MT[:, 0, h, :], Xdt[:, k, h, :]), (CTp[:, 0, h, :], H16[:, h, :]),
                          (MT[:, 0, 6 + h, :], Xdt[:, k, 6 + h, :]), (CTp[:, 0, 6 + h, :], Hbin[:, k2, h, :])],
                         r=[("MT", 0, (h // 3) * 3), ("MT", 0, 6 + (h // 3) * 3), ("CTp", 0, (h // 3) * 3), ("CTp", 0, 6 + (h // 3) * 3),
                            ("Xdt", k, 0), ("Xdt", k, 1), "H16", ("Hbin", k2)], w=[("ps", by)])
            if t + 1 < NT:
                prep2(t + 1)
            if need_y:
                B.tt("dve", tmp32[:, :].rearrange("p (h d) -> p h d", h=6), xtok[:, k, :, :],
                     C.srow[:, SR_DSKIP:SR_DSKIP + 6].unsqueeze(2).to_broadcast([128, 6, 64]), ALU.mult, r=[("xtok", k), "srow"], w=["tmp32"])
                B.tt("dve", yb32[:, :], ps[:, by, 0:384], tmp32[:, :], ALU.add, r=[("ps", by), "tmp32"], w=["yb32"])
                bz = tok_proj(C, li, t, 0, 384, wz)
                B.act(sz[:, :], ps[:, bz, 0:384], AF.Silu, r=[("ps", bz)], w=["tmp32"])
                B.tt("dve", yb32[:, :], yb32[:, :], sz[:, :], ALU.mult, r=["yb32", "tmp32"], w=["yb32"])
                B.act(tmp32[:, :], yb32[:, :], AF.Square, accum_out=ssq[:, 0:1], r=["yb32"], w=["tmp32", "ssq"])
                B.act(ssq[:, 1:2], ssq[:, 0:1], AF.Sqrt, scale=1.0 / 384, bias=EPS, r=["ssq"], w=["ssq"])
                P.add("dve", lambda e_: e_.reciprocal(out=ssq[:, 1:2], in_=ssq[:, 1:2]), r=["ssq"], w=["ssq"])
                B.stt(yb16[:, k2, :], yb32[:, :], ssq[:, 1:2], C.srow[:, SR_SNW:SR_SNW + 384], ALU.mult, ALU.mult,
                      r=["yb32", "ssq", "srow"], w=[("yb16", k2)])
                if "mixB%d" % li in C.dbg:
                    d_ = dbg_out(C, "mixB%d" % li, 384)
                    B.dma("sp", d_[t * 128:(t + 1) * 128, :], yb16[:, k2, :], r=[("yb16", k2)])
                out_proj_tile(C, t, yb16[:, k2, :], 3, wo, ("yb16", k2))
            if t < NT - 1:
                state_update(0, k, t)
        P.barrier()


_NC_CACHE = {}


def kernel(**inputs):
    inp = {k: np.asarray(v) for k, v in inputs.items()}
    if "nc" not in _NC_CACHE:
        nc = bass.Bass("TRN2", target_bir_lowering=False)
        build_program(nc, nlayers=DEPTH)
        _NC_CACHE["nc"] = nc
    nc = _NC_CACHE["nc"]
    shared = _shared_inputs(inp)
    in_maps = [host_inputs(inp, b, shared) for b in range(8)]
    res = run_bass_kernel_spmd(nc, in_maps, core_ids=list(range(8)))
    out = np.stack([np.asarray(r["out"], dtype=np.float32) for r in res.results], axis=0)
    return out
```

```python
import contextlib
import math
import numpy as np
import ml_dtypes
import concourse.bass as bass
import concourse.mybir as mybir
from concourse.bass_utils import run_bass_kernel_spmd

F32, BF16 = mybir.dt.float32, mybir.dt.bfloat16
AF = mybir.ActivationFunctionType
ALU = mybir.AluOpType
AX = mybir.AxisListType

D = 1024
NT = 18
T = NT * 128
KC = 8
DEPTH = 4
DFF = 2816
FG = 256
NFG = DFF // FG
NEXP = 8
ALPHA = (2 * DEPTH) ** 0.25
EPS = 1e-6
TGS = [(0, 256), (256, 512), (768, 512), (1280, 512), (1792, 512)]

O_QA, O_KA, O_VA, O_Z, O_XBC, O_DTF, O_DTB, O_QC, O_KC, O_VC = 0, 384, 512, 640, 1024, 1920, 1926, 1932, 2188, 2444


class Op:
    __slots__ = ("idx", "eng", "fn", "dma", "deps", "sig", "cnt", "dsem", "dval", "dprev")

    def __init__(self, idx, eng, fn, dma):
        self.idx, self.eng, self.fn, self.dma = idx, eng, fn, dma
        self.deps = set()
        self.sig = False
        self.cnt = 0
        self.dsem = None
        self.dval = 0
        self.dprev = 0


class Prog:
    ENGS = ("pe", "act", "dve", "pool", "sp")
    NDS = 20
    SEM_CAP = 30000

    def __init__(self, nc):
        self.nc = nc
        self.ops = []
        self.lastw = {}
        self.rd = {}
        self.bar_from = 0

    def add(self, eng, fn, r=(), w=(), dma=False):
        op = Op(len(self.ops), eng, fn, dma)
        deps = set()
        for t in r:
            lw = self.lastw.get(t)
            if lw is not None:
                deps.add(lw)
        for t in w:
            lw = self.lastw.get(t)
            if lw is not None:
                deps.add(lw)
            for o in self.rd.get(t, ()):
                deps.add(o)
        deps.discard(op)
        op.deps = deps
        for t in r:
            self.rd.setdefault(t, set()).add(op)
        for t in w:
            self.lastw[t] = op
            self.rd[t] = set()
        self.ops.append(op)
        return op

    def barrier(self):
        last = {}
        dmas = []
        for op in self.ops[self.bar_from:]:
            if op.dma:
                dmas.append(op)
            elif op.fn is not None:
                last[op.eng] = op
        self.bar_from = len(self.ops)
        for e in self.ENGS:
            op = Op(len(self.ops), e, None, False)
            op.deps = set(last.values()) | set(dmas)
            self.ops.append(op)

    def emit(self):
        nc = self.nc
        ops = self.ops
        for op in ops:
            for d in op.deps:
                if d.dma:
                    continue
                if d.eng == op.eng and op.eng in ("pe", "sp") and not op.dma and op.fn is not None:
                    continue
                d.sig = True
        cnt = {e: 0 for e in self.ENGS}
        for op in ops:
            if op.sig and not op.dma:
                cnt[op.eng] += 1
                op.cnt = cnt[op.eng]
        dcount = {"sp": 0, "pool": 0}
        duse = {}
        for op in ops:
            if op.dma:
                k = dcount[op.eng] % self.NDS
                dcount[op.eng] += 1
                key = (op.eng, k)
                op.dsem = key
                op.dprev = duse.get(key, 0)
                op.dval = op.dprev + 16
                duse[key] = op.dval
        with contextlib.ExitStack() as st:
            esems = {}
            for e in self.ENGS:
                n = cnt[e] // self.SEM_CAP + 1
                esems[e] = [st.enter_context(nc.semaphore(f"se_{e}_{i}")) for i in range(n)]
            dsems = {}
            for q in ("sp", "pool"):
                for k in range(min(self.NDS, dcount[q])):
                    dsems[(q, k)] = st.enter_context(nc.semaphore(f"sd_{q}_{k}"))
            block = st.enter_context(nc.Block())
            cap = self.SEM_CAP

            def run(engname, eh):
                waited = {}

                def wait(sem_key, sem, val):
                    if waited.get(sem_key, 0) >= val:
                        return
                    waited[sem_key] = val
                    eh.wait_ge(sem, val)

                for op in ops:
                    if op.eng != engname:
                        continue
                    for d in sorted(op.deps, key=lambda o: o.idx):
                        if d.dma:
                            wait(("d",) + d.dsem, dsems[d.dsem], d.dval)
                        else:
                            if d.eng == engname and engname in ("pe", "sp") and not op.dma and op.fn is not None:
                                continue
                            si, sv = divmod(d.cnt - 1, cap)
                            wait(("e", d.eng, si), esems[d.eng][si], sv + 1)
                    if op.fn is None:
                        continue
                    if op.dma:
                        if op.dprev > 0:
                            wait(("d",) + op.dsem, dsems[op.dsem], op.dprev)
                        ins = op.fn(eh)
                        ins.then_inc(dsems[op.dsem], 16)
                    else:
                        ins = op.fn(eh)
                        if op.sig:
                            si, sv = divmod(op.cnt - 1, cap)
                            ins.then_inc(esems[engname][si], 1)
                if engname == "sp":
                    for key, v in duse.items():
                        wait(("d",) + key, dsems[key], v)

            @block.tensor
            def _(t):
                run("pe", t)

            @block.scalar
            def _(s):
                run("act", s)

            @block.vector
            def _(v):
                run("dve", v)

            @block.gpsimd
            def _(g):
                run("pool", g)

            @block.sync
            def _(s):
                run("sp", s)


class PsumPool:
    def __init__(self, banks):
        self.banks = list(banks)
        self.i = 0

    def next(self):
        b = self.banks[self.i % len(self.banks)]
        self.i += 1
        return b


class Builder:
    def __init__(self, nc, dbg=None, nlayers=DEPTH):
        self.nc = nc
        self.P = Prog(nc)
        self.dbg = dbg or {}
        self.nlayers = nlayers
        self.uid = 0

    def tok(self, name):
        self.uid += 1
        return (name, self.uid)

    def dram_in(self, name, shape, dt=F32):
        return self.nc.dram_tensor(name, list(shape), dt, kind="ExternalInput").ap()

    def dram_out(self, name, shape, dt=F32):
        return self.nc.dram_tensor(name, list(shape), dt, kind="ExternalOutput").ap()

    def dma(self, q, out, in_, r=(), w=()):
        return self.P.add(q, lambda e: e.dma_start(out=out, in_=in_), r, w, dma=True)

    def mm(self, out, pairs, r=(), w=(), tile_position=None, start=True):
        def fn(e):
            n = len(pairs)
            ins = None
            for i, (l, rh) in enumerate(pairs):
                kw = {}
                if tile_position is not None:
                    kw["tile_position"] = tile_position
                ins = e.matmul(out, lhsT=l, rhs=rh, start=(start and i == 0), stop=(i == n - 1), **kw)
            return ins
        return self.P.add("pe", fn, r, w)

    def act(self, out, in_, func, r=(), w=(), bias=None, scale=None, accum_out=None):
        def fn(e):
            kw = {}
            if bias is not None:
                kw["bias"] = bias
            if scale is not None:
                kw["scale"] = scale
            if accum_out is not None:
                kw["accum_out"] = accum_out
            return e.activation(out=out, in_=in_, func=func, **kw)
        return self.P.add("act", fn, r, w)

    def tt(self, eng, out, in0, in1, op, r=(), w=()):
        return self.P.add(eng, lambda e: e.tensor_tensor(out=out, in0=in0, in1=in1, op=op), r, w)

    def ts(self, eng, out, in0, s1, op0, s2=None, op1=None, r=(), w=()):
        def fn(e):
            if op1 is None:
                return e.tensor_scalar(out=out, in0=in0, scalar1=s1, scalar2=None, op0=op0)
            return e.tensor_scalar(out=out, in0=in0, scalar1=s1, scalar2=s2, op0=op0, op1=op1)
        return self.P.add(eng, fn, r, w)

    def stt(self, out, in0, scalar, in1, op0, op1, r=(), w=()):
        return self.P.add("dve", lambda e: e.scalar_tensor_tensor(out=out, in0=in0, scalar=scalar, in1=in1,
                                                                   op0=op0, op1=op1), r, w)

    def copy(self, eng, out, in_, r=(), w=()):
        if eng == "act":
            return self.P.add("act", lambda e: e.copy(out=out, in_=in_), r, w)
        return self.P.add(eng, lambda e: e.tensor_copy(out=out, in_=in_), r, w)

    def memset(self, eng, ap, val, w=()):
        return self.P.add(eng, lambda e: e.memset(ap, val), (), w)


class Ctx:
    pass


def declare_inputs(B):
    I = {}
    I["xin"] = B.dram_in("xin", [T, D])
    I["cvec"] = B.dram_in("cvec", [128, KC, 2])
    I["ident"] = B.dram_in("ident", [128, 128])
    I["w_ada"] = B.dram_in("w_ada", [DEPTH, D, 6 * D])
    I["b_adaT"] = B.dram_in("b_adaT", [DEPTH, 128, 48])
    I["lnrows"] = B.dram_in("lnrows", [DEPTH, 4, 128, D])
    I["ffn_gu"] = B.dram_in("ffn_gu", [2, NFG, 2, 128, KC, FG])
    I["ffn_dn"] = B.dram_in("ffn_dn", [2, DFF, D])
    I["exp_gu"] = B.dram_in("exp_gu", [2, NEXP, NFG, 2, 128, KC, FG])
    I["exp_dn"] = B.dram_in("exp_dn", [2, NEXP, DFF, D])
    I["router_w"] = B.dram_in("router_w", [2, 128, KC, NEXP])
    I["router_b"] = B.dram_in("router_b", [2, 128, NEXP])
    I["w_fm"] = B.dram_in("w_fm", [DEPTH, NCH, 128, KC, 128])
    I["w_tok"] = B.dram_in("w_tok", [DEPTH, 128, KC, NTK])
    I["w_out"] = B.dram_in("w_out", [DEPTH, D, D])
    I["srow"] = B.dram_in("srow", [DEPTH, 128, NSR])
    I["convp"] = B.dram_in("convp", [DEPTH, 128, 7, 6])
    I["ropeA"] = B.dram_in("ropeA", [2, 128, T], BF16)
    I["ropeC"] = B.dram_in("ropeC", [2, 128, T], BF16)
    I["bmask"] = B.dram_in("bmask", [128, 3, 128], BF16)
    I["ssdc"] = B.dram_in("ssdc", [4, 128, 128])
    I["bandm"] = B.dram_in("bandm", [128, 6])
    return I


def build_program(nc, nlayers=DEPTH, dbg=(), inject_x1=False):
    B = Builder(nc)
    P = B.P
    I = declare_inputs(B)
    out_d = B.dram_out("out", [2048, D])
    dbg_d = {}
    st = contextlib.ExitStack()
    sb = lambda name, shape, dt=F32: st.enter_context(nc.sbuf_tensor(name, list(shape), dt))
    C = Ctx()
    C.B, C.P, C.I, C.nc = B, P, I, nc
    C.x = sb("x", [128, NT, D])
    C.xT = sb("xT", [128, KC, T], BF16)
    C.identf = sb("identf", [128, 128])
    C.identb = sb("identb", [128, 128], BF16)
    C.onesf = sb("onesf", [128, 128])
    C.cact = sb("cact", [128, KC, 2])
    C.adaT = sb("adaT", [128, 48, 2])
    C.scp = sb("scp", [128, 2, KC, 2])
    C.mv = sb("mv", [128, NT, 2])
    C.rstd = sb("rstd", [128, NT])
    C.nmr = sb("nmr", [128, NT])
    C.bnst = sb("bnst", [128, NT, 2, 6])
    C.psum = st.enter_context(nc.psum_tensor("psum", [128, 8, 512], F32))
    ps = C.psum

    xin_v = I["xin"].rearrange("(t p) d -> p t d", p=128)
    for t in range(NT):
        B.dma("sp", C.x[:, t, :], xin_v[:, t, :], w=[("x", t)])
    B.dma("sp", C.identf[:, :], I["ident"][:, :], w=["identf"])
    B.dma("sp", C.cact[:, :, :], I["cvec"][:, :, :], w=["cact"])
    B.copy("dve", C.identb[:, :], C.identf[:, :], r=["identf"], w=["identb"])
    B.memset("pool", C.onesf[:, :], 1.0, w=["onesf"])
    B.act(C.cact[:, :, :], C.cact[:, :, :], AF.Silu, r=["cact"], w=["cact"])
    for t in range(NT):
        B.ts("pool", C.x[:, t, :], C.x[:, t, :], ALPHA, ALU.mult, r=[("x", t)], w=[("x", t)])

    C.nlayers = nlayers
    C.dbg = dbg
    C.dbgd = {}
    for li in range(nlayers):
        last = (li == DEPTH - 1)
        tiles = list(range(2, NT)) if last else list(range(NT))
        mk = lambda sc, tag, li=li: (lambda name, shape, dt=F32: sc.enter_context(nc.sbuf_tensor("%s_%s%d" % (name, tag, li), list(shape), dt)))
        with contextlib.ExitStack() as sc:
            C.sb = mk(sc, "m")
            C.dg = C.sb("dg", [128, 2, 128])
            with contextlib.ExitStack() as sc2:
                C.sb2 = mk(sc2, "m0")
                C.xn = C.sb2("xn", [128, 2, D], BF16)
                ada_params(C, li)
                ln_modulate(C, which_sc=0, sh_base=0)
                P.barrier()
            if "xT%d" % li in dbg:
                d = B.dram_out("dbg_xT%d" % li, [128, KC, T], BF16)
                B.dma("sp", d[:, :, :], C.xT[:, :, :], r=[("xT", t) for t in range(NT)])
            if inject_x1:
                d = B.dram_in("dbg_x1_%d" % li, [T, D])
                dv = d.rearrange("(t p) d -> p t d", p=128)
                for t in range(NT):
                    B.dma("sp", C.x[:, t, :], dv[:, t, :], w=[("x", t)])
            else:
                mixer_phase(C, li, tiles)
                with contextlib.ExitStack() as sc2:
                    C.rows = mk(sc2, "m9")("lnrows", [128, 2, D])
                    post_ln(C, li, 0, tiles, out_scale=ALPHA)
                    P.barrier()
            if "x1_%d" % li in dbg:
                dump_x(C, "dbg_x1_%d" % li)
            P.barrier()
        with contextlib.ExitStack() as sc:
            C.sb = lambda name, shape, dt=F32, li=li, sc=sc: sc.enter_context(nc.sbuf_tensor("%s_f%d" % (name, li), list(shape), dt))
            C.xn = C.sb("xn", [128, 2, D], BF16)
            C.rows = C.sb("lnrows", [128, 2, D])
            C.dg = C.sb("dg", [128, 2, 128])
            ffn_phase(C, li, tiles)
            if "xpre%d" % li in dbg:
                dump_x(C, "dbg_xpre%d" % li)
            post_ln(C, li, 1, tiles, out_scale=(1.0 if li == nlayers - 1 else ALPHA))
            if "x2_%d" % li in dbg:
                dump_x(C, "dbg_x2_%d" % li)
            P.barrier()
    ov = out_d.rearrange("(t p) d -> p t d", p=128)
    for t in range(2, NT):
        B.dma("sp", ov[:, t - 2, :], C.x[:, t, :], r=[("x", t)])
    P.emit()
    st.close()
    return nc


def dbg_out(C, name, w):
    if name not in C.dbgd:
        C.dbgd[name] = C.B.dram_out("dbg_" + name, [T, w], BF16)
    return C.dbgd[name]


def dump_x(C, name):
    d = C.B.dram_out(name, [T, D])
    dv = d.rearrange("(t p) d -> p t d", p=128)
    for t in range(NT):
        C.B.dma("sp", dv[:, t, :], C.x[:, t, :], r=[("x", t)])


def ada_params(C, li):
    B, nc, I, ps = C.B, C.nc, C.I, C.psum
    wb = C.sb2("adaw", [128, 2, KC, 256])
    bT = C.sb2("adab", [128, 48])
    B.dma("sp", bT[:, :], I["b_adaT"][li, :, :], w=["adab"])
    wv = I["w_ada"][li].rearrange("(kc p) f -> p kc f", p=128)
    bank = 7
    for piece in range(24):
        s = piece % 2
        B.dma("sp", wb[:, s, :, :], wv[:, :, piece * 256:(piece + 1) * 256], w=[("adaw", s)])
        for j in range(2):
            fc = piece * 2 + j
            B.mm(ps[:, bank, fc * 2:fc * 2 + 2],
                 [(wb[:, s, kc, j * 128:(j + 1) * 128], C.cact[:, kc, :]) for kc in range(KC)],
                 r=[("adaw", s), "cact"], w=[("ps", bank)])
    B.tt("dve", C.adaT[:, :, :], ps[:, bank, 0:96].rearrange("p (f w) -> p f w", w=2),
         bT[:, :].unsqueeze(2).to_broadcast([128, 48, 2]), ALU.add,
         r=[("ps", bank), "adab"], w=["adaT"])
    B.ts("pool", C.scp[:, 0, :, :], C.adaT[:, 8:16, :], 1.0, ALU.add, r=["adaT"], w=["scp"])
    B.ts("pool", C.scp[:, 1, :, :], C.adaT[:, 32:40, :], 1.0, ALU.add, r=["adaT"], w=["scp"])


def ln_stats(C, tiles, eps=EPS):
    B = C.B
    for t in tiles:
        for h in range(2):
            C.P.add("dve", lambda e, t=t, h=h: e.bn_stats(out=C.bnst[:, t, h, :], in_=C.x[:, t, h * 512:(h + 1) * 512]),
                    r=[("x", t), ("x", t, h)], w=[("bnst", t)])
        C.P.add("dve", lambda e, t=t: e.bn_aggr(out=C.mv[:, t, :], in_=C.bnst[:, t, :, :].rearrange("p a b -> p (a b)")),
                r=[("bnst", t)], w=[("mv", t)])
    t0, t1 = tiles[0], tiles[-1] + 1
    B.act(C.rstd[:, t0:t1], C.mv[:, t0:t1, 1], AF.Sqrt, bias=eps, r=[("mv", t) for t in tiles], w=[("rstd", t) for t in tiles])
    C.P.add("dve", lambda e: e.reciprocal(out=C.rstd[:, t0:t1], in_=C.rstd[:, t0:t1]),
            r=[("rstd", t) for t in tiles], w=[("rstd", t) for t in tiles])


def ln_modulate(C, which_sc, sh_base, ctx_tiles=True):
    B, nc, ps = C.B, C.nc, C.psum
    tiles = list(range(NT)) if ctx_tiles else list(range(2, NT))
    ln_stats(C, tiles, eps=EPS * ALPHA * ALPHA)
    if True:
        xn = C.xn
        pool = PsumPool([0, 1])
        for i, t in enumerate(tiles):
            s = i % 2
            wch = 1 if t < 2 else 0
            B.ts("dve", xn[:, s, :], C.x[:, t, :], C.mv[:, t, 0:1], ALU.subtract, C.rstd[:, t:t + 1], ALU.mult,
                 r=[("x", t), ("mv", t), ("rstd", t)], w=[("xn", s)])
            bk = pool.next()
            psb = ps[:, bk, :].bitcast(BF16)

            def tr(e, s=s, psb=psb):
                ins = None
                for kc in range(KC):
                    ins = e.transpose(out=psb[:, kc * 128:(kc + 1) * 128], in_=xn[:, s, kc * 128:(kc + 1) * 128],
                                      identity=C.identb[:, :])
                return ins
            C.P.add("pe", tr, r=[("xn", s), "identb"], w=[("ps", bk)])
            for kc in range(KC):
                B.act(C.xT[:, kc, t * 128:(t + 1) * 128], psb[:, kc * 128:(kc + 1) * 128], AF.Identity,
                      scale=C.scp[:, which_sc, kc, wch:wch + 1], bias=C.adaT[:, sh_base + kc, wch:wch + 1],
                      r=[("ps", bk), "scp", "adaT"], w=[("xT", t)])


def _shared_inputs(inp):
    f32 = np.float32
    S = {}
    S["ident"] = np.eye(128, dtype=f32)
    S["w_ada"] = np.ascontiguousarray(inp["w_ada"], dtype=f32)
    S["b_adaT"] = np.ascontiguousarray(inp["b_ada"].reshape(DEPTH, 48, 128).transpose(0, 2, 1), dtype=f32)
    ln = np.stack([inp["ln1_g"], inp["ln1_b"], inp["ln2_g"], inp["ln2_b"]], axis=1)
    S["lnrows"] = np.ascontiguousarray(np.broadcast_to(ln[:, :, None, :], (DEPTH, 4, 128, D)), dtype=f32)
    g = inp["ffn_w_gu"].reshape(2, KC, 128, 2, NFG, FG)
    S["ffn_gu"] = np.ascontiguousarray(g.transpose(0, 4, 3, 2, 1, 5), dtype=f32)
    S["ffn_dn"] = np.ascontiguousarray(inp["ffn_w_down"], dtype=f32)
    g = inp["exp_w_gu"].reshape(2, NEXP, KC, 128, 2, NFG, FG)
    S["exp_gu"] = np.ascontiguousarray(g.transpose(0, 1, 5, 4, 3, 2, 6), dtype=f32)
    S["exp_dn"] = np.ascontiguousarray(inp["exp_w_down"], dtype=f32)
    S["router_w"] = np.ascontiguousarray(inp["router_w"].reshape(2, KC, 128, NEXP).transpose(0, 2, 1, 3), dtype=f32)
    S["router_b"] = np.ascontiguousarray(np.broadcast_to(inp["router_b"][:, None, :], (2, 128, NEXP)), dtype=f32)
    i128 = np.arange(128)

    def rot(i, hd):
        d = i % hd
        return (i // hd) * hd + np.where(d < hd // 2, d + hd // 2, d - hd // 2)
    cols = []
    for c in range(3):
        cols.append(O_QA + c * 128 + i128)
    for c in range(3):
        cols.append(O_QA + c * 128 + rot(i128, 64))
    for g in range(2):
        cols.append(O_KA + g * 64 + (i128 % 64))
    for g in range(2):
        cols.append(O_KA + g * 64 + rot(i128 % 64, 64))
    for base in (O_QC, O_KC):
        for c in range(2):
            cols.append(base + c * 128 + i128)
        for c in range(2):
            cols.append(base + c * 128 + rot(i128, 32))
    for ci in range(7):
        cols.append(O_XBC + ci * 128 + i128)
    cols = np.stack(cols)
    w_in = inp["w_in"]
    wf = w_in[:, :, cols]
    S["w_fm"] = np.ascontiguousarray(wf.reshape(DEPTH, KC, 128, NCH, 128).transpose(0, 3, 2, 1, 4), dtype=f32)
    tcols = np.concatenate([O_VA + np.arange(128), O_VC + np.arange(256), O_DTF + np.arange(12), O_Z + np.arange(384)])
    wt = w_in[:, :, tcols]
    S["w_tok"] = np.ascontiguousarray(wt.reshape(DEPTH, KC, 128, NTK).transpose(0, 2, 1, 3), dtype=f32)
    S["w_out"] = np.ascontiguousarray(inp["w_out"], dtype=f32)
    sr = np.concatenate([inp["attn_sink"], inp["dt_bias"].reshape(DEPTH, 12), inp["a_log"].reshape(DEPTH, 12), inp["d_skip"],
                         inp["lam_q"].reshape(DEPTH, 64), inp["lam_k"].reshape(DEPTH, 64), inp["diff_norm_w"], inp["ssm_norm_w"]], axis=1)
    S["srow"] = np.ascontiguousarray(np.broadcast_to(sr[:, None, :], (DEPTH, 128, NSR)), dtype=f32)
    cw = np.concatenate([inp["conv_w"], inp["conv_b"][:, None, :]], axis=1)
    S["convp"] = np.ascontiguousarray(cw.reshape(DEPTH, 6, 7, 128).transpose(0, 3, 2, 1), dtype=f32)
    tt = np.arange(2048)
    rows, colsg = (tt // 64).astype(f32), (tt % 64).astype(f32)

    def rope_tab(hd):
        quarter = hd // 4
        inv = (10000.0 ** (-np.arange(quarter, dtype=f32) / quarter)).astype(f32)
        ang = np.concatenate([rows[:, None] * inv, colsg[:, None] * inv], axis=-1).astype(f32)
        d = i128 % hd
        jj = d % (hd // 2)
        sign = np.where(d < hd // 2, -1.0, 1.0).astype(f32)
        cos = np.ones((128, T), f32)
        sin = np.zeros((128, T), f32)
        cos[:, 256:] = np.cos(ang).astype(f32)[:, jj].T
        sin[:, 256:] = np.sin(ang).astype(f32)[:, jj].T * sign[:, None]
        return np.stack([cos, sin]).astype(ml_dtypes.bfloat16)
    S["ropeA"] = rope_tab(64)
    S["ropeC"] = rope_tab(32)
    kk, qq = np.meshgrid(np.arange(128), np.arange(128), indexing="ij")
    S["bmask"] = np.stack([(qq <= kk), np.ones_like(kk, bool), (qq >= kk)], axis=1).astype(f32).astype(ml_dtypes.bfloat16)
    jj, ll = np.meshgrid(np.arange(128), np.arange(128), indexing="ij")
    pp = np.arange(128)
    S["bandm"] = np.stack([(pp // 32 == 0), (pp // 32 == 1), (pp // 32 == 2), (pp // 32 == 3), (pp // 64 == 0), (pp // 64 == 1)], axis=1).astype(f32)
    S["ssdc"] = np.stack([(jj <= ll).astype(f32), (jj >= ll).astype(f32),
                          np.where(ll >= jj, 0.0, -30000.0).astype(f32), np.where(ll <= jj, 0.0, -30000.0).astype(f32)]).astype(f32)
    return S


def host_inputs(inp, b, shared=None):
    f32 = np.float32
    m = dict(shared if shared is not None else _shared_inputs(inp))
    m["xin"] = np.ascontiguousarray(np.concatenate([inp["ctx"][b], inp["x"][b]], axis=0), dtype=f32)
    cc = np.stack([inp["c"][b], inp["c_ctx"]], axis=0)
    m["cvec"] = np.ascontiguousarray(cc.reshape(2, KC, 128).transpose(2, 1, 0), dtype=f32)
    return m


def bcast_rows(C, dst, src_fc0, name):
    B, nc, ps = C.B, C.nc, C.psum
    if True:
        dg = C.dg
        k = 0
        for wch in range(2):
            for half in range(2):
                bank = 6 + (k % 2)
                for q in range(4):
                    kc = half * 4 + q
                    s = (k * 4 + q) % 2
                    B.ts("dve", dg[:, s, :], C.identf[:, :], C.adaT[:, src_fc0 + kc, wch:wch + 1], ALU.mult,
                         r=["identf", "adaT"], w=[("diag", s)])
                    B.mm(ps[:, bank, q * 128:(q + 1) * 128], [(C.onesf[:, :], dg[:, s, :])],
                         r=["onesf", ("diag", s)], w=[("ps", bank)])
                B.copy("act", dst[:, wch, half * 512:(half + 1) * 512], ps[:, bank, :], r=[("ps", bank)], w=[name])
                k += 1


def post_ln(C, li, idx, tiles, out_scale=1.0):
    B, nc = C.B, C.nc
    ln_stats(C, tiles)
    if True:
        rows = C.rows
        for j in range(2):
            B.dma("sp", rows[:, j, :], C.I["lnrows"][li, 2 * idx + j, :, :], w=[("lnrow", j)])
            if out_scale != 1.0:
                B.ts("pool", rows[:, j, :], rows[:, j, :], out_scale, ALU.mult, r=[("lnrow", j)], w=[("lnrow", j)])
        t0_, t1_ = tiles[0], tiles[-1] + 1
        B.stt(C.nmr[:, t0_:t1_], C.mv[:, t0_:t1_, 0], -1.0, C.rstd[:, t0_:t1_], ALU.mult, ALU.mult,
              r=[("mv", t) for t in tiles] + [("rstd", t) for t in tiles], w=["nmr"])
        for t in tiles:
            B.act(C.x[:, t, :], C.x[:, t, :], AF.Identity, scale=C.rstd[:, t:t + 1], bias=C.nmr[:, t:t + 1],
                  r=[("x", t), ("x", t, 0), ("x", t, 1), "nmr", ("rstd", t)], w=[("x", t)])
            B.tt("dve", C.x[:, t, :], C.x[:, t, :], rows[:, 0, :], ALU.mult, r=[("x", t), ("lnrow", 0)], w=[("x", t)])
            B.tt("dve", C.x[:, t, :], C.x[:, t, :], rows[:, 1, :], ALU.add, r=[("x", t), ("lnrow", 1)], w=[("x", t)])


def ffn_phase(C, li, tiles):
    B, nc, I, ps, P = C.B, C.nc, C.I, C.psum, C.P
    moe = (li % 2 == 1)
    j = li // 2
    use_ctx = 0 in tiles
    ln_modulate(C, which_sc=1, sh_base=24, ctx_tiles=use_ctx)
    if "xTf%d" % li in C.dbg:
        d = B.dram_out("dbg_xTf%d" % li, [128, KC, T], BF16)
        B.dma("sp", d[:, :, :], C.xT[:, :, :], r=[("xT", t) for t in range(NT)])
    tgs = TGS if use_ctx else TGS[1:]
    if True:
        sbt = C.sb
        g2bc = sbt("g2bc", [128, 2, D])
        wgu = sbt("wgu", [128, 2, 2, KC, FG], BF16)
        wdf = sbt("wdf", [128, 1, 2, D])
        wdl = sbt("wdl", [128, 2, 2, D], BF16)
        wdc = sbt("wdc", [128, 2, 2, D], BF16)
        hT = sbt("hT", [128, 2, 2, T], BF16)
        sg = sbt("sg", [128, 3, 512], BF16)
        etmp = sbt("etmp", [128, 2, 512])
        evi = [0]
        bcast_rows(C, g2bc, 40, "g2bc")
        gates = None
        if moe:
            wr = sbt("wr", [128, KC, NEXP])
            wrb = sbt("wrb", [128, KC, NEXP], BF16)
            rb = sbt("rb", [128, NEXP])
            lg = sbt("lg", [128, NT, NEXP])
            mx8 = sbt("mx8", [128, NT, 8])
            mk1 = sbt("mk1", [128, NT, NEXP])
            mk2 = sbt("mk2", [128, NT, NEXP])
            gates = sbt("gates", [128, NT, NEXP])
            w12 = sbt("w12", [128, 3, NT])
            B.dma("sp", wr[:, :, :], I["router_w"][j, :, :, :], w=["wr"])
            B.dma("sp", rb[:, :], I["router_b"][j, :, :], w=["rb"])
            B.copy("dve", wrb[:, :, :], wr[:, :, :], r=["wr"], w=["wrb"])
            bank = 6
            for t in tiles:
                B.mm(ps[:, bank, t * 8:(t + 1) * 8],
                     [(C.xT[:, kc, t * 128:(t + 1) * 128], wrb[:, kc, :]) for kc in range(KC)],
                     r=[("xT", t), "wrb"], w=[("ps", bank)])
            t0, t1 = tiles[0], tiles[-1] + 1
            n = t1 - t0
            B.tt("dve", lg[:, t0:t1, :], ps[:, bank, t0 * 8:t1 * 8].rearrange("p (t e) -> p t e", e=8),
                 rb[:, :].unsqueeze(1).to_broadcast([128, n, NEXP]), ALU.add, r=[("ps", bank), "rb"], w=["lg"])
            for t in tiles:
                P.add("dve", lambda e, t=t: e.max(out=mx8[:, t, :], in_=lg[:, t, :]), r=["lg"], w=["mx8"])
            B.tt("dve", mk1[:, t0:t1, :], lg[:, t0:t1, :], mx8[:, t0:t1, 0:1].to_broadcast([128, n, NEXP]), ALU.is_equal,
                 r=["lg", "mx8"], w=["mk1"])
            B.tt("dve", mk2[:, t0:t1, :], lg[:, t0:t1, :], mx8[:, t0:t1, 1:2].to_broadcast([128, n, NEXP]), ALU.is_equal,
                 r=["lg", "mx8"], w=["mk2"])
            B.tt("dve", w12[:, 0, t0:t1], mx8[:, t0:t1, 1], mx8[:, t0:t1, 0], ALU.subtract, r=["mx8"], w=["w12a"])
            B.act(w12[:, 0, t0:t1], w12[:, 0, t0:t1], AF.Exp, r=["w12a"], w=["w12a"])
            B.ts("dve", w12[:, 1, t0:t1], w12[:, 0, t0:t1], 1.0, ALU.add, r=["w12a"], w=["w12b"])
            P.add("dve", lambda e: e.reciprocal(out=w12[:, 1, t0:t1], in_=w12[:, 1, t0:t1]), r=["w12b"], w=["w12b"])
            B.tt("dve", w12[:, 2, t0:t1], w12[:, 0, t0:t1], w12[:, 1, t0:t1], ALU.mult, r=["w12a", "w12b"], w=["w12c"])
            B.tt("dve", mk1[:, t0:t1, :], mk1[:, t0:t1, :], w12[:, 1, t0:t1].unsqueeze(2).to_broadcast([128, n, NEXP]),
                 ALU.mult, r=["mk1", "w12b"], w=["mk1"])
            B.tt("dve", mk2[:, t0:t1, :], mk2[:, t0:t1, :], w12[:, 2, t0:t1].unsqueeze(2).to_broadcast([128, n, NEXP]),
                 ALU.mult, r=["mk2", "w12c"], w=["mk2"])
            B.tt("dve", gates[:, t0:t1, :], mk1[:, t0:t1, :], mk2[:, t0:t1, :], ALU.add, r=["mk1", "mk2"], w=["gates"])
        groups = [(e, fg) for e in range(NEXP if moe else 1) for fg in range(NFG)]
        poolA = PsumPool([0, 1, 2, 3])
        poolB = PsumPool([4, 5, 6, 7])
        sgi = [0]

        def loads_gu(i):
            e, fg = groups[i]
            s = i % 2
            src_gu = I["exp_gu"][j, e, fg] if moe else I["ffn_gu"][j, fg]
            B.dma("pool", wgu[:, s, :, :, :], src_gu.rearrange("g p k f -> p g k f"), w=[("wgu", s)])

        def loads_dn(i):
            e, fg = groups[i]
            s = i % 2
            src_dn = I["exp_dn"][j, e, fg * FG:(fg + 1) * FG, :] if moe else I["ffn_dn"][j, fg * FG:(fg + 1) * FG, :]
            B.dma("sp", wdf[:, 0, :, :], src_dn.rearrange("(c p) d -> p c d", p=128), w=["wdf"])
            B.tt("pool", wdl[:, s, :, :], wdf[:, 0, :, :], g2bc[:, 0:1, :].to_broadcast([128, 2, D]), ALU.mult,
                 r=["wdf", "g2bc"], w=[("wdl", s)])
            if use_ctx:
                B.tt("pool", wdc[:, s, :, :], wdf[:, 0, :, :], g2bc[:, 1:2, :].to_broadcast([128, 2, D]), ALU.mult,
                     r=["wdf", "g2bc"], w=[("wdc", s)])

        def phaseA(i):
            s = i % 2
            for (t0, n) in tgs:
                tl = [("xT", t) for t in range(t0 // 128, (t0 + n) // 128)]
                for fc in range(2):
                    bg, bu = poolA.next(), poolA.next()
                    B.mm(ps[:, bg, 0:n], [(wgu[:, s, 0, kc, fc * 128:(fc + 1) * 128], C.xT[:, kc, t0:t0 + n]) for kc in range(KC)],
                         r=[("wgu", s)] + tl, w=[("ps", bg)])
                    B.mm(ps[:, bu, 0:n], [(wgu[:, s, 1, kc, fc * 128:(fc + 1) * 128], C.xT[:, kc, t0:t0 + n]) for kc in range(KC)],
                         r=[("wgu", s)] + tl, w=[("ps", bu)])
                    k = sgi[0] % 3
                    sgi[0] += 1
                    B.act(sg[:, k, 0:n], ps[:, bg, 0:n], AF.Silu, r=[("ps", bg)], w=[("sg", k)])
                    B.tt("dve", hT[:, s, fc, t0:t0 + n], sg[:, k, 0:n], ps[:, bu, 0:n], ALU.mult,
                         r=[("sg", k), ("ps", bu)], w=[("hT", s, fc, t0)])

        def phaseB(i):
            e, fg = groups[i]
            s = i % 2
            for t in tiles:
                wd = wdc if t < 2 else wdl
                tg0 = [t0 for (t0, n) in TGS if t0 <= t * 128 < t0 + n][0]
                for half in range(2):
                    bo = poolB.next()
                    B.mm(ps[:, bo, :], [(hT[:, s, fc, t * 128:(t + 1) * 128], wd[:, s, fc, half * 512:(half + 1) * 512])
                                        for fc in range(2)],
                         r=[("hT", s, 0, tg0), ("hT", s, 1, tg0), ("wdc" if t < 2 else "wdl", s)], w=[("ps", bo)])
                    xs = C.x[:, t, half * 512:(half + 1) * 512]
                    sc = gates[:, t, e:e + 1] if moe else 1.0
                    if half == 0:
                        B.stt(xs, ps[:, bo, :], sc, xs, ALU.mult, ALU.add,
                              r=[("ps", bo), ("x", t)] + (["gates"] if moe else []), w=[("x", t, 0)])
                    else:
                        kq = evi[0] % 2
                        evi[0] += 1
                        B.act(etmp[:, kq, :], ps[:, bo, :], AF.Identity, scale=sc,
                              r=[("ps", bo)] + (["gates"] if moe else []), w=[("etmp", kq)])
                        B.tt("pool", xs, xs, etmp[:, kq, :], ALU.add, r=[("etmp", kq), ("x", t)], w=[("x", t, 1)])

        n = len(groups)
        loads_gu(0)
        if n > 1:
            loads_gu(1)
        loads_dn(0)
        for i in range(n):
            phaseA(i)
            if i + 2 < n:
                loads_gu(i + 2)
            if i >= 1:
                phaseB(i - 1)
            if i + 1 < n:
                loads_dn(i + 1)
        phaseB(n - 1)


CH_QA, CH_QAR, CH_KA, CH_KAR = 0, 3, 6, 8
CH_QC, CH_QCR, CH_KC, CH_KCR = 10, 12, 14, 16
CH_XBC = 18
NCH = 25
MIXERS = "ACB"
B_STAGE = 9
B_SUB = 9
TK_VA, TK_VC, TK_DT, TK_Z, NTK = 0, 128, 384, 396, 780
SR_SINK, SR_DTB, SR_ALOG, SR_DSKIP, SR_LQ, SR_LK, SR_DNW, SR_SNW, NSR = 0, 6, 18, 30, 36, 100, 164, 228, 612
PADT = T + 8


def fm_chunks(C, li, ci_list, evac):
    B, ps, I = C.B, C.psum, C.I
    ws = []
    for ci in ci_list:
        s = C.wfm_i % 4
        C.wfm_i += 1
        B.dma("pool", C.wfm[:, s, :, :], I["w_fm"][li, ci, :, :, :], w=[("wfm", s)])
        ws.append(s)
    for gi, (t0, n) in enumerate(TGS):
        tl = [("xT", t) for t in range(t0 // 128, (t0 + n) // 128)]
        banks = []
        for s in ws:
            bk = C.poolF.next()
            B.mm(ps[:, bk, 0:n], [(C.wfm[:, s, kc, :], C.xT[:, kc, t0:t0 + n]) for kc in range(KC)],
                 r=[("wfm", s)] + tl, w=[("ps", bk)])
            banks.append(bk)
        evac(gi, t0, n, banks)


def rope_evac(C, dst, dtok, cos, sin):
    B, ps = C.B, C.psum

    def evac(gi, t0, n, banks):
        bx, br = banks
        if t0 < 256:
            B.copy("act", dst[:, t0:t0 + n], ps[:, bx, 0:n], r=[("ps", bx), ("ps", br)], w=[dtok + (gi,)])
            return
        k = C.rt_i % C.rt_n
        C.rt_i += 1
        B.tt("dve", C.rtmp[:, k, 0, 0:n], ps[:, bx, 0:n], cos[:, t0:t0 + n], ALU.mult, r=[("ps", bx), "rope"], w=[("rtmp", k, 0)])
        B.tt("dve", C.rtmp[:, k, 1, 0:n], ps[:, br, 0:n], sin[:, t0:t0 + n], ALU.mult, r=[("ps", br), "rope"], w=[("rtmp", k, 1)])
        B.tt("dve", dst[:, t0:t0 + n], C.rtmp[:, k, 0, 0:n], C.rtmp[:, k, 1, 0:n], ALU.add,
             r=[("rtmp", k, 0), ("rtmp", k, 1)], w=[dtok + (gi,)])
    return evac


def tg_of(t):
    return [i for i, (t0, n) in enumerate(TGS) if t0 <= t * 128 < t0 + n][0]


def out_proj_tile(C, t, y_bf, nchunk, wo, ytok):
    B, ps = C.B, C.psum
    wch = 1 if t < 2 else 0
    bk = C.poolT.next()
    psb = ps[:, bk, :].bitcast(BF16)

    def tr(e):
        ins = None
        for c in range(nchunk):
            ins = e.transpose(out=psb[:, c * 128:(c + 1) * 128], in_=y_bf[:, c * 128:(c + 1) * 128], identity=C.identb[:, :])
        return ins
    C.P.add("pe", tr, r=[ytok, "identb"], w=[("ps", bk)])
    k = C.yT_i % 2
    C.yT_i += 1
    B.copy("act", C.yT[:, k, 0:nchunk * 128], psb[:, 0:nchunk * 128], r=[("ps", bk)], w=[("yT", k)])
    for half in range(2):
        bo = C.poolO.next()
        B.mm(ps[:, bo, :], [(C.yT[:, k, c * 128:(c + 1) * 128], wo[:, c, half * 512:(half + 1) * 512]) for c in range(nchunk)],
             r=[("yT", k), "wo"], w=[("ps", bo)])
        m = 0
        B.tt("dve", C.otmp[:, m, :], ps[:, bo, :], C.g1bc[:, wch, half * 512:(half + 1) * 512], ALU.mult,
             r=[("ps", bo), "g1bc"], w=[("otmp", m)])
        xs = C.x[:, t, half * 512:(half + 1) * 512]
        B.tt("dve", xs, xs, C.otmp[:, m, :], ALU.add, r=[("otmp", m), ("x", t)], w=[("x", t)])


def mixer_phase(C, li, tiles):
    B, nc, I, ps, P = C.B, C.nc, C.I, C.psum, C.P
    lam_init = 0.8 - 0.6 * math.exp(-0.3 * li)
    C.g1bc = C.sb("g1bc", [128, 2, D])
    C.srow = C.sb("srow", [128, NSR])
    C.yT = C.sb("yT", [128, 2, 384], BF16)
    C.otmp = C.sb("otmp", [128, 1, 512])
    C.wfm_i = C.rt_i = C.yT_i = C.ot_i = 0
    C.poolF = PsumPool([0, 1, 2, 3])
    C.poolT = PsumPool([6])
    C.poolO = PsumPool([4, 5])
    bcast_rows(C, C.g1bc, 16, "g1bc")
    B.dma("sp", C.srow[:, :], I["srow"][li, :, :], w=["srow"])
    with contextlib.ExitStack() as sc:
        sba = lambda name, shape, dt=F32: sc.enter_context(nc.sbuf_tensor("%s_a%d" % (name, li), list(shape), dt))
        if "A" in MIXERS:
            mixer_A(C, li, tiles, sba)
        P.barrier()
    with contextlib.ExitStack() as sc:
        sba = lambda name, shape, dt=F32: sc.enter_context(nc.sbuf_tensor("%s_c%d" % (name, li), list(shape), dt))
        if "C" in MIXERS:
            mixer_C(C, li, tiles, sba, lam_init)
        P.barrier()
    with contextlib.ExitStack() as sc:
        sba = lambda name, shape, dt=F32: sc.enter_context(nc.sbuf_tensor("%s_b%d" % (name, li), list(shape), dt))
        if "B" in MIXERS:
            mixer_B(C, li, tiles, sba)
        P.barrier()


def tok_proj(C, li, t, col0, ncol, wt):
    B, ps = C.B, C.psum
    bk = C.poolF.next()
    B.mm(ps[:, bk, 0:ncol], [(C.xT[:, kc, t * 128:(t + 1) * 128], wt[:, kc, col0:col0 + ncol]) for kc in range(KC)],
         r=[("xT", t), "wtok"], w=[("ps", bk)])
    return bk


def mixer_A(C, li, tiles, sba):
    B, nc, I, ps, P = C.B, C.nc, C.I, C.psum, C.P
    C.wfm = sba("wfm", [128, 4, KC, 128], BF16)
    qT = sba("qT", [128, 3, T], BF16)
    kT = sba("kT", [128, 2, T], BF16)
    vA = sba("vA", [128, NT, 2, 66], BF16)
    rope = sba("rope", [128, 2, T], BF16)
    C.rtmp = sba("rtmp", [128, 2, 2, 512])
    C.rt_n = 2
    wt = sba("wtA", [128, KC, 128], BF16)
    wo = sba("woA", [128, 3, D], BF16)
    bm = sba("bmask", [128, 3, 128], BF16)
    E1 = sba("E1", [128, 3, 384], BF16)
    E2 = sba("E2", [128, 3, 256], BF16)
    esink = sba("esink", [128, 6])
    den = sba("den", [128, 2, 6])
    ya = sba("ya", [128, 2, 384], BF16)
    qm = sba("qm", [128, 3, 128], BF16)
    bandm = sba("bandm", [128, 6])
    B.dma("sp", bandm[:, :], I["bandm"][:, :], w=["bandm"])
    B.dma("sp", rope[:, :, :], I["ropeA"].rearrange("a p t -> p a t"), w=["rope"])
    B.dma("sp", bm[:, :, :], I["bmask"][:, :, :], w=["bmask"])
    B.dma("pool", wt[:, :, :], I["w_tok"][li, :, :, TK_VA:TK_VA + 128], w=["wtok"])
    B.dma("pool", wo[:, :, :], I["w_out"][li, 0:384, :].rearrange("(c p) d -> p c d", p=128), w=["wo"])
    B.memset("pool", vA[:, :, :, 64:66], 1.0, w=["vA1"])
    B.act(esink[:, :], C.srow[:, SR_SINK:SR_SINK + 6], AF.Exp, r=["srow"], w=["esink"])
    for c in range(3):
        fm_chunks(C, li, [CH_QA + c, CH_QAR + c], rope_evac(C, qT[:, c, :], ("qT", c), rope[:, 0, :], rope[:, 1, :]))
    for g in range(2):
        fm_chunks(C, li, [CH_KA + g, CH_KAR + g], rope_evac(C, kT[:, g, :], ("kT", g), rope[:, 0, :], rope[:, 1, :]))
    for t in range(NT):
        bk = tok_proj(C, li, t, 0, 128, wt)
        B.copy("act", vA[:, t, :, 0:64], ps[:, bk, 0:128].rearrange("p (g d) -> p g d", g=2), r=[("ps", bk)], w=[("vA", t)])
    scale = 64 ** -0.5
    poolS = PsumPool([0, 1, 2, 4])
    C.poolO = PsumPool([5])
    ei = 0
    for qi, t in enumerate(tiles):
        if t < 2:
            loc = []
        else:
            loc = [(j, t - 1 + j) for j in range(3) if 2 <= t - 1 + j < NT]
        bo = 7 if qi % 2 == 0 else 3
        pend = []
        for h in range(6):
            g, b, c = h // 3, h % 2, h // 2
            rows = slice(b * 64, (b + 1) * 64)
            qtok = ("qT", c, tg_of(t))
            e = ei % 3
            ei += 1
            pv = []
            if loc:
                b1 = poolS.next()
                for (j, kt) in loc:
                    B.mm(ps[:, b1, j * 128:(j + 1) * 128], [(kT[rows, g, kt * 128:(kt + 1) * 128], qT[rows, c, t * 128:(t + 1) * 128])],
                         r=[("kT", g, tg_of(kt)), qtok], w=[("ps", b1)])
                j0, j1 = loc[0][0], loc[-1][0] + 1
                B.act(E1[:, e, j0 * 128:j1 * 128], ps[:, b1, j0 * 128:j1 * 128], AF.Exp, scale=scale, r=[("ps", b1)], w=[("E1", e)])
                B.tt("dve", E1[:, e, j0 * 128:j1 * 128], E1[:, e, j0 * 128:j1 * 128],
                     bm[:, j0:j1, :].rearrange("p a b -> p (a b)"), ALU.mult, r=[("E1", e), "bmask"], w=[("E1", e)])
                pv += [(E1[:, e, j * 128:(j + 1) * 128], vA[:, kt, g, 0:65], ("E1", e), kt) for (j, kt) in loc]
            b2 = poolS.next()
            for kt in range(2):
                B.mm(ps[:, b2, kt * 128:(kt + 1) * 128], [(kT[rows, g, kt * 128:(kt + 1) * 128], qT[rows, c, t * 128:(t + 1) * 128])],
                     r=[("kT", g, 0), qtok], w=[("ps", b2)])
            B.act(E2[:, e, :], ps[:, b2, 0:256], AF.Exp, scale=scale, r=[("ps", b2)], w=[("E2", e)])
            pv += [(E2[:, e, kt * 128:(kt + 1) * 128], vA[:, kt, g, 0:65], ("E2", e), kt) for kt in range(2)]
            pend.append((ps[:, bo, h * 66:h * 66 + 65], [(l, r_) for (l, r_, _, _) in pv],
                         list({x[2] for x in pv}) + [("vA", x[3]) for x in pv] + ["vA1"]))
            while len(pend) > 2:
                o_, p_, r_ = pend.pop(0)
                B.mm(o_, p_, r=r_, w=[("ps", bo)])
        while pend:
            o_, p_, r_ = pend.pop(0)
            B.mm(o_, p_, r=r_, w=[("ps", bo)])
        k = qi % 2
        pv3 = ps[:, bo, 0:396].rearrange("p (h d) -> p h d", d=66)
        B.tt("dve", den[:, k, :], pv3[:, :, 64], esink[:, :], ALU.add, r=[("ps", bo), "esink"], w=[("den", k)])
        P.add("dve", lambda e_, k=k: e_.reciprocal(out=den[:, k, :], in_=den[:, k, :]), r=[("den", k)], w=[("den", k)])
        B.tt("dve", ya[:, k, :].rearrange("p (h d) -> p h d", d=64), pv3[:, :, 0:64],
             den[:, k, :].unsqueeze(2).to_broadcast([128, 6, 64]), ALU.mult, r=[("ps", bo), ("den", k)], w=[("ya", k)])
        if "mixA%d" % li in C.dbg:
            d = dbg_out(C, "mixA%d" % li, 384)
            B.dma("sp", d[t * 128:(t + 1) * 128, :], ya[:, k, :], r=[("ya", k)])
        out_proj_tile(C, t, ya[:, k, :], 3, wo, ("ya", k))


def mixer_C(C, li, tiles, sba, lam_init):
    B, nc, I, ps, P = C.B, C.nc, C.I, C.psum, C.P
    C.poolO = PsumPool([4, 5])
    C.wfm = sba("wfm", [128, 4, KC, 128], BF16)
    qT = sba("qT", [128, 2, T], BF16)
    kT = sba("kT", [128, 2, T], BF16)
    vC = sba("vC", [128, NT, 4, 66], BF16)
    rope = sba("rope", [128, 2, T], BF16)
    C.rtmp = sba("rtmp", [128, 1, 2, 512])
    C.rt_n = 1
    wt = sba("wtC", [128, KC, 256], BF16)
    wo = sba("woC", [128, 2, D], BF16)
    E = sba("E", [128, 5, 512], BF16)
    yd = sba("yd", [128, NT, 256], BF16)
    lam = sba("lam", [128, 8])
    lqk = sba("lqk", [128, 64])
    nw = sba("nw", [128, 64])
    r12 = sba("r12", [128, 1, 2, 4])
    o12 = sba("o12", [128, 1, 2, 4, 64])
    ss = sba("ss", [128, 1, 4])
    aT = sba("aT", [128, 2, 512])
    qm = sba("qm", [128, 2, 512], BF16)
    bandm = sba("bandm", [128, 6])
    B.dma("sp", bandm[:, :], I["bandm"][:, :], w=["bandm"])
    B.dma("sp", rope[:, :, :], I["ropeC"].rearrange("a p t -> p a t"), w=["rope"])
    B.dma("pool", wt[:, :, :], I["w_tok"][li, :, :, TK_VC:TK_VC + 256], w=["wtok"])
    B.dma("pool", wo[:, :, :], I["w_out"][li, 768:1024, :].rearrange("(c p) d -> p c d", p=128), w=["wo"])
    B.memset("pool", vC[:, :, :, 64:66], 1.0, w=["vC1"])
    B.memset("pool", aT[:, :, :], 0.0, w=[("aT", 0), ("aT", 1)])
    B.tt("dve", lqk[:, :], C.srow[:, SR_LQ:SR_LQ + 64], C.srow[:, SR_LK:SR_LK + 64], ALU.mult, r=["srow"], w=["lqk"])
    P.add("dve", lambda e: e.tensor_reduce(out=lam[:, 0:2], in_=lqk[:, :].rearrange("p (a b) -> p a b", a=2), axis=AX.X, op=ALU.add),
          r=["lqk"], w=["lam"])
    B.act(lam[:, 0:2], lam[:, 0:2], AF.Exp, r=["lam"], w=["lam"])
    B.tt("dve", lam[:, 2:3], lam[:, 1:2], lam[:, 0:1], ALU.subtract, r=["lam"], w=["lam"])
    B.ts("dve", lam[:, 4:5], lam[:, 2:3], -lam_init, ALU.add, r=["lam"], w=["lam"])
    B.ts("dve", nw[:, :], C.srow[:, SR_DNW:SR_DNW + 64], 1.0 - lam_init, ALU.mult, r=["srow"], w=["nw"])
    for c in range(2):
        fm_chunks(C, li, [CH_QC + c, CH_QCR + c], rope_evac(C, qT[:, c, :], ("qT", c), rope[:, 0, :], rope[:, 1, :]))
    for c in range(2):
        fm_chunks(C, li, [CH_KC + c, CH_KCR + c], rope_evac(C, kT[:, c, :], ("kT", c), rope[:, 0, :], rope[:, 1, :]))
    for t in range(NT):
        bk = tok_proj(C, li, t, 0, 256, wt)
        B.copy("act", vC[:, t, :, 0:64], ps[:, bk, 0:256].rearrange("p (g d) -> p g d", g=4), r=[("ps", bk)], w=[("vC", t)])
    scale = 32 ** -0.5
    poolS = PsumPool([0, 1, 2, 6])
    accs = PsumPool([3, 7, 4, 5])
    qgroups = []
    if 0 in tiles:
        qgroups.append((0, [0, 1], [0, 1]))
    for gi in range(1, 5):
        t0 = TGS[gi][0] // 128
        qgroups.append((gi, list(range(t0, t0 + 4)), list(range(NT))))
    ei = 0
    gk = 0
    ai_ = [0]
    for hc in range(4):
        cc = hc // 2
        for (gi, qts, kts) in qgroups:
            nq = len(qts)
            q0 = qts[0] * 128
            n = nq * 128
            k = 0
            gk += 1
            acc = []
            pend = []
            for i in range(2):
                j = 2 * (hc % 2) + i
                rows = slice(32 * j, 32 * j + 32)
                ab = accs.next()
                acc.append(ab)
                B.ts("dve", qm[:, i, 0:n], qT[:, cc, q0:q0 + n], bandm[:, j:j + 1], ALU.mult, r=[("qT", cc, gi), "bandm"], w=[("qm", i)])
                for ki, kt in enumerate(kts):
                    sbk = poolS.next()
                    B.mm(ps[:, sbk, 0:n], [(kT[:, cc, kt * 128:(kt + 1) * 128], qm[:, i, 0:n])],
                         r=[("kT", cc, tg_of(kt)), ("qm", i)], w=[("ps", sbk)])
                    e = ei % 5
                    ei += 1
                    B.act(E[:, e, 0:n], ps[:, sbk, 0:n], AF.Exp, scale=scale, r=[("ps", sbk)], w=[("E", e)])

                    def pv(eng, e=e, kt=kt, ab=ab, ki=ki, n=n, hc=hc, last=(ki == len(kts) - 1)):
                        return eng.matmul(ps[0:65, ab, 0:n], lhsT=vC[:, kt, hc, 0:65], rhs=E[:, e, 0:n], start=(ki == 0), stop=last)
                    pend.append((pv, [("E", e), ("vC", kt), "vC1"], [("ps", ab)]))
                    while len(pend) > 3:
                        f_, r_, w_ = pend.pop(0)
                        P.add("pe", f_, r=r_, w=w_)
            while pend:
                f_, r_, w_ = pend.pop(0)
                P.add("pe", f_, r=r_, w=w_)
            for i in range(2):
                ab = acc[i]
                m = ai_[0] % 2
                ai_[0] += 1
                B.copy("act", aT[0:65, m, 0:n], ps[0:65, ab, 0:n], r=[("ps", ab)], w=[("aT", m)])

                def trb(eng, ab=ab, m=m, nq=nq):
                    ins = None
                    for qi in range(nq):
                        ins = eng.transpose(out=ps[:, ab, qi * 66:qi * 66 + 66], in_=aT[0:66, m, qi * 128:(qi + 1) * 128],
                                            identity=C.identf[0:66, 0:66])
                    return ins
                P.add("pe", trb, r=[("aT", m), "identf"], w=[("ps", ab)])
            a1 = ps[:, acc[0], 0:nq * 66].rearrange("p (q d) -> p q d", d=66)
            a2 = ps[:, acc[1], 0:nq * 66].rearrange("p (q d) -> p q d", d=66)
            P.add("dve", lambda e_, k=k, a1=a1, nq=nq: e_.reciprocal(out=r12[:, k, 0, 0:nq], in_=a1[:, :, 64]), r=[("ps", acc[0])], w=[("r12", k)])
            P.add("dve", lambda e_, k=k, a2=a2, nq=nq: e_.reciprocal(out=r12[:, k, 1, 0:nq], in_=a2[:, :, 64]), r=[("ps", acc[1])], w=[("r12", k)])
            B.ts("dve", r12[:, k, 1, 0:nq], r12[:, k, 1, 0:nq], lam[:, 4:5], ALU.mult, r=[("r12", k), "lam"], w=[("r12", k)])
            B.tt("dve", o12[:, k, 0, 0:nq, :], a1[:, :, 0:64], r12[:, k, 0, 0:nq].unsqueeze(2).to_broadcast([128, nq, 64]), ALU.mult,
                 r=[("ps", acc[0]), ("r12", k)], w=[("o1", k)])
            B.tt("dve", o12[:, k, 1, 0:nq, :], a2[:, :, 0:64], r12[:, k, 1, 0:nq].unsqueeze(2).to_broadcast([128, nq, 64]), ALU.mult,
                 r=[("ps", acc[1]), ("r12", k)], w=[("o2", k)])
            B.tt("dve", o12[:, k, 0, 0:nq, :], o12[:, k, 0, 0:nq, :], o12[:, k, 1, 0:nq, :], ALU.add, r=[("o1", k), ("o2", k)], w=[("o1", k)])
            B.tt("dve", o12[:, k, 1, 0:nq, :], o12[:, k, 0, 0:nq, :], o12[:, k, 0, 0:nq, :], ALU.mult, r=[("o1", k)], w=[("o2", k)])
            P.add("dve", lambda e_, k=k, nq=nq: e_.tensor_reduce(out=ss[:, k, 0:nq], in_=o12[:, k, 1, 0:nq, :], axis=AX.X, op=ALU.add),
                  r=[("o2", k)], w=[("ss", k)])
            B.act(ss[:, k, 0:nq], ss[:, k, 0:nq], AF.Sqrt, scale=1.0 / 64, bias=EPS, r=[("ss", k)], w=[("ss", k)])
            P.add("dve", lambda e_, k=k, nq=nq: e_.reciprocal(out=ss[:, k, 0:nq], in_=ss[:, k, 0:nq]), r=[("ss", k)], w=[("ss", k)])
            B.tt("dve", o12[:, k, 0, 0:nq, :], o12[:, k, 0, 0:nq, :], ss[:, k, 0:nq].unsqueeze(2).to_broadcast([128, nq, 64]), ALU.mult,
                 r=[("o1", k), ("ss", k)], w=[("o1", k)])
            B.tt("dve", yd[:, qts[0]:qts[0] + nq, hc * 64:(hc + 1) * 64], o12[:, k, 0, 0:nq, :],
                 nw[:, :].unsqueeze(1).to_broadcast([128, nq, 64]), ALU.mult, r=[("o1", k), "nw"], w=[("yd", qt) for qt in qts])
    for t in tiles:
        if "mixC%d" % li in C.dbg:
            d = dbg_out(C, "mixC%d" % li, 256)
            B.dma("sp", d[t * 128:(t + 1) * 128, :], yd[:, t, :], r=[("yd", t)])
        out_proj_tile(C, t, yd[:, t, :], 2, wo, ("yd", t))


def mixer_B(C, li, tiles, sba):
    B, nc, I, ps, P = C.B, C.nc, C.I, C.psum, C.P
    C.poolO = PsumPool([4, 5])
    xsT = sba("xsT", [128, 3, T], BF16)
    BT = sba("BT", [128, 2, T], BF16)
    CT = sba("CT", [128, 2, T], BF16)
    wz = sba("wz", [128, KC, 384], BF16)
    wdt = sba("wdt", [128, KC, 12], BF16)
    wo = sba("woB", [128, 3, D], BF16)
    cst = sba("ssdc", [128, 4, 128])
    Abc = sba("Abc", [128, 12])
    hb_scr = B.dram_out("scr_hb%d" % li, [NT, 128, 384], BF16)
    B.dma("pool", wz[:, :, :], I["w_tok"][li, :, :, TK_Z:TK_Z + 384], w=["wtok"])
    B.dma("pool", wdt[:, :, :], I["w_tok"][li, :, :, TK_DT:TK_DT + 12], w=["wdt"])
    B.dma("pool", wo[:, :, :], I["w_out"][li, 384:768, :].rearrange("(c p) d -> p c d", p=128), w=["wo"])
    B.dma("sp", cst[:, :, :], I["ssdc"].rearrange("a p l -> p a l"), w=["ssdc"])
    B.act(Abc[:, :], C.srow[:, SR_ALOG:SR_ALOG + 12], AF.Exp, r=["srow"], w=["Abc"])
    B.ts("dve", Abc[:, :], Abc[:, :], -1.0, ALU.mult, r=["Abc"], w=["Abc"])
    with contextlib.ExitStack() as sc:
        sbc = lambda name, shape, dt=F32: sc.enter_context(nc.sbuf_tensor("%s_bc%d" % (name, li), list(shape), dt))
        C.wfm = sbc("wfm", [128, 4, KC, 128], BF16)
        pre = sbc("pre", [128, 2, PADT])
        acc = sbc("acc", [128, 2, 512])
        cp = sbc("convp", [128, 7, 6])
        B.dma("sp", cp[:, :, :], I["convp"][li, :, :, :], w=["convp"])
        for k in range(2):
            B.memset("pool", pre[:, k, :], 0.0, w=[("pre", k, gi) for gi in range(5)])
        dsts = [xsT[:, 0, :], xsT[:, 1, :], xsT[:, 2, :], BT[:, 0, :], BT[:, 1, :], CT[:, 0, :], CT[:, 1, :]]
        ai = 0
        for ci in range(7):
            k = ci % 2

            def evac(gi, t0, n, banks, k=k):
                off = 2 if t0 < 256 else 6
                B.copy("act", pre[:, k, off + t0:off + t0 + n], ps[:, banks[0], 0:n], r=[("ps", banks[0])], w=[("pre", k, gi)])
            fm_chunks(C, li, [CH_XBC + ci], evac)
            for gi, (t0, n) in enumerate(TGS):
                a = ai % 2
                ai += 1
                base = t0 if t0 < 256 else t0 + 4
                rd = [("pre", k, g2) for g2 in range(5)]
                B.ts("dve", acc[:, a, 0:n], pre[:, k, base:base + n], cp[:, ci, 0:1], ALU.mult, r=rd + ["convp"], w=[("acc", a)])
                for tap in range(1, 5):
                    B.stt(acc[:, a, 0:n], pre[:, k, base + tap:base + tap + n], cp[:, ci, tap:tap + 1], acc[:, a, 0:n], ALU.mult, ALU.add,
                          r=rd + ["convp", ("acc", a)], w=[("acc", a)])
                B.act(dsts[ci][:, t0:t0 + n], acc[:, a, 0:n], AF.Silu, bias=cp[:, ci, 5:6], r=[("acc", a), "convp"], w=[("u", ci, gi)])
        P.barrier()
    if B_STAGE < 1:
        return
    with contextlib.ExitStack() as sc:
        sbp = lambda name, shape, dt=F32: sc.enter_context(nc.sbuf_tensor("%s_bp%d" % (name, li), list(shape), dt))
        sc_all = sbp("sc_all", [128, 4, NT, 12])
        ect_all = sbp("ect_all", [128, NT, 12])
        AT = sbp("AT", [128, 6, 128])
        Dm = sbp("Dm", [128, 1, 3, 128])
        E = sbp("E", [128, 2, 3, 128], BF16)
        E0 = sbp("E0", [128, 2, 3, 128], BF16)
        MT = sbp("MT", [128, 1, 12, 128], BF16)
        CTp = sbp("CTp", [128, 1, 12, 128], BF16)
        xtok = sbp("xtok", [128, 2, 6, 64], BF16)
        Btok = sbp("Btok", [128, 2, 2, 128], BF16)
        Xdt = sbp("Xdt", [128, 2, 12, 64], BF16)
        Xw = sbp("Xw", [128, 2, 12, 64], BF16)
        H32 = sbp("H32", [128, 2, 6, 64])
        H16 = sbp("H16", [128, 6, 64], BF16)
        Hbin = sbp("Hbin", [128, 2, 6, 64], BF16)
        yb32 = sbp("yb32", [128, 384])
        tmp32 = sbp("tmp32", [128, 384])
        sz = tmp32
        ssq = sbp("ssq", [128, 2])
        yb16 = sbp("yb16", [128, 2, 384], BF16)
        poolP = PsumPool([0, 1, 2])
        C.poolF = PsumPool([0, 1, 2])
        st_i = [0]
        dt_a, a_a, cum_a, dtw_a = [sc_all[:, i, :, :] for i in range(4)]
        mtf = MT[:, 0, :, :].rearrange("p h l -> p (h l)").bitcast(F32)
        xb, ax, mx = [mtf[:, i * 216:(i + 1) * 216].rearrange("p (t h) -> p t h", h=12) for i in range(3)]
        fl = lambda ap: ap.rearrange("p t h -> p (t h)")
        for t in range(NT):
            B.mm(ps[:, 0, t * 12:(t + 1) * 12], [(C.xT[:, kc, t * 128:(t + 1) * 128], wdt[:, kc, :]) for kc in range(KC)],
                 r=[("xT", t), "wdt"], w=[("ps", 0)])
        B.tt("dve", xb, ps[:, 0, 0:NT * 12].rearrange("p (t h) -> p t h", h=12),
             C.srow[:, SR_DTB:SR_DTB + 12].unsqueeze(1).to_broadcast([128, NT, 12]), ALU.add, r=[("ps", 0), "srow"], w=["sc0"])
        B.act(fl(ax), fl(xb), AF.Abs, r=["sc0"], w=["sc1"])
        B.act(fl(ax), fl(ax), AF.Exp, scale=-1.0, r=["sc1"], w=["sc1"])
        B.act(fl(ax), fl(ax), AF.Ln, bias=1.0, r=["sc1"], w=["sc1"])
        B.ts("dve", fl(mx), fl(xb), 0.0, ALU.max, r=["sc0"], w=["sc2"])
        B.tt("dve", fl(dt_a), fl(mx), fl(ax), ALU.add, r=["sc1", "sc2"], w=["dt_a"])
        B.tt("dve", a_a, dt_a, Abc[:, :].unsqueeze(1).to_broadcast([128, NT, 12]), ALU.mult, r=["dt_a", "Abc"], w=["a_a"])
        for t in range(NT):
            B.mm(ps[:, 1, t * 12:t * 12 + 6], [(cst[:, 0, :], a_a[:, t, 0:6])], r=["ssdc", "a_a"], w=[("ps", 1)])
            B.mm(ps[:, 1, t * 12 + 6:t * 12 + 12], [(cst[:, 1, :], a_a[:, t, 6:12])], r=["ssdc", "a_a"], w=[("ps", 1)])
            B.mm(ps[:, 2, t * 12:(t + 1) * 12], [(C.onesf[:, :], a_a[:, t, :])], r=["onesf", "a_a"], w=[("ps", 2)])
        B.copy("act", fl(cum_a), ps[:, 1, 0:NT * 12], r=[("ps", 1)], w=["cum_a"])
        B.copy("act", fl(ect_all[:, :, :]), ps[:, 2, 0:NT * 12], r=[("ps", 2)], w=["ect"])
        B.tt("dve", fl(dtw_a), fl(ect_all[:, :, :]), fl(cum_a), ALU.subtract, r=["ect", "cum_a"], w=["dtw_a"])
        B.act(fl(dtw_a), fl(dtw_a), AF.Exp, r=["dtw_a"], w=["dtw_a"])
        B.tt("dve", fl(dtw_a), fl(dtw_a), fl(dt_a), ALU.mult, r=["dtw_a", "dt_a"], w=["dtw_a"])
        B.act(fl(ect_all[:, :, :]), fl(ect_all[:, :, :]), AF.Exp, r=["ect", "dtw_a"], w=["ect"])
        P.barrier()

        def prep(t, full):
            k = st_i[0] % 2
            st_i[0] += 1
            tgt = tg_of(t)
            av = a_a[:, t, :]
            bk = poolP.next()
            psb = ps[:, bk, :].bitcast(BF16)

            def tr(e, t=t, psb=psb):
                ins = None
                for c in range(3):
                    ins = e.transpose(out=psb[:, c * 128:(c + 1) * 128], in_=xsT[:, c, t * 128:(t + 1) * 128], identity=C.identb[:, :])
                for g in range(2):
                    ins = e.transpose(out=psb[:, 384 + g * 128:384 + (g + 1) * 128], in_=BT[:, g, t * 128:(t + 1) * 128], identity=C.identb[:, :])
                return ins
            P.add("pe", tr, r=[("u", ci, tgt) for ci in range(5)] + ["identb"], w=[("ps", bk)])
            B.copy("act", xtok[:, k, :, :].rearrange("p h d -> p (h d)"), psb[:, 0:384], r=[("ps", bk)], w=[("xtok", k)])
            B.copy("act", Btok[:, k, :, :].rearrange("p g n -> p (g n)"), psb[:, 384:640], r=[("ps", bk)], w=[("Btok", k)])
            dirs = (0, 1) if full else (1,)
            for d in dirs:
                B.tt("dve", Xw[:, k, d * 6:(d + 1) * 6, :], xtok[:, k, :, :],
                     dtw_a[:, t, d * 6:(d + 1) * 6].unsqueeze(2).to_broadcast([128, 6, 64]),
                     ALU.mult, r=[("xtok", k), "dtw_a"], w=[("Xw", k, d)])
            if not full:
                return k
            for d in range(2):
                B.tt("dve", Xdt[:, k, d * 6:(d + 1) * 6, :], xtok[:, k, :, :],
                     dt_a[:, t, d * 6:(d + 1) * 6].unsqueeze(2).to_broadcast([128, 6, 64]),
                     ALU.mult, r=[("xtok", k), "dt_a"], w=[("Xdt", k, d)])
            return k

        def prep2(t):
            tgt = tg_of(t)
            av = a_a[:, t, :]
            bg = 3
            for g in range(2):
                B.mm(ps[:, bg, g * 128:(g + 1) * 128], [(BT[:, g, t * 128:(t + 1) * 128], CT[:, g, t * 128:(t + 1) * 128])],
                     r=[("u", 3 + g, tgt), ("u", 5 + g, tgt)], w=[("ps", bg)])
            for d in range(2):
                B.tt("dve", AT[:, :, :], cst[:, d, :].unsqueeze(1).to_broadcast([128, 6, 128]),
                     av[:, d * 6:(d + 1) * 6].unsqueeze(2).to_broadcast([128, 6, 128]), ALU.mult, r=["ssdc", "a_a"], w=["AT"])
                for g in range(2):
                    hd0 = d * 6 + g * 3
                    bc = poolP.next()
                    B.mm(ps[:, bc, 0:384], [(C.onesf[:, :], AT[:, g * 3:(g + 1) * 3, :].rearrange("p h l -> p (h l)"))],
                         r=["onesf", "AT"], w=[("ps", bc)])
                    cr = ps[:, bc, 0:384].rearrange("p (h l) -> p h l", h=3)
                    m = 0
                    for i3 in range(3):
                        B.stt(Dm[:, m, i3, :], cr[:, i3, :], cum_a[:, t, hd0 + i3:hd0 + i3 + 1], cst[:, 2 + d, :], ALU.subtract, ALU.add,
                              r=[("ps", bc), "cum_a", "ssdc"], w=[("Dm", m)])
                    B.act(E[:, g, :, :], Dm[:, m, :, :], AF.Exp, r=[("Dm", m)], w=[("E", g)])
                    B.act(E0[:, g, :, :], cr, AF.Exp, r=[("ps", bc)], w=[("E0", g)])
                    for i3 in range(3):
                        B.tt("dve", MT[:, 0, hd0 + i3, :], E[:, g, i3, :], ps[:, bg, g * 128:(g + 1) * 128], ALU.mult,
                             r=[("E", g), ("ps", bg)], w=[("MT", 0, hd0)])
                    B.tt("dve", CTp[:, 0, hd0:hd0 + 3, :], E0[:, g, :, :],
                         CT[:, g, t * 128:(t + 1) * 128].unsqueeze(1).to_broadcast([128, 3, 128]), ALU.mult,
                         r=[("E0", g), ("u", 5 + g, tgt)], w=[("CTp", 0, hd0)])

        def state_update(d, k, t):
            bh = poolP.next()
            for h in range(6):
                B.mm(ps[:, bh, h * 64:(h + 1) * 64], [(Btok[:, k, h // 3, :], Xw[:, k, d * 6 + h, :])],
                     r=[("Btok", k), ("Xw", k, d)], w=[("ps", bh)])
            for h in range(6):
                B.stt(H32[:, d, h, :], H32[:, d, h, :], ect_all[:, t, d * 6 + h:d * 6 + h + 1], ps[:, bh, h * 64:(h + 1) * 64],
                      ALU.mult, ALU.add, r=[("H32", d, h), "ect", ("ps", bh)], w=[("H32", d, h)])

        B.memset("pool", H32[:, :, :, :], 0.0, w=[("H32", d_, h_) for d_ in range(2) for h_ in range(6)])
        border = [1, 0] + list(range(NT - 1, 1, -1))
        kn = prep(border[0], False)
        for i, t in enumerate(border):
            k2 = t % 2
            B.copy("act", Hbin[:, k2, :, :], H32[:, 1, :, :], r=[("H32", 1, h_) for h_ in range(6)], w=[("Hbin", k2)])
            B.dma("sp", hb_scr[t, :, :], Hbin[:, k2, :, :].rearrange("p h d -> p (h d)"), r=[("Hbin", k2)], w=[("hbscr", t)])
            if t == 2:
                break
            k = kn
            if border[i + 1] != 2:
                kn = prep(border[i + 1], False)
            state_update(1, k, t)
        kn = prep(0, True)
        prep2(0)
        for t in range(NT):
            need_y = t in tiles
            k = kn
            if t + 1 < NT:
                kn = prep(t + 1, True)
            if need_y:
                k2 = t % 2
                B.dma("sp", Hbin[:, k2, :, :].rearrange("p h d -> p (h d)"), hb_scr[t, :, :], r=[("hbscr", t)], w=[("Hbin", k2)])
                B.copy("act", H16[:, :, :], H32[:, 0, :, :], r=[("H32", 0, h_) for h_ in range(6)], w=["H16"])
                by = 7
                for h in range(6):
                    B.mm(ps[:, by, h * 64:(h + 1) * 64],
                         [(MT[:, 0, h, :], Xdt[:, k, h, :]), (CTp[:, 0, h, :], H16[:, h, :]),
                          (MT[:, 0, 6 + h, :], Xdt[:, k, 6 + h, :]), (CTp[:, 0, 6 + h, :], Hbin[:, k2, h, :])],
                         r=[("MT", 0, (h // 3) * 3), ("MT", 0, 6 + (h // 3) * 3), ("CTp", 0, (h // 3) * 3), ("CTp", 0, 6 + (h // 3) * 3),
                            ("Xdt", k, 0), ("Xdt", k, 1), "H16", ("Hbin", k2)], w=[("ps", by)])
            if t + 1 < NT:
                prep2(t + 1)
            if need_y:
                B.tt("dve", tmp32[:, :].rearrange("p (h d) -> p h d", h=6), xtok[:, k, :, :],
                     C.srow[:, SR_DSKIP:SR_DSKIP + 6].unsqueeze(2).to_broadcast([128, 6, 64]), ALU.mult, r=[("xtok", k), "srow"], w=["tmp32"])
                B.tt("dve", yb32[:, :], ps[:, by, 0:384], tmp32[:, :], ALU.add, r=[("ps", by), "tmp32"], w=["yb32"])
                bz = tok_proj(C, li, t, 0, 384, wz)
                B.act(sz[:, :], ps[:, bz, 0:384], AF.Silu, r=[("ps", bz)], w=["tmp32"])
                B.tt("dve", yb32[:, :], yb32[:, :], sz[:, :], ALU.mult, r=["yb32", "tmp32"], w=["yb32"])
                B.act(tmp32[:, :], yb32[:, :], AF.Square, accum_out=ssq[:, 0:1], r=["yb32"], w=["tmp32", "ssq"])
                B.act(ssq[:, 1:2], ssq[:, 0:1], AF.Sqrt, scale=1.0 / 384, bias=EPS, r=["ssq"], w=["ssq"])
                P.add("dve", lambda e_: e_.reciprocal(out=ssq[:, 1:2], in_=ssq[:, 1:2]), r=["ssq"], w=["ssq"])
                B.stt(yb16[:, k2, :], yb32[:, :], ssq[:, 1:2], C.srow[:, SR_SNW:SR_SNW + 384], ALU.mult, ALU.mult,
                      r=["yb32", "ssq", "srow"], w=[("yb16", k2)])
                if "mixB%d" % li in C.dbg:
                    d_ = dbg_out(C, "mixB%d" % li, 384)
                    B.dma("sp", d_[t * 128:(t + 1) * 128, :], yb16[:, k2, :], r=[("yb16", k2)])
                out_proj_tile(C, t, yb16[:, k2, :], 3, wo, ("yb16", k2))
            if t < NT - 1:
                state_update(0, k, t)
        P.barrier()


_NC_CACHE = {}


def kernel(**inputs):
    inp = {k: np.asarray(v) for k, v in inputs.items()}
    if "nc" not in _NC_CACHE:
        nc = bass.Bass("TRN2", target_bir_lowering=False)
        build_program(nc, nlayers=DEPTH)
        _NC_CACHE["nc"] = nc
    nc = _NC_CACHE["nc"]
    shared = _shared_inputs(inp)
    in_maps = [host_inputs(inp, b, shared) for b in range(8)]
    res = run_bass_kernel_spmd(nc, in_maps, core_ids=list(range(8)))
    out = np.stack([np.asarray(r["out"], dtype=np.float32) for r in res.results], axis=0)
    return out
```
